# Optimizing a Trainium2 kernel written in Bass

```python
import math
import jax, jax.numpy as jnp
from jax import lax
import numpy as np

D_MODEL = 1024
BATCH = 4
SEQ = 8192
DEPTH = 4

GRID_W = 64
CTX_LEN = 256
S5_GROUP = 16
S5_GROUPS = D_MODEL // S5_GROUP
S5_STATE = 64
S5_DT_MIN = 0.001
S5_DT_MAX = 0.1
GLA_HEADS = 4
GLA_DK = D_MODEL // (2 * GLA_HEADS)
GLA_DV = D_MODEL // GLA_HEADS
GLA_GATE_RANK = 16
GLA_GATE_NORM = 16.0
GLA_CHUNK = 64
N_EXPERTS = 32
TOP_K = 4
EXPERT_FF = D_MODEL
SWIGLU_ALPHA = 1.702
SWIGLU_LIMIT = 7.0
MOE_BLOCK = 128
DN_ALPHA = (2 * DEPTH) ** 0.25
DN_BETA = (8 * DEPTH) ** -0.25
LN_EPS = 1e-5
N_S5_LAYERS = (DEPTH + 1) // 2
N_GLA_LAYERS = DEPTH // 2

kernel_name = 'hybrid_s5_gla_moe_deepnorm_dit'

F32 = jnp.float32


def _layer_norm(x, g, b):
    xf = x.astype(F32)
    mu = jnp.mean(xf, axis=-1, keepdims=True)
    var = jnp.mean(jnp.square(xf - mu), axis=-1, keepdims=True)
    return ((xf - mu) * lax.rsqrt(var + LN_EPS) * g.astype(F32) + b.astype(F32)).astype(x.dtype)


def _modulate(x, shift, scale):
    return x * (1 + scale) + shift


def _s5_discretize(lam_re, lam_im, log_step, b_re, b_im):
    lr = lam_re.astype(F32)
    li = lam_im.astype(F32)
    dt = jnp.exp(log_step.astype(F32))[:, None]
    mag = jnp.exp(lr * dt)
    ar = mag * jnp.cos(li * dt)
    ai = mag * jnp.sin(li * dt)
    den = lr * lr + li * li
    nr = ar - 1.0
    kr = (nr * lr + ai * li) / den
    ki = (ai * lr - nr * li) / den
    br = b_re.astype(F32)
    bi = b_im.astype(F32)
    bbr = kr[..., None] * br - ki[..., None] * bi
    bbi = kr[..., None] * bi + ki[..., None] * br
    return ar, ai, bbr, bbi


def _cplx_affine_op(e1, e2):
    a1r, a1i, b1r, b1i = e1
    a2r, a2i, b2r, b2i = e2
    return (a2r * a1r - a2i * a1i, a2r * a1i + a2i * a1r,
            a2r * b1r - a2i * b1i + b2r, a2r * b1i + a2i * b1r + b2i)


def _s5_scan(u, ar, ai, bbr, bbi, cr, ci, s0, reverse, readout):
    l = u.shape[1]
    bu_re = jnp.einsum('blgh,gph->blgp', u, bbr)
    bu_im = jnp.einsum('blgh,gph->blgp', u, bbi)
    shp = (1, l) + ar.shape
    acc_re, acc_im, s_re, s_im = lax.associative_scan(
        _cplx_affine_op,
        (jnp.broadcast_to(ar, shp), jnp.broadcast_to(ai, shp), bu_re, bu_im),
        reverse=reverse, axis=1)
    if s0 is not None:
        s0r = s0[0][:, None]
        s0i = s0[1][:, None]
        s_re = s_re + acc_re * s0r - acc_im * s0i
        s_im = s_im + acc_re * s0i + acc_im * s0r
    end = 0 if reverse else l - 1
    s_end = (s_re[:, end], s_im[:, end])
    if not readout:
        return None, s_end
    y = jnp.einsum('blgp,ghp->blgh', s_re, cr) - jnp.einsum('blgp,ghp->blgh', s_im, ci)
    return y, s_end


def _s5_glu(y, w_glu):
    d = y.shape[-1]
    z = jax.nn.gelu(y) @ w_glu.astype(F32)
    return z[..., :d] * jax.nn.sigmoid(z[..., d:])


def _s5_mixer(u, uc, lam_re, lam_im, log_step, b_re, b_im, c_re, c_im, d_skip, w_glu, ctx_out):
    bn, l, d = u.shape
    lc = uc.shape[1]
    ul = u.astype(F32)
    ucf = uc.astype(F32)
    dsk = d_skip.astype(F32)
    grp = lambda t: t.reshape(t.shape[0], t.shape[1], S5_GROUPS, S5_GROUP)
    y = dsk * ul
    yc = dsk * ucf if ctx_out else None
    for dr, rev in ((0, False), (1, True)):
        ar, ai, bbr, bbi = _s5_discretize(lam_re[dr], lam_im[dr], log_step[dr], b_re[dr], b_im[dr])
        cr = c_re[dr].astype(F32)
        ci = c_im[dr].astype(F32)
        y_ctx, s_ctx = _s5_scan(grp(ucf), ar, ai, bbr, bbi, cr, ci, None, rev, ctx_out)
        y_lat, _ = _s5_scan(grp(ul), ar, ai, bbr, bbi, cr, ci, s_ctx, rev, True)
        y = y + y_lat.reshape(bn, l, d)
        if ctx_out:
            yc = yc + y_ctx.reshape(bn, lc, d)
    out = _s5_glu(y, w_glu).astype(u.dtype)
    out_c = _s5_glu(yc, w_glu).astype(uc.dtype) if ctx_out else None
    return out, out_c


def _gla_chunked(q, k, v, log_a, s0):
    bn, nh, l, dk = q.shape
    dv = v.shape[-1]
    n = l // GLA_CHUNK
    q = q.reshape(bn, nh, n, GLA_CHUNK, dk)
    k = k.reshape(bn, nh, n, GLA_CHUNK, dk)
    v = v.reshape(bn, nh, n, GLA_CHUNK, dv)
    b = jnp.cumsum(log_a.reshape(bn, nh, n, GLA_CHUNK, dk), axis=3)
    b_end = b[:, :, :, -1:, :]
    q_d = q * jnp.exp(b)
    k_d = k * jnp.exp(-b)
    k_e = k * jnp.exp(b_end - b)
    tri = jnp.tril(jnp.ones((GLA_CHUNK, GLA_CHUNK), dtype=bool))
    att = jnp.where(tri, jnp.einsum('bhnid,bhnjd->bhnij', q_d, k_d), 0.0)
    o = jnp.einsum('bhnij,bhnje->bhnie', att, v)
    u_chunk = jnp.einsum('bhnjd,bhnje->nbhde', k_e, v)
    g_chunk = jnp.moveaxis(jnp.exp(b_end[:, :, :, 0, :]), 2, 0)

    def step(s, inp):
        g, uc = inp
        return s * g[..., None] + uc, s

    s_end, s_prev = lax.scan(step, s0, (g_chunk, u_chunk))
    o = o + jnp.einsum('bhnid,nbhde->bhnie', q_d, s_prev)
    return o.reshape(bn, nh, l, dv), s_end


def _gla_mixer(u, uc, rows, w_in, w_a2, b_a2, norm_g, w_out, ctx_out):
    bn, l, d = u.shape
    u_cm = u.reshape(bn, rows, GRID_W, d).transpose(0, 2, 1, 3).reshape(bn, l, d)
    qk_w = GLA_HEADS * GLA_DK
    v_w = GLA_HEADS * GLA_DV
    splits = np.cumsum([qk_w, qk_w, v_w, v_w, GLA_GATE_RANK])

    def heads(t, hd):
        return t.reshape(t.shape[0], t.shape[1], GLA_HEADS, hd).transpose(0, 2, 1, 3)

    def project(z):
        p = (z @ w_in).astype(F32)
        q, k, v, g, a_f, a_b = jnp.split(p, splits, axis=-1)
        log_a = [heads(jax.nn.log_sigmoid(a @ w_a2[dr].astype(F32) + b_a2[dr].astype(F32)) / GLA_GATE_NORM, GLA_DK)
                 for dr, a in enumerate((a_f, a_b))]
        return heads(q, GLA_DK) * (GLA_DK ** -0.5), heads(k, GLA_DK), heads(v, GLA_DV), g, log_a

    def readout(o, g):
        o = o * lax.rsqrt(jnp.mean(o * o, axis=-1, keepdims=True) + LN_EPS) * norm_g.astype(F32)
        o = o.transpose(0, 2, 1, 3).reshape(g.shape)
        return (o * jax.nn.silu(g)) @ w_out.astype(F32)

    flip = lambda t: jnp.flip(t, axis=2)
    ql, kl, vl, gl, la_l = project(u_cm)
    qc, kc, vc, gc, la_c = project(uc)
    s_zero = jnp.zeros((bn, GLA_HEADS, GLA_DK, GLA_DV), F32)
    oc_f, sc_f = _gla_chunked(qc, kc, vc, la_c[0], s_zero)
    ol_f, _ = _gla_chunked(ql, kl, vl, la_l[0], sc_f)
    oc_b, sc_b = _gla_chunked(flip(qc), flip(kc), flip(vc), flip(la_c[1]), s_zero)
    ol_b, _ = _gla_chunked(flip(ql), flip(kl), flip(vl), flip(la_l[1]), sc_b)
    y = readout(ol_f + flip(ol_b), gl)
    y = y.reshape(bn, GRID_W, rows, d).transpose(0, 2, 1, 3).reshape(bn, l, d).astype(u.dtype)
    y_c = readout(oc_f + flip(oc_b), gc).astype(uc.dtype) if ctx_out else None
    return y, y_c


def _moe(xf, w_router, b_router, w_up, b_up, w_down, b_down):
    t, d = xf.shape
    tk = t * TOP_K
    logits = (xf @ w_router).astype(F32) + b_router.astype(F32)
    top_v, top_i = lax.top_k(logits, TOP_K)
    gate = jax.nn.softmax(top_v, axis=-1)
    e_flat = top_i.reshape(-1)
    tok_flat = jnp.arange(tk, dtype=jnp.int32) // TOP_K
    order = jnp.argsort(e_flat)
    e_s = e_flat[order]
    tok_s = tok_flat[order]
    g_s = gate.reshape(-1)[order]
    counts = jnp.bincount(e_flat, length=N_EXPERTS)
    starts = jnp.cumsum(counts) - counts
    padded = (counts + MOE_BLOCK - 1) // MOE_BLOCK * MOE_BLOCK
    pends = jnp.cumsum(padded)
    pstarts = pends - padded
    dest = pstarts[e_s] + jnp.arange(tk, dtype=jnp.int32) - starts[e_s]
    n_blocks = -(-tk // MOE_BLOCK) + N_EXPERTS
    n_rows = n_blocks * MOE_BLOCK
    x_pad = jnp.zeros((n_rows, d), xf.dtype).at[dest].set(xf[tok_s])
    tok_pad = jnp.zeros((n_rows,), jnp.int32).at[dest].set(tok_s)
    g_pad = jnp.zeros((n_rows,), F32).at[dest].set(g_s)
    blk_e = jnp.minimum(jnp.searchsorted(pends, jnp.arange(n_blocks, dtype=jnp.int32) * MOE_BLOCK, side='right'),
                        N_EXPERTS - 1)

    def expert_block(args):
        xb, e = args
        h = (xb @ w_up[e] + b_up[e]).astype(F32)
        h_glu, h_lin = jnp.split(h, 2, axis=-1)
        h_glu = jnp.minimum(h_glu, SWIGLU_LIMIT)
        h_lin = jnp.clip(h_lin, -SWIGLU_LIMIT, SWIGLU_LIMIT)
        a = h_glu * jax.nn.sigmoid(SWIGLU_ALPHA * h_glu) * (h_lin + 1.0)
        return (a.astype(xb.dtype) @ w_down[e] + b_down[e]).astype(F32)

    y = lax.map(expert_block, (x_pad.reshape(n_blocks, MOE_BLOCK, d), blk_e)).reshape(n_rows, d)
    out = jnp.zeros((t, d), F32).at[tok_pad].add(y * g_pad[:, None])
    return out.astype(xf.dtype)


def setup_inputs(seed: int = 0) -> dict:
    key = jax.random.key(seed)
    ks = iter(jax.random.split(key, 40))
    nrm = lambda shape, scale: jax.random.normal(next(ks), shape, F32) * scale
    d = D_MODEL
    na, nb = N_S5_LAYERS, N_GLA_LAYERS
    g, p, hg = S5_GROUPS, S5_STATE, S5_GROUP
    n_idx = jnp.arange(p, dtype=F32)
    inp = {}
    inp['x'] = nrm((BATCH, SEQ, d), 1.0)
    inp['c'] = nrm((BATCH, d), 1.0)
    inp['ctx'] = nrm((BATCH, CTX_LEN, d), 1.0)
    inp['c_ctx'] = nrm((d,), 1.0)
    inp['w_ada'] = nrm((DEPTH, d, 6 * d), 0.5 * d ** -0.5)
    inp['b_ada'] = nrm((DEPTH, 6 * d), 0.01)
    inp['ln1_g'] = 1.0 + nrm((DEPTH, d), 0.01)
    inp['ln1_b'] = nrm((DEPTH, d), 0.01)
    inp['ln2_g'] = 1.0 + nrm((DEPTH, d), 0.01)
    inp['ln2_b'] = nrm((DEPTH, d), 0.01)
    inp['s5_lam_re'] = -0.5 + nrm((na, 2, g, p), 0.01)
    inp['s5_lam_im'] = math.pi * n_idx + nrm((na, 2, g, p), 0.01)
    inp['s5_log_step'] = jax.random.uniform(next(ks), (na, 2, g), F32, math.log(S5_DT_MIN), math.log(S5_DT_MAX))
    inp['s5_b_re'] = nrm((na, 2, g, p, hg), (2 * hg) ** -0.5)
    inp['s5_b_im'] = nrm((na, 2, g, p, hg), (2 * hg) ** -0.5)
    inp['s5_c_re'] = nrm((na, 2, g, hg, p), p ** -0.5)
    inp['s5_c_im'] = nrm((na, 2, g, hg, p), p ** -0.5)
    inp['s5_d'] = nrm((na, d), 1.0)
    inp['s5_w_glu'] = jnp.concatenate([nrm((na, d, d), DN_BETA * d ** -0.5), nrm((na, d, d), d ** -0.5)], axis=-1)
    in_w = 2 * GLA_HEADS * GLA_DK + 2 * GLA_HEADS * GLA_DV + 2 * GLA_GATE_RANK
    inp['gla_w_in'] = nrm((nb, d, in_w), d ** -0.5)
    inp['gla_w_a2'] = nrm((nb, 2, GLA_GATE_RANK, GLA_HEADS * GLA_DK), GLA_GATE_RANK ** -0.5)
    inp['gla_b_a2'] = nrm((nb, 2, GLA_HEADS * GLA_DK), 0.1)
    inp['gla_norm_g'] = 1.0 + nrm((nb, GLA_DV), 0.01)
    inp['gla_w_out'] = nrm((nb, d, d), DN_BETA * d ** -0.5)
    inp['moe_w_router'] = nrm((DEPTH, d, N_EXPERTS), d ** -0.5)
    inp['moe_b_router'] = nrm((DEPTH, N_EXPERTS), 0.01)
    inp['moe_w_up'] = nrm((DEPTH, N_EXPERTS, d, 2 * EXPERT_FF), d ** -0.5)
    inp['moe_b_up'] = nrm((DEPTH, N_EXPERTS, 2 * EXPERT_FF), 0.01)
    inp['moe_w_down'] = nrm((DEPTH, N_EXPERTS, EXPERT_FF, d), DN_BETA * EXPERT_FF ** -0.5)
    inp['moe_b_down'] = nrm((DEPTH, N_EXPERTS, d), 0.01)
    return inp


def reference(x, c, ctx, c_ctx, w_ada, b_ada, ln1_g, ln1_b, ln2_g, ln2_b,
              s5_lam_re, s5_lam_im, s5_log_step, s5_b_re, s5_b_im, s5_c_re, s5_c_im, s5_d, s5_w_glu,
              gla_w_in, gla_w_a2, gla_b_a2, gla_norm_g, gla_w_out,
              moe_w_router, moe_b_router, moe_w_up, moe_b_up, moe_w_down, moe_b_down):
    bn, l, d = x.shape
    lc = ctx.shape[1]
    rows = l // GRID_W
    cond = jax.nn.silu(c)
    cond_ctx = jax.nn.silu(c_ctx)[None]
    h, hc = x, ctx
    for i in range(DEPTH):
        keep_ctx = i < DEPTH - 1
        sh1, sc1, g1, sh2, sc2, g2 = [m[:, None, :] for m in jnp.split(cond @ w_ada[i] + b_ada[i], 6, axis=-1)]
        sh1c, sc1c, g1c, sh2c, sc2c, g2c = [m[:, None, :] for m in jnp.split(cond_ctx @ w_ada[i] + b_ada[i], 6, axis=-1)]
        u = _modulate(h, sh1, sc1)
        uc = _modulate(hc, sh1c, sc1c)
        j = i // 2
        if i % 2 == 0:
            y, yc = _s5_mixer(u, uc, s5_lam_re[j], s5_lam_im[j], s5_log_step[j], s5_b_re[j], s5_b_im[j],
                              s5_c_re[j], s5_c_im[j], s5_d[j], s5_w_glu[j], keep_ctx)
        else:
            y, yc = _gla_mixer(u, uc, rows, gla_w_in[j], gla_w_a2[j], gla_b_a2[j], gla_norm_g[j],
                               gla_w_out[j], keep_ctx)
        h = _layer_norm(DN_ALPHA * h + g1 * y, ln1_g[i], ln1_b[i])
        u2 = _modulate(h, sh2, sc2).reshape(bn * l, d)
        if keep_ctx:
            hc = _layer_norm(DN_ALPHA * hc + g1c * yc, ln1_g[i], ln1_b[i])
            u2c = _modulate(hc, sh2c, sc2c).reshape(bn * lc, d)
            tokens = jnp.concatenate([u2, u2c], axis=0)
        else:
            tokens = u2
        f = _moe(tokens, moe_w_router[i], moe_b_router[i], moe_w_up[i], moe_b_up[i], moe_w_down[i], moe_b_down[i])
        h = _layer_norm(DN_ALPHA * h + g2 * f[:bn * l].reshape(bn, l, d), ln2_g[i], ln2_b[i])
        if keep_ctx:
            hc = _layer_norm(DN_ALPHA * hc + g2c * f[bn * l:].reshape(bn, lc, d), ln2_g[i], ln2_b[i])
    return h
```

```python
from contextlib import ExitStack
import math
import numpy as np
import concourse.bass as bass
import concourse.mybir as mybir
from concourse.bass_utils import run_bass_kernel_spmd

F32 = mybir.dt.float32
BF16 = mybir.dt.bfloat16
ALU = mybir.AluOpType
AF = mybir.ActivationFunctionType

D = 1024
NCTX = 256
NLAT = 8192
NSEQ = NCTX + NLAT
MYCTX = 128
MYLAT = 4096
MYTOK = MYCTX + MYLAT
NT_MY = MYTOK // 128
NT_SEQ = NSEQ // 128
NE = 32
DN_ALPHA = 8 ** 0.25
LN_EPS = 1e-5
PI = math.pi

ENGS = ("pe", "act", "dve", "pool", "sp")
DEBUG_SCR = False
NV = 4
NPHYS = 2
NPHASES = 100
S5_NGB = 8
S5_NG8 = 8
S5_STOP = 99
S5_SUB = 99


class Buf:
    __slots__ = ("name", "lw", "rd", "dsem", "psem", "excl")

    def __init__(self, name, excl=False):
        self.name = name
        self.excl = excl
        self.lw = None
        self.rd = []
        self.dsem = None
        self.psem = None


class _Rec:
    def __getattr__(self, name):
        def call(*a, **kw):
            self.rec = (name, a, kw)
            return self
        return call


class K:
    def __init__(self, nc, stack):
        self.nc = nc
        self.gstack = stack
        self.sems = {}
        self.cnt = {}
        for e in ENGS:
            self._mksem("E_" + e)
        self.free_d = []
        self.free_p = []
        self.nd = 0
        self.nbuf = 0
        self._reset()

    def _reset(self):
        self.prog = {e: [] for e in ENGS}
        self.seen = {e: dict(self.cnt) for e in ENGS}
        self.dbufs = []
        self.pbufs_ = []

    def _mksem(self, key):
        self.sems[key] = self.gstack.enter_context(self.nc.semaphore(key))
        self.cnt[key] = 0
        return key

    def buf(self, name=None, excl=False):
        self.nbuf += 1
        return Buf(name or f"b{self.nbuf}", excl)

    def bufs(self, n, name="b", excl=False):
        return [self.buf(f"{name}{i}", excl) for i in range(n)]

    def pbuf(self, name=None):
        return self.buf(name, True)

    def pbufs(self, n, name="p"):
        return self.bufs(n, name, True)

    def phase(self):
        self.pstack = ExitStack()
        return self.pstack

    def sb(self, name, shape, dt=F32, glob=False):
        st = self.gstack if glob else self.pstack
        self.nbuf += 1
        return st.enter_context(self.nc.sbuf_tensor(f"{name}_{self.nbuf}", list(shape), dt))

    def ps(self, name, shape, dt=F32):
        self.nbuf += 1
        esz = 4 if dt == F32 else 2
        n = 1
        for d_ in shape[1:]:
            n *= d_
        per_bank = 2048 // esz
        npad = ((n + per_bank - 1) // per_bank) * per_bank
        tt = self.pstack.enter_context(self.nc.psum_tensor(f"{name}_{self.nbuf}", [128, npad], dt))
        v = tt[0:shape[0], 0:n]
        if len(shape) == 3:
            v = v.rearrange("p (a b) -> p a b", b=shape[2])
        elif len(shape) == 4:
            v = v.rearrange("p (a b c) -> p a b c", b=shape[2], c=shape[3])
        return v

    def _deps(self, eng, reads, writes):
        waits = {}

        def need(ev):
            if ev is not None and waits.get(ev[0], 0) < ev[1]:
                waits[ev[0]] = ev[1]
        for b in reads:
            need(b.lw)
        for b in writes:
            need(b.lw)
            for r in b.rd:
                need(r)
        out = {}
        seen = self.seen[eng]
        for kk, v in waits.items():
            if eng == "pe" and kk == "E_pe":
                continue
            if seen.get(kk, 0) >= v:
                continue
            seen[kk] = v
            out[kk] = v
        return out

    def _commit(self, ev, reads, writes):
        for b in reads:
            b.rd.append(ev)
            if len(b.rd) > 24:
                m = {}
                for kk, v in b.rd:
                    if m.get(kk, 0) < v:
                        m[kk] = v
                b.rd = list(m.items())
        for b in writes:
            b.lw = ev
            b.rd = []

    def op(self, eng, fn, reads=(), writes=()):
        ex = [b for b in reads if b.excl]
        if ex:
            writes = list(writes) + ex
        waits = self._deps(eng, reads, writes)
        key = "E_" + eng
        self.cnt[key] += 1
        ev = (key, self.cnt[key])
        sems = self.sems
        rec = _Rec()
        fn(rec)
        name, a, kw = rec.rec

        def run(e, waits=waits, name=name, a=a, kw=kw, sem=sems[key]):
            for kk, v in waits.items():
                e.wait_ge(sems[kk], v)
            getattr(e, name)(*a, **kw).then_inc(sem, 1)
        self.prog[eng].append(run)
        self._commit(ev, reads, writes)

    def dma(self, eng, out, in_, reads=(), writes=(), **kw):
        waits = self._deps(eng, reads, writes)
        b = writes[0]
        if eng == "pool":
            if getattr(b, "psem", None) is None:
                if self.free_p:
                    b.psem = self.free_p.pop()
                else:
                    self.nd += 1
                    b.psem = self._mksem(f"DP_{self.nd}")
                self.pbufs_.append(b)
            key = b.psem
        else:
            if b.dsem is None:
                if self.free_d:
                    b.dsem = self.free_d.pop()
                else:
                    self.nd += 1
                    b.dsem = self._mksem(f"D_{self.nd}")
                self.dbufs.append(b)
            key = b.dsem
        self.cnt[key] += 16
        ev = (key, self.cnt[key])
        sems = self.sems

        def run(e, waits=waits, sem=sems[key]):
            for kk, v in waits.items():
                e.wait_ge(sems[kk], v)
            e.dma_start(out=out, in_=in_, **kw).then_inc(sem, 16)
        self.prog[eng].append(run)
        self._commit(ev, reads, writes)

    def flush(self):
        final = {kk: v for kk, v in self.cnt.items() if v > 0}
        sems = self.sems

        def bar(e, final=final):
            for kk, v in final.items():
                e.wait_ge(sems[kk], v)
        prog = self.prog
        for eng in ENGS:
            prog[eng].append(bar)
        nc = self.nc
        with nc.Block() as block:
            @block.sync
            def _(e):
                for f in prog["sp"]:
                    f(e)

            @block.tensor
            def _(e):
                for f in prog["pe"]:
                    f(e)

            @block.vector
            def _(e):
                for f in prog["dve"]:
                    f(e)

            @block.scalar
            def _(e):
                for f in prog["act"]:
                    f(e)

            @block.gpsimd
            def _(e):
                for f in prog["pool"]:
                    f(e)
        for b in self.dbufs:
            self.free_d.append(b.dsem)
            b.dsem = None
        for b in self.pbufs_:
            self.free_p.append(b.psem)
            b.psem = None
        self._reset()
        self.pstack.close()


def bc(ap, shape):
    return ap.broadcast_to(list(shape))


class Prog:
    def __init__(self, kind):
        self.kind = kind
        self.nc = bass.Bass("TRN2", target_bir_lowering=False)
        self.t = {}
        self.ext = {}
        self.scr = set()
        self.prefix = ""
        self.sw = 0

    def din(self, name, shape, dt=F32):
        full = self.prefix + name
        if full not in self.ext:
            self.ext[full] = self.nc.dram_tensor(full, list(shape), dt, kind="ExternalInput").ap()
        self.t[name] = self.ext[full]
        return self.t[name]

    def dout(self, name, shape, dt=F32):
        self.t[name] = self.nc.dram_tensor(name, list(shape), dt, kind="ExternalOutput").ap()
        self.ext[name] = self.t[name]
        return self.t[name]

    def dscr(self, name, shape, dt=F32):
        if name in self.t and name in self.scr:
            return self.t[name]
        self.scr.add(name)
        self.t[name] = self.nc.dram_tensor(name, list(shape), dt, kind=("ExternalOutput" if DEBUG_SCR else "Internal")).ap()
        return self.t[name]


def declare_io(P):
    for v in range(NV):
        P.din(f"hseq_{v}", [NSEQ, D])
        P.din(f"cvec_{v}", [2, D])
        P.dout(f"hout_{v}", [MYTOK, D])


def declare_common(P):
    P.din("w_ada", [D, 6 * D])
    P.din("b_ada", [1, 6 * D])
    for n in ("ln1_g", "ln1_b", "ln2_g", "ln2_b"):
        P.din(n, [1, D])
    P.din("w_router", [D, NE])
    P.din("b_router", [1, NE])
    if NPHASES >= 7:
        P.din("w_up", [NE, D, 2 * D])
        P.din("b_up", [NE, 2 * D])
        P.din("w_down", [NE, D, D])
        P.din("b_down", [NE, D])
    _pf, P.prefix = P.prefix, ""
    P.din("ident", [128, 128])
    P.prefix = _pf
    P.dscr("MOD", [2, 6 * D])
    P.dscr("H1", [MYTOK, D])
    P.dscr("U2T", [D, MYTOK], BF16)
    P.dscr("FS", [MYTOK, D])


def my_rows(t):
    if t == 0:
        return 0, 128
    return NCTX + (t - 1) * 128, NCTX + t * 128


def phase_adaln(k, P):
    t = P.t
    k.phase()
    cT = k.sb("cT", [128, 8, 2]); b_cT = k.buf()
    for r in range(2):
        k.dma("sp", cT[:, :, r], t["cvec"][r].rearrange("(kc p) -> p kc", p=128), writes=[b_cT],
              allow_slow_non_contiguous=True)
    sT = k.sb("sT", [128, 8, 2]); b_sT = k.buf()
    k.op("act", lambda e: e.activation(sT[:], cT[:], AF.Silu), reads=[b_cT], writes=[b_sT])
    wbuf = [k.sb(f"wada{i}", [128, 8, 512]) for i in range(2)]
    b_w = k.bufs(2, "wada")
    bb = [k.sb(f"bada{i}", [2, 512]) for i in range(2)]
    b_bb = k.bufs(2, "bada")
    pm = [k.ps(f"pmod{i}", [2, 512]) for i in range(2)]
    b_pm = k.pbufs(2, "pmod")
    res = [k.sb(f"rmod{i}", [2, 512]) for i in range(2)]
    b_res = k.bufs(2, "rmod")
    b_mod = k.buf("MODscr")
    for j in range(12):
        i = j % 2
        k.dma("sp", wbuf[i][:], t["w_ada"][:, j * 512:(j + 1) * 512].rearrange("(kc p) n -> p kc n", p=128),
              writes=[b_w[i]])
        k.dma("act", bb[i][:], bc(t["b_ada"][0:1, j * 512:(j + 1) * 512], [2, 512]), writes=[b_bb[i]])
        for kc in range(8):
            k.op("pe", lambda e, i=i, kc=kc: e.matmul(pm[i][:], lhsT=sT[:, kc, :], rhs=wbuf[i][:, kc, :],
                                                      start=(kc == 0), stop=(kc == 7)),
                 reads=[b_sT, b_w[i]], writes=[b_pm[i]])
        add1 = 1.0 if j in (2, 3, 8, 9) else 0.0
        k.op("dve", lambda e, i=i, add1=add1: e.scalar_tensor_tensor(
            res[i][:], pm[i][:], add1, bb[i][:], ALU.add, ALU.add),
            reads=[b_pm[i], b_bb[i]], writes=[b_res[i]])
        k.dma("sp", t["MOD"][:, j * 512:(j + 1) * 512], res[i][:], reads=[b_res[i]], writes=[b_mod])
    k.flush()


MOD_SH1, MOD_SC1, MOD_G1, MOD_SH2, MOD_SC2, MOD_G2 = range(6)


def mod_bc(P, r, which, n=128):
    return bc(P.t["MOD"][r:r + 1, which * D:(which + 1) * D], [n, D])


def phase_modulate(k, P, uscr):
    t = P.t
    k.phase()
    A = [k.sb(f"mA{r}", [128, D]) for r in range(2)]
    B = [k.sb(f"mB{r}", [128, D]) for r in range(2)]
    b_ab = k.buf()
    for r in range(2):
        k.dma("sp", A[r][:], mod_bc(P, r, MOD_SC1), writes=[b_ab])
        k.dma("sp", B[r][:], mod_bc(P, r, MOD_SH1), writes=[b_ab])
    NB = 3
    hb = [k.sb(f"mh{i}", [128, D]) for i in range(NB)]; b_h = k.bufs(NB, "mh")
    tb = [k.sb(f"mt{i}", [128, D]) for i in range(NB)]; b_t = k.bufs(NB, "mt")
    ub = [k.sb(f"mu{i}", [128, D], BF16) for i in range(NB)]; b_u = k.bufs(NB, "mu")
    b_us = k.buf("uscr")
    for tt in range(NT_SEQ):
        i = tt % NB
        r = 1 if tt < 2 else 0
        k.dma("sp", hb[i][:], t["hseq"][tt * 128:(tt + 1) * 128, :], writes=[b_h[i]])
        k.op("pool", lambda e, i=i, r=r: e.tensor_tensor(tb[i][:], hb[i][:], A[r][:], ALU.mult),
             reads=[b_h[i], b_ab], writes=[b_t[i]])
        k.op("dve", lambda e, i=i, r=r: e.tensor_tensor(ub[i][:], tb[i][:], B[r][:], ALU.add),
             reads=[b_t[i], b_ab], writes=[b_u[i]])
        k.dma("act", uscr[tt * 128:(tt + 1) * 128, :], ub[i][:], reads=[b_u[i]], writes=[b_us])
    k.flush()


def emit_ln(k, x, b_x, out, b_out, gB, bB, b_gb, wk):
    st, b_st = wk["st"], wk["b_st"]
    mv, b_mv = wk["mv"], wk["b_mv"]
    rs, b_rs = wk["rs"], wk["b_rs"]
    xn, b_xn = wk["xn"], wk["b_xn"]
    for c in range(2):
        k.op("dve", lambda e, c=c: e.bn_stats(st[:, c, :], x[:, c * 512:(c + 1) * 512]),
             reads=[b_x], writes=[b_st])
    k.op("dve", lambda e: e.bn_aggr(mv[:], st[:]), reads=[b_st], writes=[b_mv])
    k.op("act", lambda e: e.activation(rs[:], mv[:, 1:2], AF.Sqrt, bias=wk["eps"][:], scale=1.0),
         reads=[b_mv, wk["b_eps"]], writes=[b_rs])
    k.op("dve", lambda e: e.reciprocal(rs[:], rs[:]), reads=[b_rs], writes=[b_rs])
    k.op("dve", lambda e: e.tensor_scalar(xn[:], x[:], mv[:, 0:1], rs[:], ALU.subtract, ALU.mult),
         reads=[b_x, b_mv, b_rs], writes=[b_xn])
    k.op("pool", lambda e: e.tensor_tensor(xn[:], xn[:], gB[:], ALU.mult), reads=[b_xn, b_gb], writes=[b_xn])
    k.op("dve", lambda e: e.tensor_tensor(out[:], xn[:], bB[:], ALU.add), reads=[b_xn, b_gb], writes=[b_out])


def ln_work(k, pfx):
    eps = k.sb(pfx + "eps", [128, 1]); b_eps = k.buf()
    k.op("dve", lambda e: e.memset(eps[:], LN_EPS), writes=[b_eps])
    return dict(eps=eps, b_eps=b_eps, st=k.sb(pfx + "st", [128, 2, 6]), b_st=k.buf(), mv=k.sb(pfx + "mv", [128, 2]), b_mv=k.buf(),
                rs=k.sb(pfx + "rs", [128, 1]), b_rs=k.buf(), xn=k.sb(pfx + "xn", [128, D]), b_xn=k.buf())


class PostMixer:
    def __init__(self, k, P, G, b_G):
        self.k, self.P, self.G, self.b_G = k, P, G, b_G
        t = P.t
        self.b_c = k.buf("pmconst")
        self.g1B = [k.sb(f"g1B{r}", [128, D]) for r in range(2)]
        for r in range(2):
            k.dma("sp", self.g1B[r][:], mod_bc(P, r, MOD_G1), writes=[self.b_c])
        self.lgB = k.sb("ln1gB", [128, D]); self.lbB = k.sb("ln1bB", [128, D])
        k.dma("sp", self.lgB[:], bc(t["ln1_g"], [128, D]), writes=[self.b_c])
        k.dma("sp", self.lbB[:], bc(t["ln1_b"], [128, D]), writes=[self.b_c])
        self.sc2c = k.sb("sc2c", [128, 2, 8]); self.sh2c = k.sb("sh2c", [128, 2, 8])
        for r in range(2):
            k.dma("sp", self.sc2c[:, r, :], t["MOD"][r, MOD_SC2 * D:(MOD_SC2 + 1) * D].rearrange("(kc p) -> p kc", p=128),
                  writes=[self.b_c], allow_slow_non_contiguous=True)
            k.dma("sp", self.sh2c[:, r, :], t["MOD"][r, MOD_SH2 * D:(MOD_SH2 + 1) * D].rearrange("(kc p) -> p kc", p=128),
                  writes=[self.b_c], allow_slow_non_contiguous=True)
        self.wr = k.sb("wr32", [128, 8, NE])
        k.dma("sp", self.wr[:], t["w_router"].rearrange("(kc p) n -> p kc n", p=128), writes=[self.b_c])
        self.brB = k.sb("brB", [128, NE])
        k.dma("sp", self.brB[:], bc(t["b_router"], [128, NE]), writes=[self.b_c])
        self.idt = k.sb("pm_ident", [128, 128])
        k.dma("sp", self.idt[:], t["ident"], writes=[self.b_c])
        NB = 2
        self.NB = NB
        self.h = [k.sb(f"pmh{i}", [128, D]) for i in range(NB)]; self.b_h = k.bufs(NB, "pmh")
        self.t1 = [k.sb(f"pmt{i}", [128, D]) for i in range(NB)]; self.b_t1 = k.bufs(NB, "pmt")
        self.h1 = [k.sb(f"pmh1{i}", [128, D]) for i in range(NB)]; self.b_h1 = k.bufs(NB, "pmh1")
        self.lnw = [ln_work(k, f"pml{i}") for i in range(NB)]
        _pT = k.ps("pmpT", [128, 8, 128]); _bpT = k.pbuf("pmpT")
        self.pT = [_pT] * NB; self.b_pT = [_bpT] * NB
        self.u32 = [k.sb(f"pmu32{i}", [128, 8, 128]) for i in range(NB)]; self.b_u32 = k.bufs(NB, "pmu32")
        self.ubf = [k.sb(f"pmubf{i}", [128, 8, 128], BF16) for i in range(NB)]; self.b_ubf = k.bufs(NB, "pmubf")
        _pl = k.ps("pmpl", [128, NE]); _bpl = k.pbuf("pmpl")
        self.pl = [_pl] * NB; self.b_pl = [_bpl] * NB
        self.lg = [k.sb(f"pmlg{i}", [128, NE]) for i in range(NB)]; self.b_lg = k.bufs(NB, "pmlg")
        self.m8 = [k.sb(f"pmm8{i}", [128, 8]) for i in range(NB)]; self.b_m8 = k.bufs(NB, "pmm8")
        self.ex = [k.sb(f"pmex{i}", [128, NE]) for i in range(NB)]; self.b_ex = k.bufs(NB, "pmex")
        self.mk = [k.sb(f"pmmk{i}", [128, NE]) for i in range(NB)]; self.b_mk = k.bufs(NB, "pmmk")
        self.sm = [k.sb(f"pmsm{i}", [128, 2]) for i in range(NB)]; self.b_sm = k.bufs(NB, "pmsm")
        self.b_h1s = k.buf("H1scr")
        self.b_u2s = k.buf("U2Tscr")

    def prefetch(self, tt):
        k, t = self.k, self.P.t
        i = tt % self.NB
        r0, r1 = my_rows(tt)
        k.dma("sp", self.h[i][:], t["hseq"][r0:r1, :], writes=[self.b_h[i]])

    def emit(self, tt, ymix, b_ymix):
        k, t = self.k, self.P.t
        i = tt % self.NB
        r = 1 if tt == 0 else 0
        h, t1, h1 = self.h[i], self.t1[i], self.h1[i]
        k.op("pool", lambda e: e.tensor_tensor(t1[:], ymix, self.g1B[r][:], ALU.mult),
             reads=[b_ymix, self.b_c], writes=[self.b_t1[i]])
        k.op("dve", lambda e: e.scalar_tensor_tensor(t1[:], h[:], DN_ALPHA, t1[:], ALU.mult, ALU.add),
             reads=[self.b_h[i], self.b_t1[i]], writes=[self.b_t1[i]])
        emit_ln(k, t1, self.b_t1[i], h1, self.b_h1[i], self.lgB, self.lbB, self.b_c, self.lnw[i])
        k.dma("act", t["H1"][tt * 128:(tt + 1) * 128, :], h1[:], reads=[self.b_h1[i]], writes=[self.b_h1s])
        pT, u32, ubf = self.pT[i], self.u32[i], self.ubf[i]
        for kc in range(8):
            k.op("pe", lambda e, kc=kc: e.transpose(pT[:, kc, :], h1[:, kc * 128:(kc + 1) * 128], self.idt[:]),
                 reads=[self.b_h1[i], self.b_c], writes=[self.b_pT[i]])
        k.op("dve", lambda e: e.tensor_tensor(u32[:], pT[:], bc(self.sc2c[:, r, :].unsqueeze(2), [128, 8, 128]),
                                              ALU.mult), reads=[self.b_pT[i], self.b_c], writes=[self.b_u32[i]])
        k.op("pool", lambda e: e.tensor_tensor(u32[:], u32[:], bc(self.sh2c[:, r, :].unsqueeze(2), [128, 8, 128]),
                                               ALU.add), reads=[self.b_u32[i], self.b_c], writes=[self.b_u32[i]])
        k.op("act", lambda e: e.copy(ubf[:], u32[:]), reads=[self.b_u32[i]], writes=[self.b_ubf[i]])
        k.dma("act", t["U2T"][:, tt * 128:(tt + 1) * 128].rearrange("(kc p) n -> p kc n", p=128), ubf[:],
              reads=[self.b_ubf[i]], writes=[self.b_u2s])
        pl, lg, m8, ex, mk, sm = self.pl[i], self.lg[i], self.m8[i], self.ex[i], self.mk[i], self.sm[i]
        for kc in range(8):
            k.op("pe", lambda e, kc=kc: e.matmul(pl[:], lhsT=u32[:, kc, :], rhs=self.wr[:, kc, :],
                                                 start=(kc == 0), stop=(kc == 7)),
                 reads=[self.b_u32[i], self.b_c], writes=[self.b_pl[i]])
        k.op("dve", lambda e: e.tensor_tensor(lg[:], pl[:], self.brB[:], ALU.add),
             reads=[self.b_pl[i], self.b_c], writes=[self.b_lg[i]])
        k.op("dve", lambda e: e.max(out=m8[:], in_=lg[:]), reads=[self.b_lg[i]], writes=[self.b_m8[i]])
        k.op("dve", lambda e: e.tensor_scalar(mk[:], lg[:], m8[:, 3:4], None, ALU.is_ge),
             reads=[self.b_lg[i], self.b_m8[i]], writes=[self.b_mk[i]])
        k.op("dve", lambda e: e.tensor_scalar(ex[:], lg[:], m8[:, 0:1], None, ALU.subtract),
             reads=[self.b_lg[i], self.b_m8[i]], writes=[self.b_ex[i]])
        k.op("act", lambda e: e.activation(ex[:], ex[:], AF.Exp), reads=[self.b_ex[i]], writes=[self.b_ex[i]])
        k.op("dve", lambda e: e.tensor_tensor(ex[:], ex[:], mk[:], ALU.mult),
             reads=[self.b_ex[i], self.b_mk[i]], writes=[self.b_ex[i]])
        k.op("dve", lambda e: e.reduce_sum(sm[:, 0:1], ex[:], axis=mybir.AxisListType.X),
             reads=[self.b_ex[i]], writes=[self.b_sm[i]])
        k.op("dve", lambda e: e.reciprocal(sm[:, 1:2], sm[:, 0:1]), reads=[self.b_sm[i]], writes=[self.b_sm[i]])
        k.op("dve", lambda e: e.tensor_scalar(self.G[:, tt, :], ex[:], sm[:, 1:2], None, ALU.mult),
             reads=[self.b_ex[i], self.b_sm[i]], writes=[self.b_G])


def phase_moe(k, P, G, b_G, supertiles):
    t = P.t
    k.phase()
    b_c = k.buf("moeconst")
    idt = k.sb("moe_ident", [128, 128])
    k.dma("sp", idt[:], t["ident"], writes=[b_c])
    bup = k.sb("bup", [128, NE, 16])
    for e4 in range(0, NE, 8):
        k.dma("sp", bup[:, e4:e4 + 8, :], t["b_up"][e4:e4 + 8, :].rearrange("e (fc p) -> p e fc", p=128),
              writes=[b_c], allow_slow_non_contiguous=True)
    bdn = k.sb("bdn", [NE, D])
    k.dma("sp", bdn[:], t["b_down"], writes=[b_c])
    NW = 2
    wu = [k.sb(f"wu{i}", [128, 8, 1024], BF16) for i in range(NW)]; b_wu = k.bufs(NW, "wu")
    wd = [k.sb(f"wd{i}", [128, 4, 1024], BF16) for i in range(NW)]; b_wd = k.bufs(NW, "wd")
    maxtok = max(n for _, n in supertiles) * 128
    maxtl = max(n for _, n in supertiles)
    uT = k.sb("moe_uT", [128, 8, maxtok], BF16); b_uT = k.buf("moe_uT")
    acc = k.sb("moe_acc", [128, maxtl, D]); b_acc = [k.buf(f"acc{i}") for i in range(maxtl)]
    phg = [k.ps(f"phg{i}", [128, 512]) for i in range(2)]; b_phg = k.pbufs(2, "phg")
    phl = [k.ps(f"phl{i}", [128, 512]) for i in range(2)]; b_phl = k.pbufs(2, "phl")
    py = [k.ps(f"py{i}", [128, 1024]) for i in range(2)]; b_py = k.pbufs(2, "py")
    NA = 2
    g1 = [k.sb(f"mg1{i}", [128, 512]) for i in range(NA)]; b_g1 = k.bufs(NA, "mg1")
    sg = [k.sb(f"msg{i}", [128, 512]) for i in range(NA)]; b_sg = k.bufs(NA, "msg")
    l1 = [k.sb(f"ml1{i}", [128, 512]) for i in range(NA)]; b_l1 = k.bufs(NA, "ml1")
    aT = [k.sb(f"maT{i}", [128, 4, 512], BF16) for i in range(2)]; b_aT = k.bufs(2, "maT")
    gt = k.sb("moe_gT", [NE, 128]); b_gt = k.buf("moe_gT")
    b_fs = k.buf("FSscr")

    units = [(e, fh) for e in range(NE) for fh in range(2)]

    def load_unit(ui, slot):
        e, fh = units[ui]
        k.dma("pool", wu[slot][:, :, 0:512],
              t["w_up"][e, :, fh * 512:(fh + 1) * 512].rearrange("(kc p) f -> p kc f", p=128), writes=[b_wu[slot]])
        k.dma("pool", wu[slot][:, :, 512:1024],
              t["w_up"][e, :, 1024 + fh * 512:1024 + (fh + 1) * 512].rearrange("(kc p) f -> p kc f", p=128),
              writes=[b_wu[slot]])
        k.dma("pool", wd[slot][:], t["w_down"][e, fh * 512:(fh + 1) * 512, :].rearrange("(fc p) d -> p fc d", p=128),
              writes=[b_wd[slot]])

    cnt_act = 0
    cnt_chunk = 0
    cnt_y = 0
    for (t0, ntl) in supertiles:
        ntok = ntl * 128
        k.dma("sp", uT[:, :, 0:ntok], t["U2T"][:, t0 * 128:t0 * 128 + ntok].rearrange("(kc p) n -> p kc n", p=128),
              writes=[b_uT])
        for tl in range(ntl):
            pT = py[cnt_y % 2]; bpT = b_py[cnt_y % 2]; cnt_y += 1
            k.op("pe", lambda e, tl=tl, pT=pT: e.transpose(pT[0:NE, 0:128], G[:, t0 + tl, :], idt[:]),
                 reads=[b_G, b_c], writes=[bpT])
            k.op("act", lambda e, pT=pT: e.copy(gt[:], pT[0:NE, 0:128]), reads=[bpT], writes=[b_gt])
            for hf in range(2):
                k.op("pe", lambda e, hf=hf, pT=pT: e.matmul(pT[:, hf * 512:(hf + 1) * 512], lhsT=gt[:],
                                                            rhs=bdn[:, hf * 512:(hf + 1) * 512], start=True, stop=True),
                     reads=[b_gt, b_c, bpT], writes=[bpT])
            k.op("act", lambda e, tl=tl, pT=pT: e.copy(acc[:, tl, :], pT[:]), reads=[bpT], writes=[b_acc[tl]])
        load_unit(0, 0)
        for ui, (ex, fh) in enumerate(units):
            slot = ui % NW
            if ui + 1 < len(units):
                load_unit(ui + 1, (ui + 1) % NW)
            chunks = [(c0, min(512, ntok - c0)) for c0 in range(0, ntok, 512)]
            for (c0, n) in chunks:
                ab = cnt_chunk % 2; cnt_chunk += 1
                for fc in range(4):
                    pb = cnt_act % 2
                    ai = cnt_act % NA
                    cnt_act += 1
                    for kc in range(8):
                        k.op("pe", lambda e, kc=kc, fc=fc, pb=pb, c0=c0, n=n: e.matmul(
                            phg[pb][:, 0:n], lhsT=wu[slot][:, kc, fc * 128:(fc + 1) * 128],
                            rhs=uT[:, kc, c0:c0 + n], start=(kc == 0), stop=(kc == 7)),
                            reads=[b_wu[slot], b_uT], writes=[b_phg[pb]])
                    for kc in range(8):
                        k.op("pe", lambda e, kc=kc, fc=fc, pb=pb, c0=c0, n=n: e.matmul(
                            phl[pb][:, 0:n], lhsT=wu[slot][:, kc, 512 + fc * 128:512 + (fc + 1) * 128],
                            rhs=uT[:, kc, c0:c0 + n], start=(kc == 0), stop=(kc == 7)),
                            reads=[b_wu[slot], b_uT], writes=[b_phl[pb]])
                    fcg = fh * 4 + fc
                    k.op("dve", lambda e, pb=pb, ai=ai, n=n, fcg=fcg, ex=ex: e.tensor_scalar(
                        g1[ai][:, 0:n], phg[pb][:, 0:n], bup[:, ex, fcg:fcg + 1], 7.0, ALU.add, ALU.min),
                        reads=[b_phg[pb], b_c], writes=[b_g1[ai]])
                    k.op("dve", lambda e, pb=pb, ai=ai, n=n, fcg=fcg, ex=ex: e.tensor_scalar(
                        l1[ai][:, 0:n], phl[pb][:, 0:n], bup[:, ex, 8 + fcg:9 + fcg], 7.0, ALU.add, ALU.min),
                        reads=[b_phl[pb], b_c], writes=[b_l1[ai]])
                    k.op("act", lambda e, ai=ai, n=n: e.activation(sg[ai][:, 0:n], g1[ai][:, 0:n], AF.Sigmoid,
                                                                   scale=1.702),
                         reads=[b_g1[ai]], writes=[b_sg[ai]])
                    k.op("pool", lambda e, ai=ai, n=n: e.tensor_scalar(l1[ai][:, 0:n], l1[ai][:, 0:n], -7.0, 1.0,
                                                                       ALU.max, ALU.add),
                         reads=[b_l1[ai]], writes=[b_l1[ai]])
                    k.op("pool", lambda e, ai=ai, n=n: e.tensor_tensor(sg[ai][:, 0:n], sg[ai][:, 0:n], g1[ai][:, 0:n],
                                                                       ALU.mult),
                         reads=[b_sg[ai], b_g1[ai]], writes=[b_sg[ai]])
                    k.op("dve", lambda e, ai=ai, n=n, ab=ab, fc=fc: e.tensor_tensor(
                        aT[ab][:, fc, 0:n], sg[ai][:, 0:n], l1[ai][:, 0:n], ALU.mult),
                        reads=[b_sg[ai], b_l1[ai]], writes=[b_aT[ab]])
                for j in range(n // 128):
                    tl = (c0 // 128) + j
                    yb = cnt_y % 2; cnt_y += 1
                    for hf in range(2):
                        for fc in range(4):
                            k.op("pe", lambda e, hf=hf, fc=fc, yb=yb, j=j, ab=ab: e.matmul(
                                py[yb][:, hf * 512:(hf + 1) * 512], lhsT=aT[ab][:, fc, j * 128:(j + 1) * 128],
                                rhs=wd[slot][:, fc, hf * 512:(hf + 1) * 512], start=(fc == 0), stop=(fc == 3)),
                                reads=[b_aT[ab], b_wd[slot]], writes=[b_py[yb]])
                    k.op("dve", lambda e, yb=yb, tl=tl, ex=ex: e.scalar_tensor_tensor(
                        acc[:, tl, :], py[yb][:], G[:, t0 + tl, ex:ex + 1], acc[:, tl, :], ALU.mult, ALU.add),
                        reads=[b_py[yb], b_G, b_acc[tl]], writes=[b_acc[tl]])
        for tl in range(ntl):
            k.dma("sp", t["FS"][(t0 + tl) * 128:(t0 + tl + 1) * 128, :], acc[:, tl, :], reads=[b_acc[tl]],
                  writes=[b_fs])
    k.flush()


def phase_post_moe(k, P):
    t = P.t
    k.phase()
    b_c = k.buf("t2const")
    g2B = [k.sb(f"g2B{r}", [128, D]) for r in range(2)]
    for r in range(2):
        k.dma("sp", g2B[r][:], mod_bc(P, r, MOD_G2), writes=[b_c])
    lgB = k.sb("ln2gB", [128, D]); lbB = k.sb("ln2bB", [128, D])
    k.dma("sp", lgB[:], bc(t["ln2_g"], [128, D]), writes=[b_c])
    k.dma("sp", lbB[:], bc(t["ln2_b"], [128, D]), writes=[b_c])
    NB = 2
    f = [k.sb(f"t2f{i}", [128, D]) for i in range(NB)]; b_f = k.bufs(NB, "t2f")
    h1 = [k.sb(f"t2h{i}", [128, D]) for i in range(NB)]; b_h1 = k.bufs(NB, "t2h")
    o = [k.sb(f"t2o{i}", [128, D]) for i in range(NB)]; b_o = k.bufs(NB, "t2o")
    lnw = [ln_work(k, f"t2l{i}") for i in range(NB)]
    b_out = k.buf("hout")
    for tt in range(NT_MY):
        i = tt % NB
        r = 1 if tt == 0 else 0
        k.dma("sp", f[i][:], t["FS"][tt * 128:(tt + 1) * 128, :], writes=[b_f[i]])
        k.dma("act", h1[i][:], t["H1"][tt * 128:(tt + 1) * 128, :], writes=[b_h1[i]])
        k.op("pool", lambda e, i=i, r=r: e.tensor_tensor(f[i][:], f[i][:], g2B[r][:], ALU.mult),
             reads=[b_f[i], b_c], writes=[b_f[i]])
        k.op("dve", lambda e, i=i: e.scalar_tensor_tensor(f[i][:], h1[i][:], DN_ALPHA, f[i][:], ALU.mult, ALU.add),
             reads=[b_h1[i], b_f[i]], writes=[b_f[i]])
        emit_ln(k, f[i], b_f[i], o[i], b_o[i], lgB, lbB, b_c, lnw[i])
        k.dma("sp", t["hout"][tt * 128:(tt + 1) * 128, :], o[i][:], reads=[b_o[i]], writes=[b_out])
    k.flush()


SUPERTILES = [(0, 17), (17, 16)]


NCH = NSEQ // 8
NCC = NCTX // 8
NPH = NCH + NCC
NF = NCC + MYLAT // 8
NB_ = NCH
HS_F = 10
HS_B = 11


def declare_s5(P):
    P.din("lam_re", [2, 64, 64]); P.din("lam_im", [2, 64, 64]); P.din("log_step", [2, 64])
    P.din("b_re", [2, 64, 64, 16]); P.din("b_im", [2, 64, 64, 16])
    P.din("c_re", [2, 64, 16, 64]); P.din("c_im", [2, 64, 16, 64])
    P.din("s5_d", [1, D]); P.din("w_glu", [D, 2 * D])
    _pf, P.prefix = P.prefix, ""
    P.din("maskF", [128, 128]); P.din("maskB", [128, 128]); P.din("swapm", [128, 128])
    P.prefix = _pf
    P.dscr("US", [NSEQ, D], BF16)
    P.dscr("YS", [MYTOK, D], BF16)
    P.dscr("S5P", [2, 128, 64 * 128], BF16)
    P.dscr("S5Q", [2, 128, 64 * 128], BF16)
    P.dscr("S5HS", [2, 3, 128, 64 * 11])


def s5_consts(k, P):
    t = P.t
    b_c = k.buf("s5const")
    idt = k.sb("s5_ident", [128, 128]); k.dma("sp", idt[:], t["ident"], writes=[b_c])
    idb = k.sb("s5_identb", [128, 128], BF16); k.dma("pool", idb[:], t["ident"], writes=[b_c])
    swb = k.sb("s5_swapb", [128, 128], BF16); k.dma("pool", swb[:], t["swapm"], writes=[b_c])
    mask = [k.sb("s5_mF", [128, 128]), k.sb("s5_mB", [128, 128])]
    k.dma("sp", mask[0][:], t["maskF"], writes=[b_c]); k.dma("sp", mask[1][:], t["maskB"], writes=[b_c])
    sgn = k.sb("s5_sgn", [128, 1])
    k.op("dve", lambda e: e.memset(sgn[0:64, :], -1.0), writes=[b_c])
    k.op("dve", lambda e: e.memset(sgn[64:128, :], 1.0), writes=[b_c])
    npi = k.sb("s5_npi", [128, 1])
    k.op("dve", lambda e: e.memset(npi[:], -PI), writes=[b_c])
    dcol = k.sb("s5_dcol", [128, 64])
    for tau in range(8):
        k.dma("sp", dcol[tau * 16:(tau + 1) * 16, :], t["s5_d"][0, :].rearrange("(g h) -> h g", h=16),
              writes=[b_c], allow_slow_non_contiguous=True)
    return b_c, idt, idb, swb, mask, sgn, npi, dcol


def phase_s5_setup(k, P, d_out):
    t = P.t
    k.phase()
    d = d_out
    pd = d_out ^ P.sw
    b_c, idt, idb, swb, mask, sgn, npi, dcol = s5_consts(k, P)
    Pbf = {d: k.sb(f"s5_P{d}", [128, 64, 128], BF16)}
    Qbf = {d: k.sb(f"s5_Q{d}", [128, 64, 128], BF16)}
    hsR = {d: k.sb(f"s5_hsR{d}", [128, 64, 11])}
    hsI = {d: k.sb(f"s5_hsI{d}", [128, 64, 11])}
    hsN = {d: k.sb(f"s5_hsN{d}", [128, 64, 11])}
    b_tab = k.buf("s5tab")

    ld = k.sb("s5_ld", [64, 128]); b_ld = k.buf()
    pst = k.ps("s5_pst", [128, 128]); b_pst = k.pbuf()
    cnt = [0]

    def tmp(shape):
        cnt[0] += 1
        return k.sb(f"s5_t{cnt[0]}", shape)

    b_s = k.buf("s5setup")

    def dv(fn):
        k.op("dve", fn, reads=[b_s, b_c], writes=[b_s])

    def cmul(orr, oi, xr, xi, yr, yi, t1, t2):
        dv(lambda e: e.tensor_tensor(t1, xr, yr, ALU.mult))
        dv(lambda e: e.tensor_tensor(t2, xi, yi, ALU.mult))
        dv(lambda e: e.tensor_tensor(t2, t1, t2, ALU.subtract))
        dv(lambda e: e.tensor_tensor(t1, xr, yi, ALU.mult))
        dv(lambda e: e.tensor_tensor(oi, xi, yr, ALU.mult))
        dv(lambda e: e.tensor_tensor(oi, oi, t1, ALU.add))
        dv(lambda e: e.tensor_copy(orr, t2))

    if True:
        lr, li, dt = tmp([128, 64]), tmp([128, 64]), tmp([128, 64])
        for (dst, src) in ((lr, "lam_re"), (li, "lam_im")):
            for hf in range(2):
                k.dma("sp", ld[:, hf * 64:(hf + 1) * 64], t[src][pd], writes=[b_ld])
            k.op("pe", lambda e: e.transpose(pst[:, 0:64], ld[:], idt[0:64, 0:64]), reads=[b_ld, b_c], writes=[b_pst])
            k.op("dve", lambda e, dst=dst: e.tensor_copy(dst[:], pst[:, 0:64]), reads=[b_pst, b_s], writes=[b_s])
        k.dma("sp", dt[:], bc(t["log_step"][pd:pd + 1, :], [128, 64]), writes=[b_s])
        hx, hp = tmp([128, 64]), tmp([128, 64])

        def horner(dst, x, coef):
            dv(lambda e: e.tensor_scalar(dst[:], x[:], float(coef[-1]), float(coef[-2]), ALU.mult, ALU.add))
            for cfv in coef[-3::-1]:
                dv(lambda e: e.tensor_tensor(dst[:], dst[:], x[:], ALU.mult))
                dv(lambda e, cfv=cfv: e.tensor_scalar(dst[:], dst[:], float(cfv), None, ALU.add))

        expc = [1.0 / math.factorial(i) for i in range(11)]
        dv(lambda e: e.tensor_scalar(hx[:], dt[:], 0.125, None, ALU.mult))
        horner(dt, hx, expc)
        for _ in range(3):
            dv(lambda e: e.tensor_tensor(dt[:], dt[:], dt[:], ALU.mult))
        mag, ang, cs, sn = tmp([128, 64]), tmp([128, 64]), tmp([128, 64]), tmp([128, 64])
        dv(lambda e: e.tensor_tensor(hx[:], lr[:], dt[:], ALU.mult))
        horner(mag, hx, expc)
        dv(lambda e: e.tensor_tensor(ang[:], li[:], dt[:], ALU.mult))
        qf, qi = tmp([128, 64]), k.sb("s5_qi", [128, 64], mybir.dt.int32)
        dv(lambda e: e.tensor_scalar(qf[:], ang[:], 1.0 / (2 * PI), None, ALU.mult))
        dv(lambda e: e.tensor_copy(qi[:], qf[:]))
        dv(lambda e: e.tensor_copy(qf[:], qi[:]))
        dv(lambda e: e.scalar_tensor_tensor(ang[:], qf[:], -2 * PI, ang[:], ALU.mult, ALU.add))
        dv(lambda e: e.tensor_scalar(qf[:], ang[:], PI, 2 * PI, ALU.is_gt, ALU.mult))
        dv(lambda e: e.tensor_tensor(ang[:], ang[:], qf[:], ALU.subtract))
        dv(lambda e: e.tensor_scalar(qf[:], ang[:], -PI, 2 * PI, ALU.is_lt, ALU.mult))
        dv(lambda e: e.tensor_tensor(ang[:], ang[:], qf[:], ALU.add))
        dv(lambda e: e.tensor_scalar(ang[:], ang[:], 0.5, None, ALU.mult))
        dv(lambda e: e.tensor_tensor(hx[:], ang[:], ang[:], ALU.mult))
        sinc = [(-1.0) ** i / math.factorial(2 * i + 1) for i in range(7)]
        cosc = [(-1.0) ** i / math.factorial(2 * i) for i in range(7)]
        horner(hp, hx, sinc)
        dv(lambda e: e.tensor_tensor(hp[:], hp[:], ang[:], ALU.mult))
        horner(qf, hx, cosc)
        dv(lambda e: e.scalar_tensor_tensor(sn[:], hp[:], 2.0, qf[:], ALU.mult, ALU.mult))
        dv(lambda e: e.tensor_tensor(cs[:], hp[:], hp[:], ALU.mult))
        dv(lambda e: e.tensor_scalar(cs[:], cs[:], -2.0, 1.0, ALU.mult, ALU.add))
        ar, ai = tmp([128, 64]), tmp([128, 64])
        dv(lambda e: e.tensor_tensor(ar[:], mag[:], cs[:], ALU.mult))
        dv(lambda e: e.tensor_tensor(ai[:], mag[:], sn[:], ALU.mult))
        m2, vr, vi = tmp([128, 64]), tmp([128, 64]), tmp([128, 64])
        dv(lambda e: e.tensor_tensor(m2[:], mag[:], mag[:], ALU.mult))
        dv(lambda e: e.reciprocal(m2[:], m2[:]))
        dv(lambda e: e.tensor_tensor(vr[:], ar[:], m2[:], ALU.mult))
        dv(lambda e: e.scalar_tensor_tensor(vi[:], ai[:], -1.0, m2[:], ALU.mult, ALU.mult))
        den, nr, kr, ki, t1, t2 = (tmp([128, 64]) for _ in range(6))
        dv(lambda e: e.tensor_tensor(den[:], lr[:], lr[:], ALU.mult))
        dv(lambda e: e.tensor_tensor(t1[:], li[:], li[:], ALU.mult))
        dv(lambda e: e.tensor_tensor(den[:], den[:], t1[:], ALU.add))
        dv(lambda e: e.reciprocal(den[:], den[:]))
        dv(lambda e: e.tensor_scalar(nr[:], ar[:], -1.0, None, ALU.add))
        dv(lambda e: e.tensor_tensor(kr[:], nr[:], lr[:], ALU.mult))
        dv(lambda e: e.tensor_tensor(t1[:], ai[:], li[:], ALU.mult))
        dv(lambda e: e.tensor_tensor(kr[:], kr[:], t1[:], ALU.add))
        dv(lambda e: e.tensor_tensor(kr[:], kr[:], den[:], ALU.mult))
        dv(lambda e: e.tensor_tensor(ki[:], ai[:], lr[:], ALU.mult))
        dv(lambda e: e.tensor_tensor(t1[:], nr[:], li[:], ALU.mult))
        dv(lambda e: e.tensor_tensor(ki[:], ki[:], t1[:], ALU.subtract))
        dv(lambda e: e.tensor_tensor(ki[:], ki[:], den[:], ALU.mult))

        def powtab(br_, bi_, desc):
            R, I = tmp([128, 64, 8]), tmp([128, 64, 8])
            s2r, s2i, s4r, s4i = (tmp([128, 64]) for _ in range(4))
            ta, tb = tmp([128, 64, 4]), tmp([128, 64, 4])
            cmul(s2r[:], s2i[:], br_[:], bi_[:], br_[:], bi_[:], ta[:, :, 0], tb[:, :, 0])
            cmul(s4r[:], s4i[:], s2r[:], s2i[:], s2r[:], s2i[:], ta[:, :, 0], tb[:, :, 0])
            i0, i1 = (7, 6) if desc else (0, 1)
            dv(lambda e: e.memset(R[:, :, i0:i0 + 1], 1.0))
            dv(lambda e: e.memset(I[:, :, i0:i0 + 1], 0.0))
            dv(lambda e: e.tensor_copy(R[:, :, i1], br_[:]))
            dv(lambda e: e.tensor_copy(I[:, :, i1], bi_[:]))
            if desc:
                src2, dst2, src4, dst4 = slice(6, 8), slice(4, 6), slice(4, 8), slice(0, 4)
            else:
                src2, dst2, src4, dst4 = slice(0, 2), slice(2, 4), slice(0, 4), slice(4, 8)
            cmul(R[:, :, dst2], I[:, :, dst2], R[:, :, src2], I[:, :, src2],
                 bc(s2r[:].unsqueeze(2), [128, 64, 2]), bc(s2i[:].unsqueeze(2), [128, 64, 2]),
                 ta[:, :, 0:2], tb[:, :, 0:2])
            cmul(R[:, :, dst4], I[:, :, dst4], R[:, :, src4], I[:, :, src4],
                 bc(s4r[:].unsqueeze(2), [128, 64, 4]), bc(s4i[:].unsqueeze(2), [128, 64, 4]),
                 ta[:], tb[:])
            return R, I, s4r, s4i

        desc = (d == 0)
        PR, PI_, a4r, a4i = powtab(ar, ai, desc)
        QR, QI, _, _ = powtab(vr, vi, desc)
        t1w, t2w = tmp([128, 64]), tmp([128, 64])
        cmul(hsR[d][:, :, 0], hsI[d][:, :, 0], a4r[:], a4i[:], a4r[:], a4i[:], t1w[:], t2w[:])
        for lv in range(1, 11):
            cmul(hsR[d][:, :, lv], hsI[d][:, :, lv], hsR[d][:, :, lv - 1], hsI[d][:, :, lv - 1],
                 hsR[d][:, :, lv - 1], hsI[d][:, :, lv - 1], t1w[:], t2w[:])
        dv(lambda e, d=d: e.tensor_scalar(hsI[d][:], hsI[d][:], sgn[:], None, ALU.mult))
        dv(lambda e, d=d: e.tensor_scalar(hsN[d][:], hsI[d][:], -1.0, None, ALU.mult))
        br_, bi_ = tmp([128, 64, 16]), tmp([128, 64, 16])
        for (dst, src) in ((br_, "b_re"), (bi_, "b_im")):
            for hf in range(2):
                k.dma("sp", dst[hf * 64:(hf + 1) * 64], t[src][pd].rearrange("g p h -> p g h"), writes=[b_s])
        bbr, bbi, tq = tmp([128, 64, 16]), tmp([128, 64, 16]), tmp([128, 64, 16])
        krb = bc(kr[:].unsqueeze(2), [128, 64, 16]); kib = bc(ki[:].unsqueeze(2), [128, 64, 16])
        dv(lambda e: e.tensor_tensor(bbr[:], br_[:], krb, ALU.mult))
        dv(lambda e: e.tensor_tensor(tq[:], bi_[:], kib, ALU.mult))
        dv(lambda e: e.tensor_tensor(bbr[:], bbr[:], tq[:], ALU.subtract))
        dv(lambda e: e.tensor_tensor(bbi[:], bi_[:], krb, ALU.mult))
        dv(lambda e: e.tensor_tensor(tq[:], br_[:], kib, ALU.mult))
        dv(lambda e: e.tensor_tensor(bbi[:], bbi[:], tq[:], ALU.add))
        BB1, BB2 = tmp([128, 64, 16]), tmp([128, 64, 16])
        dv(lambda e: e.tensor_copy(BB1[0:64], bbr[0:64]))
        dv(lambda e: e.tensor_copy(BB1[64:128], bbi[64:128]))
        dv(lambda e: e.tensor_copy(BB2[0:64], bbi[0:64]))
        dv(lambda e: e.tensor_copy(BB2[64:128], bbr[64:128]))
        cr, ci = tmp([128, 64, 16]), tmp([128, 64, 16])
        ldc = tmp([128, 128])
        for (dst, src) in ((cr, "c_re"), (ci, "c_im")):
            cv = t[src][pd].rearrange("g h p -> (g h) p")
            for blk in range(8):
                for hf in range(2):
                    k.dma("sp", ldc[:, hf * 64:(hf + 1) * 64], cv[blk * 128:(blk + 1) * 128, :],
                          reads=[b_s], writes=[b_s])
                k.op("pe", lambda e: e.transpose(pst[:], ldc[:], idt[:]), reads=[b_s, b_c], writes=[b_pst])
                k.op("dve", lambda e, dst=dst, blk=blk: e.tensor_copy(
                    dst[:, blk * 8:(blk + 1) * 8, :], pst[:].rearrange("p (g h) -> p g h", h=16)),
                    reads=[b_pst, b_s], writes=[b_s])
        CC1, CC2 = tmp([128, 64, 16]), tmp([128, 64, 16])
        dv(lambda e: e.tensor_copy(CC1[0:64], cr[0:64]))
        dv(lambda e: e.tensor_scalar(CC1[64:128], ci[64:128], -1.0, None, ALU.mult))
        dv(lambda e: e.tensor_scalar(CC2[0:64], ci[0:64], -1.0, None, ALU.mult))
        dv(lambda e: e.tensor_scalar(CC2[64:128], cr[64:128], -1.0, None, ALU.mult))
        PIs = tmp([128, 64, 8])
        dv(lambda e: e.tensor_scalar(PIs[:], PI_[:], sgn[:], None, ALU.mult))
        w1, w2 = tmp([128, 16, 8, 16]), tmp([128, 16, 8, 16])
        for (dstT, XR, XI, M1, M2) in ((Pbf[d], PR, PIs, BB1, BB2), (Qbf[d], QR, QI, CC1, CC2)):
            for g0 in range(0, 64, 16):
                gs = slice(g0, g0 + 16)
                xr = bc(XR[:, gs, :].unsqueeze(3), [128, 16, 8, 16])
                xi = bc(XI[:, gs, :].unsqueeze(3), [128, 16, 8, 16])
                m1 = bc(M1[:, gs, :].unsqueeze(2), [128, 16, 8, 16])
                m2_ = bc(M2[:, gs, :].unsqueeze(2), [128, 16, 8, 16])
                dv(lambda e, xr=xr, m1=m1: e.tensor_tensor(w1[:], xr, m1, ALU.mult))
                dv(lambda e, xi=xi, m2_=m2_: e.tensor_tensor(w2[:], xi, m2_, ALU.mult))
                k.op("dve", lambda e, dstT=dstT, gs=gs: e.tensor_tensor(
                    dstT[:, gs, :].rearrange("p g (t h) -> p g t h", h=16), w1[:], w2[:], ALU.add),
                    reads=[b_s], writes=[b_s, b_tab])

    b_o = k.buf("s5tabscr")
    k.dma("sp", t["S5P"][d], Pbf[d][:].rearrange("p g f -> p (g f)"), reads=[b_s, b_tab], writes=[b_o])
    k.dma("sp", t["S5Q"][d], Qbf[d][:].rearrange("p g f -> p (g f)"), reads=[b_s, b_tab], writes=[b_o])
    for j, tb_ in enumerate((hsR[d], hsI[d], hsN[d])):
        k.dma("sp", t["S5HS"][d, j], tb_[:].rearrange("p g f -> p (g f)"), reads=[b_s, b_tab], writes=[b_o])
    k.flush()


def phase_s5(k, P):
    t = P.t
    k.phase()
    b_c, idt, idb, swb, mask, sgn, npi, dcol = s5_consts(k, P)
    b_tab = k.buf("s5tab")
    Pbf = [k.sb(f"s5_P{d}", [128, 64, 128], BF16) for d in range(2)]
    Qbf = [k.sb(f"s5_Q{d}", [128, 64, 128], BF16) for d in range(2)]
    hsR = [k.sb(f"s5_hsR{d}", [128, 64, 11]) for d in range(2)]
    hsI = [k.sb(f"s5_hsI{d}", [128, 64, 11]) for d in range(2)]
    hsN = [k.sb(f"s5_hsN{d}", [128, 64, 11]) for d in range(2)]
    for d in range(2):
        k.dma("sp", Pbf[d][:].rearrange("p g f -> p (g f)"), t["S5P"][d], writes=[b_tab])
        k.dma("sp", Qbf[d][:].rearrange("p g f -> p (g f)"), t["S5Q"][d], writes=[b_tab])
        for j, tb_ in enumerate((hsR[d], hsI[d], hsN[d])):
            k.dma("sp", tb_[:].rearrange("p g f -> p (g f)"), t["S5HS"][d, j], writes=[b_tab])
    UIN = k.sb("s5_uin", [128, 9, 8, 128], BF16); b_uin = k.buf("s5_uin")
    UG = [k.sb(f"s5_ug{i}", [128, 9, 128], BF16) for i in range(2)]; b_UG = k.bufs(2, "s5_ug")
    UT = [k.sb(f"s5_UT{i}", [128, NPH], BF16) for i in range(2)]; b_UT = k.bufs(2, "s5_UT")
    ptr = [k.ps(f"s5_ptr{i}", [128, 4, 128], BF16) for i in range(2)]; b_ptr = k.pbufs(2, "s5_ptr")
    pgen = k.ps("s5_pgen", [128, 3, 128]); b_pgen = k.pbuf("s5_pgen")
    PT = [k.sb(f"s5_PT{d}", [128, 128], BF16) for d in range(2)]; b_PT = k.bufs(2, "s5_PT")
    PTs = [k.sb(f"s5_PTs{d}", [128, 128], BF16) for d in range(2)]; b_PTs = k.bufs(2, "s5_PTs")
    TM = [k.sb(f"s5_TM{d}", [128, 128], BF16) for d in range(2)]; b_TM = k.bufs(2, "s5_TM")
    tmk = k.sb("s5_tmk", [128, 128]); b_tmk = k.buf()
    px = [k.ps(f"s5_px{i}", [128, 512]) for i in range(2)]; b_px = k.pbufs(2, "s5_px")
    X32 = k.sb("s5_X32", [128, NCH]); b_X32 = k.buf()
    SA = k.sb("s5_SA", [128, NCH]); SB_ = k.sb("s5_SB", [128, NCH]); b_SA, b_SB = k.buf(), k.buf()
    WA = k.sb("s5_WA", [128, NCH]); WB = k.sb("s5_WB", [128, NCH]); b_WA, b_WB = k.buf(), k.buf()
    tS = k.sb("s5_tS", [128, NCH]); tW = k.sb("s5_tW", [128, NCH]); b_tS, b_tW = k.buf(), k.buf()
    tW2 = k.sb("s5_tW2", [128, NCH]); b_tW2 = k.buf()
    SP = [k.sb(f"s5_SP{d}", [128, NCH], BF16) for d in range(2)]; b_SP = k.bufs(2, "s5_SP")
    pyo = k.ps("s5_pyo", [128, 512]); b_pyo = k.pbuf()
    pyc = k.ps("s5_pyc", [128, 16]); b_pyc = k.pbuf()
    ybf = k.sb("s5_ybf", [128, 528], BF16); b_ybf = k.buf()
    pyt = k.ps("s5_pyt", [128, 5, 128], BF16); b_pyt = k.pbuf()
    YO = [k.sb(f"s5_YO{i}", [128, 4, 8, 128], BF16) for i in range(2)]; b_YO = k.bufs(2, "s5_YO")
    YOC = [k.sb(f"s5_YOC{i}", [16, 8, 128], BF16) for i in range(2)]; b_YOC = k.bufs(2, "s5_YOC")
    b_ys = k.buf("YSscr")
    US = t["US"]
    gi = 0
    for gb in range(S5_NGB):
        cs_ = slice(gb * 128, (gb + 1) * 128)
        for blk in range(8):
            r0 = NCTX + blk * 1024
            k.dma("sp", UIN[:, blk], US[r0:r0 + 1024, cs_].rearrange("(c t) ch -> c t ch", t=8), writes=[b_uin])
        k.dma("sp", UIN[0:32, 8], US[0:NCTX, cs_].rearrange("(c t) ch -> c t ch", t=8), writes=[b_uin])
        yo, yoc = YO[gb % 2], YOC[gb % 2]
        for g8 in range(S5_NG8):
            g = gb * 8 + g8
            ut, b_ut = UT[gi % 2], b_UT[gi % 2]
            gi += 1
            hs_ = slice(g8 * 16, (g8 + 1) * 16)
            ug, b_ug = UG[g % 2], b_UG[g % 2]
            k.op("pool", lambda e: e.tensor_copy(ug[:, 0:8, :].rearrange("p a (t h) -> p a t h", h=16),
                                                 UIN[:, 0:8, :, hs_]), reads=[b_uin], writes=[b_ug])
            k.op("pool", lambda e: e.tensor_copy(ug[0:32, 8, :].rearrange("p (t h) -> p t h", h=16),
                                                 UIN[0:32, 8, :, hs_]), reads=[b_uin], writes=[b_ug])
            for q in range(2):
                pp, b_pp = ptr[q], b_ptr[q]
                for j in range(4):
                    blk = q * 4 + j
                    k.op("pe", lambda e, pp=pp, j=j, blk=blk, hs_=hs_: e.transpose(
                        pp[:, j, :], ug[:, blk, :], idb[:]), reads=[b_ug, b_c], writes=[b_pp])
                eng = "act" if q == 0 else "dve"
                if eng == "act":
                    k.op("act", lambda e, pp=pp, q=q, ut=ut: e.copy(
                        ut[:, 32 + q * 512:32 + (q + 1) * 512], pp[:].rearrange("p a b -> p (a b)")),
                        reads=[b_pp], writes=[b_ut])
                else:
                    k.op("dve", lambda e, pp=pp, q=q, ut=ut: e.tensor_copy(
                        ut[:, 32 + q * 512:32 + (q + 1) * 512], pp[:].rearrange("p a b -> p (a b)")),
                        reads=[b_pp], writes=[b_ut])
            pp, b_pp = ptr[0], b_ptr[0]
            k.op("pe", lambda e, pp=pp, hs_=hs_: e.transpose(pp[:, 0, 0:32], ug[0:32, 8, :], idb[0:32, 0:32]),
                 reads=[b_ug, b_c], writes=[b_pp])
            k.op("act", lambda e, pp=pp, ut=ut: e.copy(ut[:, 0:32], pp[:, 0, 0:32]), reads=[b_pp], writes=[b_ut])
            k.op("dve", lambda e, pp=pp, ut=ut: e.tensor_copy(ut[:, NCH:NPH], pp[:, 0, 0:32]), reads=[b_pp],
                 writes=[b_ut])
            if S5_STOP <= 1:
                continue
            for d in range(2):
                c0 = 0 if d == 0 else NCC
                n = NF if d == 0 else NB_
                nlv = HS_F if d == 0 else HS_B
                k.op("pe", lambda e, d=d, g=g: e.matmul(pgen[:, 0, :], lhsT=Pbf[d][:, g, :], rhs=idb[:],
                                                         start=True, stop=True),
                     reads=[b_tab, b_c], writes=[b_pgen])
                k.op("pe", lambda e, d=d, g=g: e.matmul(pgen[:, 1, :], lhsT=Pbf[d][:, g, :], rhs=swb[:],
                                                         start=True, stop=True),
                     reads=[b_tab, b_c], writes=[b_pgen])
                k.op("pe", lambda e, d=d, g=g: e.matmul(pgen[:, 2, :], lhsT=Pbf[d][:, g, :], rhs=Qbf[d][:, g, :],
                                                         start=True, stop=True),
                     reads=[b_tab], writes=[b_pgen])
                if S5_SUB <= 1:
                    continue
                k.op("act", lambda e, d=d: e.copy(PT[d][:], pgen[:, 0, :]), reads=[b_pgen], writes=[b_PT[d]])
                k.op("act", lambda e, d=d: e.copy(PTs[d][:], pgen[:, 1, :]), reads=[b_pgen], writes=[b_PTs[d]])
                if S5_SUB <= 2:
                    continue
                k.op("dve", lambda e, d=d: e.tensor_tensor(tmk[:], pgen[:, 2, :], mask[d][:], ALU.mult),
                     reads=[b_pgen, b_c], writes=[b_tmk])
                if S5_SUB <= 3:
                    continue
                if d == 0:
                    k.op("dve", lambda e, d=d, g=g: e.scalar_tensor_tensor(
                        TM[d][:], idt[:], dcol[:, g:g + 1], tmk[:], ALU.mult, ALU.add),
                        reads=[b_tmk, b_c], writes=[b_TM[d]])
                else:
                    k.op("dve", lambda e, d=d: e.tensor_copy(TM[d][:], tmk[:]), reads=[b_tmk], writes=[b_TM[d]])
                if S5_STOP <= 2:
                    continue
                cols = [(a, min(512, n - a)) for a in range(0, n, 512)]
                for wi, (lhs, b_lhs) in enumerate(((PT[d], b_PT[d]), (PTs[d], b_PTs[d]))):
                    for ci_, (a, w) in enumerate(cols):
                        pb = (wi * len(cols) + ci_) % 2
                        k.op("pe", lambda e, lhs=lhs, a=a, w=w, pb=pb, c0=c0, ut=ut: e.matmul(
                            px[pb][:, 0:w], lhsT=lhs[:], rhs=ut[:, c0 + a:c0 + a + w], start=True, stop=True),
                            reads=[b_lhs, b_ut], writes=[b_px[pb]])
                        if wi == 0:
                            k.op("act", lambda e, a=a, w=w, pb=pb: e.copy(SA[:, a:a + w], px[pb][:, 0:w]),
                                 reads=[b_px[pb]], writes=[b_SA])
                            k.op("dve", lambda e, a=a, w=w, pb=pb: e.tensor_copy(X32[:, a:a + w], px[pb][:, 0:w]),
                                 reads=[b_px[pb]], writes=[b_X32])
                        else:
                            k.op("act", lambda e, a=a, w=w, pb=pb: e.copy(WA[:, a:a + w], px[pb][:, 0:w]),
                                 reads=[b_px[pb]], writes=[b_WA])
                if S5_STOP <= 3:
                    continue
                cur = (SA, b_SA, WA, b_WA)
                nxt = (SB_, b_SB, WB, b_WB)
                for lv in range(nlv):
                    sh = 1 << lv
                    if sh >= n:
                        break
                    So, bSo, Wo, bWo = cur
                    Sn, bSn, Wn, bWn = nxt
                    if d == 0:
                        dst, src, keep = slice(sh, n), slice(0, n - sh), slice(0, sh)
                    else:
                        dst, src, keep = slice(0, n - sh), slice(sh, n), slice(n - sh, n)
                    c1 = hsR[d][:, g, lv:lv + 1]; c2 = hsI[d][:, g, lv:lv + 1]; c2n = hsN[d][:, g, lv:lv + 1]
                    k.op("dve", lambda e, So=So, c1=c1, src=src, dst=dst: e.scalar_tensor_tensor(
                        tS[:, dst], So[:, src], c1, So[:, dst], ALU.mult, ALU.add),
                        reads=[bSo, b_tab], writes=[b_tS])
                    k.op("dve", lambda e, Wo=Wo, Sn=Sn, c2=c2, src=src, dst=dst: e.scalar_tensor_tensor(
                        Sn[:, dst], Wo[:, src], c2, tS[:, dst], ALU.mult, ALU.add),
                        reads=[bWo, b_tS, b_tab], writes=[bSn])
                    k.op("act", lambda e, So=So, Sn=Sn, keep=keep: e.copy(Sn[:, keep], So[:, keep]),
                         reads=[bSo], writes=[bSn])
                    k.op("pool", lambda e, Wo=Wo, c1=c1, src=src, dst=dst: e.tensor_scalar(
                        tW[:, dst], Wo[:, src], c1, None, ALU.mult), reads=[bWo, b_tab], writes=[b_tW])
                    k.op("pool", lambda e, Wo=Wo, dst=dst: e.tensor_tensor(
                        tW[:, dst], tW[:, dst], Wo[:, dst], ALU.add), reads=[bWo, b_tW], writes=[b_tW])
                    k.op("pool", lambda e, So=So, c2n=c2n, src=src, dst=dst: e.tensor_scalar(
                        tW2[:, dst], So[:, src], c2n, None, ALU.mult), reads=[bSo, b_tab], writes=[b_tW2])
                    k.op("pool", lambda e, Wn=Wn, dst=dst: e.tensor_tensor(
                        Wn[:, dst], tW[:, dst], tW2[:, dst], ALU.add), reads=[b_tW, b_tW2], writes=[bWn])
                    k.op("act", lambda e, Wo=Wo, Wn=Wn, keep=keep: e.copy(Wn[:, keep], Wo[:, keep]),
                         reads=[bWo], writes=[bWn])
                    cur, nxt = nxt, cur
                Sf, bSf = cur[0], cur[1]
                k.op("dve", lambda e, Sf=Sf, d=d, n=n: e.tensor_tensor(SP[d][:, 0:n], Sf[:, 0:n], X32[:, 0:n],
                                                                          ALU.subtract),
                     reads=[bSf, b_X32], writes=[b_SP[d]])
            if S5_STOP <= 4:
                continue
            seq = [(TM[0], b_TM[0], ut, b_ut, 32), (Qbf[0][:, g, :], b_tab, SP[0], b_SP[0], 32),
                   (TM[1], b_TM[1], ut, b_ut, 32), (Qbf[1][:, g, :], b_tab, SP[1], b_SP[1], 0)]
            for si, (lhs, b_lhs, rhs, b_rhs, off) in enumerate(seq):
                lh = lhs[:] if si % 2 == 0 else lhs
                k.op("pe", lambda e, lh=lh, rhs=rhs, off=off, si=si: e.matmul(
                    pyo[:], lhsT=lh, rhs=rhs[:, off:off + 512], start=(si == 0), stop=(si == 3)),
                    reads=[b_lhs, b_rhs], writes=[b_pyo])
            seqc = [(TM[0], b_TM[0], ut, b_ut, 0), (Qbf[0][:, g, :], b_tab, SP[0], b_SP[0], 0),
                    (TM[1], b_TM[1], ut, b_ut, NCH), (Qbf[1][:, g, :], b_tab, SP[1], b_SP[1], NCH - NCC)]
            for si, (lhs, b_lhs, rhs, b_rhs, off) in enumerate(seqc):
                lh = lhs[:] if si % 2 == 0 else lhs
                k.op("pe", lambda e, lh=lh, rhs=rhs, off=off, si=si: e.matmul(
                    pyc[:], lhsT=lh, rhs=rhs[:, off:off + 16], start=(si == 0), stop=(si == 3)),
                    reads=[b_lhs, b_rhs], writes=[b_pyc])
            if S5_STOP <= 5:
                continue
            k.op("act", lambda e: e.copy(ybf[:, 0:512], pyo[:]), reads=[b_pyo], writes=[b_ybf])
            k.op("act", lambda e: e.copy(ybf[:, 512:528], pyc[:]), reads=[b_pyc], writes=[b_ybf])
            for j in range(4):
                k.op("pe", lambda e, j=j: e.transpose(pyt[:, j, :], ybf[:, j * 128:(j + 1) * 128], idb[:]),
                     reads=[b_ybf, b_c], writes=[b_pyt])
            k.op("pe", lambda e: e.transpose(pyt[0:16, 4, :], ybf[:, 512:528], idb[:]),
                 reads=[b_ybf, b_c], writes=[b_pyt])
            k.op("dve", lambda e, yo=yo, hs_=hs_: e.tensor_copy(
                yo[:, :, :, hs_], pyt[:, 0:4, :].rearrange("p a (t h) -> p a t h", h=16)),
                reads=[b_pyt], writes=[b_YO[gb % 2]])
            k.op("act", lambda e, yoc=yoc, hs_=hs_: e.copy(
                yoc[:, :, hs_], pyt[0:16, 4, :].rearrange("p (t h) -> p t h", h=16)),
                reads=[b_pyt], writes=[b_YOC[gb % 2]])
        for a_ in range(4):
            k.dma("act", t["YS"][MYCTX + a_ * 1024:MYCTX + (a_ + 1) * 1024, cs_].rearrange("(c t) ch -> c t ch", t=8),
                  yo[:, a_], reads=[b_YO[gb % 2]], writes=[b_ys])
        k.dma("act", t["YS"][0:MYCTX, cs_].rearrange("(c t) ch -> c t ch", t=8), yoc[:],
              reads=[b_YOC[gb % 2]], writes=[b_ys])
    k.flush()


def phase_s5_post(k, P, G, b_G):
    t = P.t
    k.phase()
    pm = PostMixer(k, P, G, b_G)
    b_c = k.buf("s5pconst")
    wg = k.sb("wglu", [128, 8, 2 * D], BF16)
    for kc in range(8):
        k.dma("pool", wg[:, kc, :], t["w_glu"][kc * 128:(kc + 1) * 128, :], writes=[b_c])
    idb = k.sb("s5p_identb", [128, 128], BF16); k.dma("pool", idb[:], t["ident"], writes=[b_c])
    NB = 2
    y = [k.sb(f"gy{i}", [128, D], BF16) for i in range(NB)]; b_y = k.bufs(NB, "gy")
    a = [k.sb(f"ga{i}", [128, D]) for i in range(NB)]; b_a = k.bufs(NB, "ga")
    s = [k.sb(f"gs{i}", [128, D]) for i in range(NB)]; b_s = k.bufs(NB, "gs")
    gl = [k.sb(f"gg{i}", [128, D], BF16) for i in range(NB)]; b_gl = k.bufs(NB, "gg")
    _pT = k.ps("gpT", [128, 8, 128], BF16); _bpT = k.pbuf("gpT")
    pT = [_pT] * NB; b_pT = [_bpT] * NB
    gT = [k.sb(f"ggT{i}", [128, 8, 128], BF16) for i in range(NB)]; b_gT = k.bufs(NB, "ggT")
    pz = k.ps("gpz", [128, 2 * D]); b_pz = k.pbuf("gpz")
    sg = [k.sb(f"gsg{i}", [128, D]) for i in range(NB)]; b_sg = k.bufs(NB, "gsg")
    ym = [k.sb(f"gym{i}", [128, D]) for i in range(NB)]; b_ym = k.bufs(NB, "gym")
    for tt in range(NT_MY):
        i = tt % NB
        pm.prefetch(tt)
        k.dma("sp", y[i][:], t["YS"][tt * 128:(tt + 1) * 128, :], writes=[b_y[i]])
        k.op("pool", lambda e, i=i: e.tensor_tensor(a[i][:], y[i][:], y[i][:], ALU.mult), reads=[b_y[i]],
             writes=[b_a[i]])
        k.op("dve", lambda e, i=i: e.tensor_scalar(a[i][:], a[i][:], 0.044715, 1.0, ALU.mult, ALU.add),
             reads=[b_a[i]], writes=[b_a[i]])
        k.op("pool", lambda e, i=i: e.tensor_tensor(a[i][:], a[i][:], y[i][:], ALU.mult), reads=[b_a[i], b_y[i]],
             writes=[b_a[i]])
        k.op("act", lambda e, i=i: e.activation(s[i][:], a[i][:], AF.Sigmoid, scale=1.5957691216),
             reads=[b_a[i]], writes=[b_s[i]])
        k.op("dve", lambda e, i=i: e.tensor_tensor(gl[i][:], s[i][:], y[i][:], ALU.mult), reads=[b_s[i], b_y[i]],
             writes=[b_gl[i]])
        for kc in range(8):
            k.op("pe", lambda e, i=i, kc=kc: e.transpose(pT[i][:, kc, :], gl[i][:, kc * 128:(kc + 1) * 128], idb[:]),
                 reads=[b_gl[i], b_c], writes=[b_pT[i]])
        k.op("act", lambda e, i=i: e.copy(gT[i][:], pT[i][:]), reads=[b_pT[i]], writes=[b_gT[i]])
        for nb in range(4):
            for kc in range(8):
                k.op("pe", lambda e, i=i, kc=kc, nb=nb: e.matmul(
                    pz[:, nb * 512:(nb + 1) * 512], lhsT=gT[i][:, kc, :], rhs=wg[:, kc, nb * 512:(nb + 1) * 512],
                    start=(kc == 0), stop=(kc == 7)), reads=[b_gT[i], b_c], writes=[b_pz])
        k.op("act", lambda e, i=i: e.activation(sg[i][:], pz[:, D:2 * D], AF.Sigmoid), reads=[b_pz],
             writes=[b_sg[i]])
        k.op("dve", lambda e, i=i: e.tensor_tensor(ym[i][:], pz[:, 0:D], sg[i][:], ALU.mult),
             reads=[b_pz, b_sg[i]], writes=[b_ym[i]])
        pm.emit(tt, ym[i][:], b_ym[i])
    k.flush()


def emit_layer(P, kind):
    with ExitStack() as st:
        k = K(P.nc, st)
        G = k.sb("Gall", [128, NT_MY, NE], glob=True); b_G = k.buf("Gall")
        for v in range(NV):
            P.sw = v % 2
            for nm in ("hseq", "cvec", "hout"):
                P.t[nm] = P.t[f"{nm}_{v}"]
            if kind == "s5":
                phases = [lambda: phase_adaln(k, P), lambda: phase_modulate(k, P, P.t["US"]),
                          lambda: phase_s5_setup(k, P, 0), lambda: phase_s5_setup(k, P, 1), lambda: phase_s5(k, P),
                          lambda: phase_s5_post(k, P, G, b_G), lambda: phase_moe(k, P, G, b_G, SUPERTILES),
                          lambda: phase_post_moe(k, P)]
            else:
                phases = [lambda: phase_adaln(k, P), lambda: phase_modulate(k, P, P.t["US"]),
                          lambda: phase_gla_pass(k, P, 0), lambda: phase_gla_pass(k, P, 1),
                          lambda: phase_gla_post(k, P, G, b_G), lambda: phase_moe(k, P, G, b_G, SUPERTILES),
                          lambda: phase_post_moe(k, P)]
            for ph in phases[:NPHASES]:
                ph()


def build_s5_layer():
    P = Prog("s5")
    declare_io(P)
    declare_common(P)
    declare_s5(P)
    emit_layer(P, "s5")
    return P


NCK = NSEQ // 128
DK = 128
DV = 256
NH = 4


def declare_gla(P):
    P.din("w_in", [D, 3104]); P.din("w_a2", [2, 16, 512]); P.din("b_a2", [2, 512])
    P.din("norm_g", [1, DV]); P.din("w_out", [D, D])
    _pf, P.prefix = P.prefix, ""
    P.din("triF", [128, 128]); P.din("triFs", [128, 128]); P.din("triB", [128, 128]); P.din("triBs", [128, 128])
    P.din("flipm", [128, 128])
    P.prefix = _pf
    P.dscr("US", [NSEQ, D], BF16)
    P.dscr("OS", [2, MYTOK, D])


def chunk_rows(c):
    return c * 128


def phase_gla_pass(k, P, d):
    t = P.t
    k.phase()
    pd = d ^ P.sw
    b_c = k.buf("glaconst"); b_cp = k.buf("glaconstp")
    idb = k.sb("gl_identb", [128, 128], BF16); k.dma("pool", idb[:], t["ident"], writes=[b_cp])
    tri = k.sb("gl_tri", [128, 128]); tris = k.sb("gl_tris", [128, 128])
    k.dma("sp", tri[:], t["triF" if d == 0 else "triB"], writes=[b_c])
    k.dma("sp", tris[:], t["triFs" if d == 0 else "triBs"], writes=[b_c])
    ones = k.sb("gl_ones", [128, 128])
    k.op("dve", lambda e: e.memset(ones[:], 1.0), writes=[b_c])
    win = k.sb("gl_win", [128, 8, 2064], BF16)
    for kc in range(8):
        k.dma("pool", win[:, kc, 0:2048], t["w_in"][kc * 128:(kc + 1) * 128, 0:2048], writes=[b_cp])
        k.dma("pool", win[:, kc, 2048:2064], t["w_in"][kc * 128:(kc + 1) * 128, 3072 + 16 * pd:3088 + 16 * pd],
              writes=[b_cp])
    wa2 = k.sb("gl_wa2", [16, 512]); k.dma("sp", wa2[:], t["w_a2"][pd], writes=[b_c])
    ba2 = k.sb("gl_ba2", [1, 512]); k.dma("sp", ba2[:], t["b_a2"][pd:pd + 1, :], writes=[b_c])
    S32 = k.sb("gl_S32", [128, NH, DV]); b_S32 = k.bufs(NH, "gl_S32")
    Sbf = k.sb("gl_Sbf", [128, NH, DV], BF16); b_Sbf = k.bufs(NH, "gl_Sbf")
    for hd in range(NH):
        k.op("dve", lambda e, hd=hd: e.memset(S32[:, hd, :], 0.0), writes=[b_S32[hd]])
        k.op("pool", lambda e, hd=hd: e.memset(Sbf[:, hd, :], 0.0), writes=[b_Sbf[hd]])
    NB = 2
    u = [k.sb(f"gl_u{i}", [128, D], BF16) for i in range(NB)]; b_u = k.bufs(NB, "gl_u")
    uT = [k.sb(f"gl_uT{i}", [128, 8, 128], BF16) for i in range(NB)]; b_uT = k.bufs(NB, "gl_uT")
    aT = k.sb("gl_aT", [16, 128]); b_aT = k.buf()
    ez = k.sb("gl_ez", [128, 512]); b_ez = k.buf()
    la = k.sb("gl_la", [128, 512]); b_la = k.buf()
    E1 = k.sb("gl_E1", [128, NH, 128]); b_E1 = k.buf()
    E2 = k.sb("gl_E2", [128, NH, 128]); b_E2 = k.buf()
    EK = k.sb("gl_EK", [128, 512]); b_EK = k.buf()
    qd = k.sb("gl_qd", [128, NH, 128], BF16); b_qd = k.buf()
    kd = k.sb("gl_kd", [128, NH, 128], BF16); b_kd = k.buf()
    ke = k.sb("gl_ke", [128, 512], BF16); b_ke = k.buf()
    v = k.sb("gl_v", [128, D], BF16); b_v = k.buf()
    attm = [k.sb(f"gl_attm{i}", [128, 128], BF16) for i in range(2)]; b_attm = k.bufs(2, "gl_attm")
    osb = [k.sb(f"gl_osb{i}", [128, D]) for i in range(2)]; b_osb = k.bufs(2, "gl_osb")
    pA = k.ps("gl_pA", [128, 8, 128], BF16); b_pA = k.pbuf("gl_pA")
    pM = k.ps("gl_pM", [128, 512]); b_pM = k.pbuf("gl_pM")
    pbT = k.ps("gl_pbT", [128, NH, 128]); b_pbT = k.pbuf("gl_pbT")
    pE = k.ps("gl_pE", [128, 512]); b_pE = k.pbuf("gl_pE")
    pq = k.ps("gl_pq", [128, NH, 128]); b_pq = k.pbuf("gl_pq")
    pk = k.ps("gl_pk", [128, NH, 128]); b_pk = k.pbuf("gl_pk")
    pat = pM; b_pat = b_pM
    pV = k.ps("gl_pV", [128, D]); b_pV = k.pbuf("gl_pV")
    b_os = k.buf("OSscr")
    if d == 0:
        order = list(range(0, 2 + 32))
    else:
        order = [1, 0] + list(range(NCK - 1, 1, -1))
    ecol = 127 if d == 0 else 0
    scale = DK ** -0.5
    US = t["US"]

    def load(ci):
        c = order[ci]
        i = ci % NB
        k.dma("sp", u[i][:], US[c * 128:(c + 1) * 128, :], writes=[b_u[i]])

    load(0)
    nmine = 0
    for ci, c in enumerate(order):
        i = ci % NB
        if ci + 1 < len(order):
            load(ci + 1)
        for kc in range(8):
            k.op("pe", lambda e, kc=kc: e.transpose(pA[:, kc, :], u[i][:, kc * 128:(kc + 1) * 128], idb[:]),
                 reads=[b_u[i], b_cp], writes=[b_pA])
        k.op("act", lambda e: e.copy(uT[i][:], pA[:]), reads=[b_pA], writes=[b_uT[i]])
        for kc in range(8):
            k.op("pe", lambda e, kc=kc: e.matmul(pM[0:16, 0:128], lhsT=win[:, kc, 2048:2064], rhs=uT[i][:, kc, :],
                                                 start=(kc == 0), stop=(kc == 7)),
                 reads=[b_cp, b_uT[i]], writes=[b_pM])
        k.op("dve", lambda e: e.tensor_copy(aT[:], pM[0:16, 0:128]), reads=[b_pM], writes=[b_aT])
        k.op("pe", lambda e: e.matmul(pM[:], lhsT=aT[:], rhs=wa2[:], start=True, stop=False),
             reads=[b_aT, b_c], writes=[b_pM])
        k.op("pe", lambda e: e.matmul(pM[:], lhsT=ones[0:1, :], rhs=ba2[:], start=False, stop=True),
             reads=[b_c], writes=[b_pM])
        k.op("act", lambda e: e.activation(ez[:], pM[:], AF.Exp, scale=-1.0), reads=[b_pM], writes=[b_ez])
        k.op("act", lambda e: e.activation(ez[:], ez[:], AF.Ln, bias=ones[:, 0:1], scale=1.0),
             reads=[b_ez, b_c], writes=[b_ez])
        k.op("pool", lambda e: e.tensor_scalar(la[:], ez[:], -1.0 / 16.0, None, ALU.mult), reads=[b_ez],
             writes=[b_la])
        for hd in range(NH):
            k.op("pe", lambda e, hd=hd: e.matmul(pbT[:, hd, :], lhsT=la[:, hd * 128:(hd + 1) * 128], rhs=tri[:],
                                                 start=True, stop=True), reads=[b_la, b_c], writes=[b_pbT])
        k.op("pe", lambda e: e.matmul(pE[:], lhsT=tris[:], rhs=la[:], start=True, stop=True),
             reads=[b_la, b_c], writes=[b_pE])
        k.op("act", lambda e: e.activation(E1[:], pbT[:], AF.Exp), reads=[b_pbT], writes=[b_E1])
        k.op("act", lambda e: e.activation(E2[:], pbT[:], AF.Exp, scale=-1.0), reads=[b_pbT], writes=[b_E2])
        k.op("act", lambda e: e.activation(EK[:], pE[:], AF.Exp), reads=[b_pE], writes=[b_EK])
        for hd in range(NH):
            for kc in range(8):
                k.op("pe", lambda e, hd=hd, kc=kc: e.matmul(pq[:, hd, :], lhsT=win[:, kc, hd * 128:(hd + 1) * 128],
                                                            rhs=uT[i][:, kc, :], start=(kc == 0), stop=(kc == 7)),
                     reads=[b_cp, b_uT[i]], writes=[b_pq])
        k.op("dve", lambda e: e.scalar_tensor_tensor(qd[:], pq[:], scale, E1[:], ALU.mult, ALU.mult),
             reads=[b_pq, b_E1], writes=[b_qd])
        for hd in range(NH):
            for kc in range(8):
                k.op("pe", lambda e, hd=hd, kc=kc: e.matmul(pk[:, hd, :],
                                                            lhsT=win[:, kc, 512 + hd * 128:512 + (hd + 1) * 128],
                                                            rhs=uT[i][:, kc, :], start=(kc == 0), stop=(kc == 7)),
                     reads=[b_cp, b_uT[i]], writes=[b_pk])
        k.op("dve", lambda e: e.tensor_tensor(kd[:], pk[:], E2[:], ALU.mult), reads=[b_pk, b_E2], writes=[b_kd])
        for kc in range(8):
            k.op("pe", lambda e, kc=kc: e.matmul(pE[:], lhsT=uT[i][:, kc, :], rhs=win[:, kc, 512:1024],
                                                 start=(kc == 0), stop=(kc == 7)),
                 reads=[b_cp, b_uT[i]], writes=[b_pE])
        k.op("dve", lambda e: e.tensor_tensor(ke[:], pE[:], EK[:], ALU.mult), reads=[b_pE, b_EK], writes=[b_ke])
        for hf in range(2):
            for kc in range(8):
                k.op("pe", lambda e, kc=kc, hf=hf: e.matmul(pV[:, hf * 512:(hf + 1) * 512], lhsT=uT[i][:, kc, :],
                                                            rhs=win[:, kc, 1024 + hf * 512:1024 + (hf + 1) * 512],
                                                            start=(kc == 0), stop=(kc == 7)),
                     reads=[b_cp, b_uT[i]], writes=[b_pV])
        k.op("act", lambda e: e.copy(v[:], pV[:]), reads=[b_pV], writes=[b_v])
        mine = (c == 0) or (2 <= c < 2 + 32)
        for hd in range(NH):
            am, b_am = attm[hd % 2], b_attm[hd % 2]
            k.op("pe", lambda e, hd=hd: e.matmul(pat[:, 0:128], lhsT=kd[:, hd, :], rhs=qd[:, hd, :],
                                                 start=True, stop=True), reads=[b_kd, b_qd], writes=[b_pat])
            k.op("dve", lambda e, am=am: e.tensor_tensor(am[:], pat[:, 0:128], tri[:], ALU.mult),
                 reads=[b_pat, b_c], writes=[b_am])
            if mine:
                k.op("pe", lambda e, hd=hd, am=am: e.matmul(pV[:, hd * DV:(hd + 1) * DV], lhsT=am[:],
                                                            rhs=v[:, hd * DV:(hd + 1) * DV], start=True, stop=False),
                     reads=[b_am, b_v], writes=[b_pV])
                k.op("pe", lambda e, hd=hd: e.matmul(pV[:, hd * DV:(hd + 1) * DV], lhsT=qd[:, hd, :],
                                                     rhs=Sbf[:, hd, :], start=False, stop=True),
                     reads=[b_qd, b_Sbf[hd]], writes=[b_pV])
            k.op("pe", lambda e, hd=hd: e.matmul(pat[:, 0:DV], lhsT=ke[:, hd * 128:(hd + 1) * 128],
                                                 rhs=v[:, hd * DV:(hd + 1) * DV], start=True, stop=True),
                 reads=[b_ke, b_v], writes=[b_pat])
            k.op("dve", lambda e, hd=hd: e.scalar_tensor_tensor(S32[:, hd, :], S32[:, hd, :],
                                                                E1[:, hd, ecol:ecol + 1], pat[:, 0:DV],
                                                                ALU.mult, ALU.add),
                 reads=[b_S32[hd], b_E1, b_pat], writes=[b_S32[hd]])
            k.op("pool", lambda e, hd=hd: e.tensor_copy(Sbf[:, hd, :], S32[:, hd, :]), reads=[b_S32[hd]],
                 writes=[b_Sbf[hd]])
        if mine:
            ob, b_ob = osb[nmine % 2], b_osb[nmine % 2]
            nmine += 1
            k.op("act", lambda e, ob=ob: e.copy(ob[:], pV[:]), reads=[b_pV], writes=[b_ob])
            row = 0 if c == 0 else MYCTX + (c - 2) * 128
            k.dma("sp", t["OS"][d, row:row + 128, :], ob[:], reads=[b_ob], writes=[b_os])
    k.flush()


def phase_gla_post(k, P, G, b_G):
    t = P.t
    k.phase()
    pm = PostMixer(k, P, G, b_G)
    b_c = k.buf("glpconst"); b_cp = k.buf("glpconstp")
    idb = k.sb("glp_identb", [128, 128], BF16); k.dma("pool", idb[:], t["ident"], writes=[b_cp])
    wg = k.sb("glp_wg", [128, 8, D], BF16)
    wo = k.sb("glp_wo", [128, 8, D], BF16)
    for kc in range(8):
        k.dma("pool", wg[:, kc, :], t["w_in"][kc * 128:(kc + 1) * 128, 2048:3072], writes=[b_cp])
        k.dma("pool", wo[:, kc, :], t["w_out"][kc * 128:(kc + 1) * 128, :], writes=[b_cp])
    ngB = k.sb("glp_ng", [128, DV]); k.dma("sp", ngB[:], bc(t["norm_g"], [128, DV]), writes=[b_c])
    epsc = k.sb("glp_eps", [128, 1]); k.op("dve", lambda e: e.memset(epsc[:], LN_EPS), writes=[b_c])
    NB = 2
    u = [k.sb(f"glp_u{i}", [128, D], BF16) for i in range(NB)]; b_u = k.bufs(NB, "glp_u")
    of = [k.sb(f"glp_of{i}", [128, D]) for i in range(NB)]; b_of = k.bufs(NB, "glp_of")
    ob = [k.sb(f"glp_ob{i}", [128, D]) for i in range(NB)]; b_ob = k.bufs(NB, "glp_ob")
    sq = k.sb("glp_sq", [128, D]); b_sq = k.buf()
    ms = k.sb("glp_ms", [128, NH]); b_ms = k.buf()
    sg = k.sb("glp_sg", [128, D]); b_sg = k.buf()
    zb = k.sb("glp_zb", [128, D], BF16); b_zb = k.buf()
    pT = k.ps("glp_pT", [128, 8, 128], BF16); b_pT = k.pbuf("glp_pT")
    xT = k.sb("glp_xT", [128, 8, 128], BF16); b_xT = k.buf()
    pg = k.ps("glp_pg", [128, D]); b_pg = k.pbuf("glp_pg")
    ym = [k.sb(f"glp_ym{i}", [128, D]) for i in range(NB)]; b_ym = k.bufs(NB, "glp_ym")
    for tt in range(NT_MY):
        i = tt % NB
        pm.prefetch(tt)
        r0, r1 = my_rows(tt)
        k.dma("sp", u[i][:], t["US"][r0:r1, :], writes=[b_u[i]])
        k.dma("act", of[i][:], t["OS"][0, tt * 128:(tt + 1) * 128, :], writes=[b_of[i]])
        k.dma("act", ob[i][:], t["OS"][1, tt * 128:(tt + 1) * 128, :], writes=[b_ob[i]])
        for kc in range(8):
            k.op("pe", lambda e, kc=kc: e.transpose(pT[:, kc, :], u[i][:, kc * 128:(kc + 1) * 128], idb[:]),
                 reads=[b_u[i], b_cp], writes=[b_pT])
        k.op("act", lambda e: e.copy(xT[:], pT[:]), reads=[b_pT], writes=[b_xT])
        for hf in range(2):
            for kc in range(8):
                k.op("pe", lambda e, kc=kc, hf=hf: e.matmul(pg[:, hf * 512:(hf + 1) * 512], lhsT=xT[:, kc, :],
                                                            rhs=wg[:, kc, hf * 512:(hf + 1) * 512],
                                                            start=(kc == 0), stop=(kc == 7)),
                     reads=[b_xT, b_cp], writes=[b_pg])
        k.op("act", lambda e: e.activation(sg[:], pg[:], AF.Silu), reads=[b_pg], writes=[b_sg])
        k.op("pool", lambda e: e.tensor_tensor(of[i][:], of[i][:], ob[i][:], ALU.add), reads=[b_of[i], b_ob[i]],
             writes=[b_of[i]])
        k.op("dve", lambda e: e.tensor_tensor(sq[:], of[i][:], of[i][:], ALU.mult), reads=[b_of[i]], writes=[b_sq])
        k.op("dve", lambda e: e.reduce_sum(ms[:], sq[:].rearrange("p (h e) -> p h e", e=DV),
                                           axis=mybir.AxisListType.X), reads=[b_sq], writes=[b_ms])
        k.op("act", lambda e: e.activation(ms[:], ms[:], AF.Sqrt, bias=epsc[:], scale=1.0 / DV),
             reads=[b_ms, b_c], writes=[b_ms])
        k.op("dve", lambda e: e.reciprocal(ms[:], ms[:]), reads=[b_ms], writes=[b_ms])
        k.op("dve", lambda e: e.tensor_tensor(sq[:].rearrange("p (h e) -> p h e", e=DV),
                                              of[i][:].rearrange("p (h e) -> p h e", e=DV),
                                              bc(ms[:].unsqueeze(2), [128, NH, DV]), ALU.mult),
             reads=[b_of[i], b_ms], writes=[b_sq])
        k.op("pool", lambda e: e.tensor_tensor(sq[:].rearrange("p (h e) -> p h e", e=DV),
                                               sq[:].rearrange("p (h e) -> p h e", e=DV),
                                               bc(ngB[:].unsqueeze(1), [128, NH, DV]), ALU.mult),
             reads=[b_sq, b_c], writes=[b_sq])
        k.op("dve", lambda e: e.tensor_tensor(zb[:], sq[:], sg[:], ALU.mult), reads=[b_sq, b_sg], writes=[b_zb])
        for kc in range(8):
            k.op("pe", lambda e, kc=kc: e.transpose(pT[:, kc, :], zb[:, kc * 128:(kc + 1) * 128], idb[:]),
                 reads=[b_zb, b_cp], writes=[b_pT])
        k.op("act", lambda e: e.copy(xT[:], pT[:]), reads=[b_pT], writes=[b_xT])
        for hf in range(2):
            for kc in range(8):
                k.op("pe", lambda e, kc=kc, hf=hf: e.matmul(pg[:, hf * 512:(hf + 1) * 512], lhsT=xT[:, kc, :],
                                                            rhs=wo[:, kc, hf * 512:(hf + 1) * 512],
                                                            start=(kc == 0), stop=(kc == 7)),
                     reads=[b_xT, b_cp], writes=[b_pg])
        k.op("act", lambda e: e.copy(ym[i][:], pg[:]), reads=[b_pg], writes=[b_ym[i]])
        pm.emit(tt, ym[i][:], b_ym[i])
    k.flush()


def build_gla_layer():
    P = Prog("gla")
    declare_io(P)
    declare_common(P)
    declare_gla(P)
    emit_layer(P, "gla")
    return P


def gla_inputs(inp, j, s):
    c = consts()
    w_in = inp["gla_w_in"][j]
    w_a2, b_a2 = inp["gla_w_a2"][j], inp["gla_b_a2"][j]
    if s == 1:
        w_in = np.concatenate([w_in[:, :3072], w_in[:, 3088:3104], w_in[:, 3072:3088]], axis=1)
        w_a2, b_a2 = w_a2[::-1], b_a2[::-1]
    return {"w_in": w_in, "w_a2": w_a2, "b_a2": b_a2, "norm_g": inp["gla_norm_g"][j][None],
            "w_out": inp["gla_w_out"][j], "triF": c["triF"], "triFs": c["triFs"], "triB": c["triB"],
            "triBs": c["triBs"]}


def phase_handoff(k, P, kind_i, kind_n, ho0, ho1, hs0, hs1):
    t = P.t
    k.phase()
    b_c = k.buf("hoconst")
    flip = k.sb("ho_flip", [128, 128]); k.dma("sp", flip[:], t["flipm"], writes=[b_c])
    NB = 3
    a = [k.sb(f"ho_a{i}", [128, D]) for i in range(NB)]; b_a = k.bufs(NB, "ho_a")
    f = [k.sb(f"ho_f{i}", [128, D]) for i in range(NB)]; b_f = k.bufs(NB, "ho_f")
    pf = [k.ps(f"ho_p{i}", [128, D]) for i in range(2)]; b_pf = k.pbufs(2, "ho_p")
    NAT, CN = t["NAT"], t["CNAT"]
    b_nat = k.buf("NAT")
    cnt = [0]

    def nat_tile(kind, j):
        if kind == "s5":
            return NAT[j * 128:(j + 1) * 128, :]
        return NAT.rearrange("(r w) d -> w r d", w=64)[j]

    def move(dst, src, do_flip, b_dst):
        i = cnt[0] % NB
        cnt[0] += 1
        k.dma("sp", a[i][:], src, reads=[b_nat], writes=[b_a[i]])
        if not do_flip:
            k.dma("act", dst, a[i][:], reads=[b_a[i]], writes=[b_dst])
            return
        pb = cnt[0] % 2
        for hf in range(2):
            k.op("pe", lambda e, hf=hf: e.matmul(pf[pb][:, hf * 512:(hf + 1) * 512], lhsT=flip[:],
                                                 rhs=a[i][:, hf * 512:(hf + 1) * 512], start=True, stop=True),
                 reads=[b_a[i], b_c], writes=[b_pf[pb]])
        k.op("act", lambda e: e.copy(f[i][:], pf[pb][:]), reads=[b_pf[pb]], writes=[b_f[i]])
        k.dma("act", dst, f[i][:], reads=[b_f[i]], writes=[b_dst])

    move(CN[0:128, :], ho0[0:128, :], False, b_nat)
    move(CN[128:256, :], ho1[0:128, :], True, b_nat)
    for j in range(32):
        move(nat_tile(kind_i, j), ho0[MYCTX + j * 128:MYCTX + (j + 1) * 128, :], False, b_nat)
        move(nat_tile(kind_i, 32 + j), ho1[MYCTX + (31 - j) * 128:MYCTX + (32 - j) * 128, :], True, b_nat)
    b_hs = k.buf("HSnext")
    for c in range(2):
        move(hs0[c * 128:(c + 1) * 128, :], CN[c * 128:(c + 1) * 128, :], False, b_hs)
        move(hs1[(1 - c) * 128:(2 - c) * 128, :], CN[c * 128:(c + 1) * 128, :], True, b_hs)
    for j in range(64):
        move(hs0[NCTX + j * 128:NCTX + (j + 1) * 128, :], nat_tile(kind_n, j), False, b_hs)
        move(hs1[NCTX + (63 - j) * 128:NCTX + (64 - j) * 128, :], nat_tile(kind_n, j), True, b_hs)
    k.flush()


def build_fused():
    P = Prog("fused")
    declare_io(P)
    P.dscr("NAT", [NLAT, D]); P.dscr("CNAT", [NCTX, D])
    for v in range(NV):
        for nm in ("HSA", "HSB"):
            P.dscr(f"{nm}_{v}", [NSEQ, D])
        P.dscr(f"HO_{v}", [MYTOK, D])
    kinds = ["s5", "gla", "s5", "gla"]
    for i, kind in enumerate(kinds):
        P.prefix = f"L{i}_"
        declare_common(P)
        (declare_s5 if kind == "s5" else declare_gla)(P)
    layer_t = {}
    with ExitStack() as st:
        k = K(P.nc, st)
        G = k.sb("Gall", [128, NT_MY, NE], glob=True); b_G = k.buf("Gall")
        for i, kind in enumerate(kinds):
            for full, ap in P.ext.items():
                if full.startswith(f"L{i}_"):
                    P.t[full[len(f"L{i}_"):]] = ap
            for v in range(NV):
                P.sw = v % 2
                P.t["cvec"] = P.ext[f"cvec_{v}"]
                if i == 0:
                    P.t["hseq"] = P.ext[f"hseq_{v}"]
                else:
                    P.t["hseq"] = P.t[f"{'HSA' if i % 2 == 1 else 'HSB'}_{v}"]
                P.t["hout"] = P.ext[f"hout_{v}"] if i == 3 else P.t[f"HO_{v}"]
                if kind == "s5":
                    phases = [lambda: phase_adaln(k, P), lambda: phase_modulate(k, P, P.t["US"]),
                              lambda: phase_s5_setup(k, P, 0), lambda: phase_s5_setup(k, P, 1),
                              lambda: phase_s5(k, P), lambda: phase_s5_post(k, P, G, b_G),
                              lambda: phase_moe(k, P, G, b_G, SUPERTILES), lambda: phase_post_moe(k, P)]
                else:
                    phases = [lambda: phase_adaln(k, P), lambda: phase_modulate(k, P, P.t["US"]),
                              lambda: phase_gla_pass(k, P, 0), lambda: phase_gla_pass(k, P, 1),
                              lambda: phase_gla_post(k, P, G, b_G),
                              lambda: phase_moe(k, P, G, b_G, SUPERTILES), lambda: phase_post_moe(k, P)]
                for ph in phases:
                    ph()
            if i < 3:
                nxt = "HSA" if (i + 1) % 2 == 1 else "HSB"
                for bb in range(NV // 2):
                    phase_handoff(k, P, kind, kinds[i + 1], P.t[f"HO_{2 * bb}"], P.t[f"HO_{2 * bb + 1}"],
                                  P.t[f"{nxt}_{2 * bb}"], P.t[f"{nxt}_{2 * bb + 1}"])
    return P


_FUSED = []


def run_fused(inp):
    if not _FUSED:
        _FUSED.append(build_fused())
    P = _FUSED[0]
    h, hc = inp["x"], inp["ctx"]
    allv = [(b, s) for b in range(4) for s in range(2)]
    groups = [allv[p * NV:(p + 1) * NV] for p in range(NPHYS)]
    maps = []
    for grp in groups:
        m = dict(consts())
        for v, (b, s) in enumerate(grp):
            lat, ctx = h[b], hc[b]
            if s == 1:
                lat, ctx = lat[::-1], ctx[::-1]
            m[f"hseq_{v}"] = np.concatenate([ctx, lat], axis=0)
            m[f"cvec_{v}"] = np.stack([inp["c"][b], inp["c_ctx"]])
        for i in range(4):
            lw = common_inputs(inp, i, 0)
            lw.update(s5_inputs(inp, i // 2, 0) if i % 2 == 0 else gla_inputs(inp, i // 2, 0))
            for kk, vv in lw.items():
                if kk not in m:
                    m[f"L{i}_{kk}"] = vv
        ins = [n for n in P.ext if not n.startswith("hout_")]
        maps.append({n: np.ascontiguousarray(m[n], dtype=np.float32) for n in ins})
    res = run_bass_kernel_spmd(P.nc, maps, core_ids=list(range(NPHYS)))
    hn = np.empty_like(h)
    for p, grp in enumerate(groups):
        for v, (b, s) in enumerate(grp):
            o = res.results[p][f"hout_{v}"][MYCTX:]
            if s == 0:
                lat0 = o
            else:
                hn[b] = seq_unorder("gla", np.concatenate([lat0, o[::-1]], axis=0))
    return hn


DEBUG = False
_CONST = {}


def consts():
    if not _CONST:
        idx = np.arange(128)
        tau = idx // 16
        _CONST["ident"] = np.eye(128, dtype=np.float32)
        _CONST["maskF"] = (tau[None, :] >= tau[:, None]).astype(np.float32)
        _CONST["maskB"] = (tau[:, None] >= tau[None, :]).astype(np.float32)
        sw = np.zeros((128, 128), np.float32)
        sw[idx, (idx + 64) % 128] = 1.0
        _CONST["swapm"] = sw
        _CONST["flipm"] = np.ascontiguousarray(np.eye(128, dtype=np.float32)[::-1])
        jj, ii = idx[:, None], idx[None, :]
        _CONST["triF"] = (jj <= ii).astype(np.float32)
        _CONST["triFs"] = (jj > ii).astype(np.float32)
        _CONST["triB"] = (jj >= ii).astype(np.float32)
        _CONST["triBs"] = (jj < ii).astype(np.float32)
    return _CONST


def seq_order(kind, hb):
    if kind == "s5":
        return hb
    return hb.reshape(128, 64, D).transpose(1, 0, 2).reshape(NLAT, D)


def seq_unorder(kind, lat):
    if kind == "s5":
        return lat
    return lat.reshape(64, 128, D).transpose(1, 0, 2).reshape(NLAT, D)


def common_inputs(inp, i, b):
    c = consts()
    m = {
        "cvec": np.ascontiguousarray(np.stack([inp["c"][b], inp["c_ctx"]])),
        "w_ada": inp["w_ada"][i], "b_ada": inp["b_ada"][i][None],
        "ln1_g": inp["ln1_g"][i][None], "ln1_b": inp["ln1_b"][i][None],
        "ln2_g": inp["ln2_g"][i][None], "ln2_b": inp["ln2_b"][i][None],
        "w_router": inp["moe_w_router"][i], "b_router": inp["moe_b_router"][i][None],
        "w_up": inp["moe_w_up"][i], "b_up": inp["moe_b_up"][i],
        "w_down": inp["moe_w_down"][i], "b_down": inp["moe_b_down"][i],
        "ident": c["ident"],
    }
    return m


def s5_inputs(inp, j, s):
    c = consts()
    sl = slice(None) if s == 0 else slice(None, None, -1)
    f = lambda a: np.ascontiguousarray(a[j][sl])
    return {
        "lam_re": f(inp["s5_lam_re"]), "lam_im": f(inp["s5_lam_im"]), "log_step": f(inp["s5_log_step"]),
        "b_re": f(inp["s5_b_re"]), "b_im": f(inp["s5_b_im"]), "c_re": f(inp["s5_c_re"]), "c_im": f(inp["s5_c_im"]),
        "s5_d": inp["s5_d"][j][None], "w_glu": inp["s5_w_glu"][j],
        "maskF": c["maskF"], "maskB": c["maskB"], "swapm": c["swapm"],
    }


_PROGS = {}


def get_prog(kind):
    if kind not in _PROGS:
        _PROGS[kind] = build_s5_layer() if kind == "s5" else build_gla_layer()
    return _PROGS[kind]


def run_layer(inp, i, h, hc, cores=None):
    kind = "s5" if i % 2 == 0 else "gla"
    P = get_prog(kind)
    allv = [(b, s) for b in range(4) for s in range(2)]
    groups = [allv[p * NV:(p + 1) * NV] for p in range(NPHYS)]
    maps = []
    for grp in groups:
        m = common_inputs(inp, i, 0)
        m.pop("cvec")
        for v, (b, s) in enumerate(grp):
            lat = seq_order(kind, h[b])
            ctx = hc[b]
            if s == 1:
                lat, ctx = lat[::-1], ctx[::-1]
            m[f"hseq_{v}"] = np.concatenate([ctx, lat], axis=0)
            m[f"cvec_{v}"] = np.stack([inp["c"][b], inp["c_ctx"]])
        m.update(s5_inputs(inp, i // 2, 0) if kind == "s5" else gla_inputs(inp, i // 2, 0))
        maps.append({kk: np.ascontiguousarray(vv, dtype=np.float32) for kk, vv in m.items() if kk in P.t})
    res = run_bass_kernel_spmd(P.nc, maps, core_ids=list(range(NPHYS)))
    hn = np.empty_like(h)
    hcn = np.empty_like(hc)
    lat_new = {}
    for p, grp in enumerate(groups):
        for v, (b, s) in enumerate(grp):
            o = res.results[p][f"hout_{v}"]
            if s == 0:
                hcn[b, 0:128] = o[0:128]
                lat_new[(b, 0)] = o[128:]
            else:
                hcn[b, 128:256] = o[0:128][::-1]
                lat_new[(b, 1)] = o[128:][::-1]
    for b in range(4):
        hn[b] = seq_unorder(kind, np.concatenate([lat_new[(b, 0)], lat_new[(b, 1)]], axis=0))
    return hn, hcn, res


def kernel(**inp):
    inp = {kk: np.asarray(v, dtype=np.float32) for kk, v in inp.items()}
    return run_fused(inp)
```

```python
from contextlib import ExitStack
import math
import numpy as np
import concourse.bass as bass
import concourse.mybir as mybir
from concourse.bass_utils import run_bass_kernel_spmd

F32 = mybir.dt.float32
BF16 = mybir.dt.bfloat16
ALU = mybir.AluOpType
AF = mybir.ActivationFunctionType

D = 1024
NCTX = 256
NLAT = 8192
NSEQ = NCTX + NLAT
MYCTX = 128
MYLAT = 4096
MYTOK = MYCTX + MYLAT
NT_MY = MYTOK // 128
NT_SEQ = NSEQ // 128
NE = 32
DN_ALPHA = 8 ** 0.25
LN_EPS = 1e-5
PI = math.pi

ENGS = ("pe", "act", "dve", "pool", "sp")
DEBUG_SCR = False
NV = 2
NPHYS = 4
NPHASES = 100
S5_NGB = 8
S5_NG8 = 8
S5_STOP = 99
S5_SUB = 99


class Buf:
    __slots__ = ("name", "lw", "rd", "dsem", "psem", "excl")

    def __init__(self, name, excl=False):
        self.name = name
        self.excl = excl
        self.lw = None
        self.rd = []
        self.dsem = None
        self.psem = None


class _Rec:
    def __getattr__(self, name):
        def call(*a, **kw):
            self.rec = (name, a, kw)
            return self
        return call


class K:
    def __init__(self, nc, stack):
        self.nc = nc
        self.gstack = stack
        self.sems = {}
        self.cnt = {}
        for e in ENGS:
            self._mksem("E_" + e)
        self.free_d = []
        self.free_p = []
        self.nd = 0
        self.nbuf = 0
        self._reset()

    def _reset(self):
        self.prog = {e: [] for e in ENGS}
        self.seen = {e: dict(self.cnt) for e in ENGS}
        self.dbufs = []
        self.pbufs_ = []

    def _mksem(self, key):
        self.sems[key] = self.gstack.enter_context(self.nc.semaphore(key))
        self.cnt[key] = 0
        return key

    def buf(self, name=None, excl=False):
        self.nbuf += 1
        return Buf(name or f"b{self.nbuf}", excl)

    def bufs(self, n, name="b", excl=False):
        return [self.buf(f"{name}{i}", excl) for i in range(n)]

    def pbuf(self, name=None):
        return self.buf(name, True)

    def pbufs(self, n, name="p"):
        return self.bufs(n, name, True)

    def phase(self):
        self.pstack = ExitStack()
        return self.pstack

    def sb(self, name, shape, dt=F32, glob=False):
        st = self.gstack if glob else self.pstack
        self.nbuf += 1
        return st.enter_context(self.nc.sbuf_tensor(f"{name}_{self.nbuf}", list(shape), dt))

    def ps(self, name, shape, dt=F32):
        self.nbuf += 1
        esz = 4 if dt == F32 else 2
        n = 1
        for d_ in shape[1:]:
            n *= d_
        per_bank = 2048 // esz
        npad = ((n + per_bank - 1) // per_bank) * per_bank
        tt = self.pstack.enter_context(self.nc.psum_tensor(f"{name}_{self.nbuf}", [128, npad], dt))
        v = tt[0:shape[0], 0:n]
        if len(shape) == 3:
            v = v.rearrange("p (a b) -> p a b", b=shape[2])
        elif len(shape) == 4:
            v = v.rearrange("p (a b c) -> p a b c", b=shape[2], c=shape[3])
        return v

    def _deps(self, eng, reads, writes):
        waits = {}

        def need(ev):
            if ev is not None and waits.get(ev[0], 0) < ev[1]:
                waits[ev[0]] = ev[1]
        for b in reads:
            need(b.lw)
        for b in writes:
            need(b.lw)
            for r in b.rd:
                need(r)
        out = {}
        seen = self.seen[eng]
        for kk, v in waits.items():
            if eng == "pe" and kk == "E_pe":
                continue
            if seen.get(kk, 0) >= v:
                continue
            seen[kk] = v
            out[kk] = v
        return out

    def _commit(self, ev, reads, writes):
        for b in reads:
            b.rd.append(ev)
            if len(b.rd) > 24:
                m = {}
                for kk, v in b.rd:
                    if m.get(kk, 0) < v:
                        m[kk] = v
                b.rd = list(m.items())
        for b in writes:
            b.lw = ev
            b.rd = []

    def op(self, eng, fn, reads=(), writes=()):
        ex = [b for b in reads if b.excl]
        if ex:
            writes = list(writes) + ex
        waits = self._deps(eng, reads, writes)
        key = "E_" + eng
        self.cnt[key] += 1
        ev = (key, self.cnt[key])
        sems = self.sems
        rec = _Rec()
        fn(rec)
        name, a, kw = rec.rec

        def run(e, waits=waits, name=name, a=a, kw=kw, sem=sems[key]):
            for kk, v in waits.items():
                e.wait_ge(sems[kk], v)
            getattr(e, name)(*a, **kw).then_inc(sem, 1)
        self.prog[eng].append(run)
        self._commit(ev, reads, writes)

    def dma(self, eng, out, in_, reads=(), writes=(), **kw):
        waits = self._deps(eng, reads, writes)
        b = writes[0]
        if eng == "pool":
            if getattr(b, "psem", None) is None:
                if self.free_p:
                    b.psem = self.free_p.pop()
                else:
                    self.nd += 1
                    b.psem = self._mksem(f"DP_{self.nd}")
                self.pbufs_.append(b)
            key = b.psem
        else:
            if b.dsem is None:
                if self.free_d:
                    b.dsem = self.free_d.pop()
                else:
                    self.nd += 1
                    b.dsem = self._mksem(f"D_{self.nd}")
                self.dbufs.append(b)
            key = b.dsem
        self.cnt[key] += 16
        ev = (key, self.cnt[key])
        sems = self.sems

        def run(e, waits=waits, sem=sems[key]):
            for kk, v in waits.items():
                e.wait_ge(sems[kk], v)
            e.dma_start(out=out, in_=in_, **kw).then_inc(sem, 16)
        self.prog[eng].append(run)
        self._commit(ev, reads, writes)

    def flush(self):
        final = {kk: v for kk, v in self.cnt.items() if v > 0}
        sems = self.sems

        def bar(e, final=final):
            for kk, v in final.items():
                e.wait_ge(sems[kk], v)
        prog = self.prog
        for eng in ENGS:
            prog[eng].append(bar)
        nc = self.nc
        with nc.Block() as block:
            @block.sync
            def _(e):
                for f in prog["sp"]:
                    f(e)

            @block.tensor
            def _(e):
                for f in prog["pe"]:
                    f(e)

            @block.vector
            def _(e):
                for f in prog["dve"]:
                    f(e)

            @block.scalar
            def _(e):
                for f in prog["act"]:
                    f(e)

            @block.gpsimd
            def _(e):
                for f in prog["pool"]:
                    f(e)
        for b in self.dbufs:
            self.free_d.append(b.dsem)
            b.dsem = None
        for b in self.pbufs_:
            self.free_p.append(b.psem)
            b.psem = None
        self._reset()
        self.pstack.close()


def bc(ap, shape):
    return ap.broadcast_to(list(shape))


class Prog:
    def __init__(self, kind):
        self.kind = kind
        self.nc = bass.Bass("TRN2", target_bir_lowering=False)
        self.t = {}
        self.ext = {}
        self.scr = set()
        self.prefix = ""
        self.sw = 0

    def din(self, name, shape, dt=F32):
        full = self.prefix + name
        if full not in self.ext:
            self.ext[full] = self.nc.dram_tensor(full, list(shape), dt, kind="ExternalInput").ap()
        self.t[name] = self.ext[full]
        return self.t[name]

    def dout(self, name, shape, dt=F32):
        self.t[name] = self.nc.dram_tensor(name, list(shape), dt, kind="ExternalOutput").ap()
        self.ext[name] = self.t[name]
        return self.t[name]

    def dscr(self, name, shape, dt=F32):
        if name in self.t and name in self.scr:
            return self.t[name]
        self.scr.add(name)
        self.t[name] = self.nc.dram_tensor(name, list(shape), dt, kind=("ExternalOutput" if DEBUG_SCR else "Internal")).ap()
        return self.t[name]


def declare_io(P):
    for v in range(NV):
        P.din(f"hseq_{v}", [NSEQ, D])
        P.din(f"cvec_{v}", [2, D])
        P.dout(f"hout_{v}", [MYTOK, D])


def declare_common(P):
    P.din("w_ada", [D, 6 * D])
    P.din("b_ada", [1, 6 * D])
    for n in ("ln1_g", "ln1_b", "ln2_g", "ln2_b"):
        P.din(n, [1, D])
    P.din("w_router", [D, NE])
    P.din("b_router", [1, NE])
    if NPHASES >= 7:
        P.din("w_up", [NE, D, 2 * D])
        P.din("b_up", [NE, 2 * D])
        P.din("w_down", [NE, D, D])
        P.din("b_down", [NE, D])
    _pf, P.prefix = P.prefix, ""
    P.din("ident", [128, 128])
    P.prefix = _pf
    P.dscr("MOD", [2, 6 * D])
    P.dscr("H1", [MYTOK, D])
    P.dscr("U2T", [D, MYTOK], BF16)
    P.dscr("FS", [MYTOK, D])


def my_rows(t):
    if t == 0:
        return 0, 128
    return NCTX + (t - 1) * 128, NCTX + t * 128


def phase_adaln(k, P):
    t = P.t
    k.phase()
    cT = k.sb("cT", [128, 8, 2]); b_cT = k.buf()
    for r in range(2):
        k.dma("sp", cT[:, :, r], t["cvec"][r].rearrange("(kc p) -> p kc", p=128), writes=[b_cT],
              allow_slow_non_contiguous=True)
    sT = k.sb("sT", [128, 8, 2]); b_sT = k.buf()
    k.op("act", lambda e: e.activation(sT[:], cT[:], AF.Silu), reads=[b_cT], writes=[b_sT])
    wbuf = [k.sb(f"wada{i}", [128, 8, 512]) for i in range(2)]
    b_w = k.bufs(2, "wada")
    bb = [k.sb(f"bada{i}", [2, 512]) for i in range(2)]
    b_bb = k.bufs(2, "bada")
    pm = [k.ps(f"pmod{i}", [2, 512]) for i in range(2)]
    b_pm = k.pbufs(2, "pmod")
    res = [k.sb(f"rmod{i}", [2, 512]) for i in range(2)]
    b_res = k.bufs(2, "rmod")
    b_mod = k.buf("MODscr")
    for j in range(12):
        i = j % 2
        k.dma("sp", wbuf[i][:], t["w_ada"][:, j * 512:(j + 1) * 512].rearrange("(kc p) n -> p kc n", p=128),
              writes=[b_w[i]])
        k.dma("act", bb[i][:], bc(t["b_ada"][0:1, j * 512:(j + 1) * 512], [2, 512]), writes=[b_bb[i]])
        for kc in range(8):
            k.op("pe", lambda e, i=i, kc=kc: e.matmul(pm[i][:], lhsT=sT[:, kc, :], rhs=wbuf[i][:, kc, :],
                                                      start=(kc == 0), stop=(kc == 7)),
                 reads=[b_sT, b_w[i]], writes=[b_pm[i]])
        add1 = 1.0 if j in (2, 3, 8, 9) else 0.0
        k.op("dve", lambda e, i=i, add1=add1: e.scalar_tensor_tensor(
            res[i][:], pm[i][:], add1, bb[i][:], ALU.add, ALU.add),
            reads=[b_pm[i], b_bb[i]], writes=[b_res[i]])
        k.dma("sp", t["MOD"][:, j * 512:(j + 1) * 512], res[i][:], reads=[b_res[i]], writes=[b_mod])
    k.flush()


MOD_SH1, MOD_SC1, MOD_G1, MOD_SH2, MOD_SC2, MOD_G2 = range(6)


def mod_bc(P, r, which, n=128):
    return bc(P.t["MOD"][r:r + 1, which * D:(which + 1) * D], [n, D])


def phase_modulate(k, P, uscr):
    t = P.t
    k.phase()
    A = [k.sb(f"mA{r}", [128, D]) for r in range(2)]
    B = [k.sb(f"mB{r}", [128, D]) for r in range(2)]
    b_ab = k.buf()
    for r in range(2):
        k.dma("sp", A[r][:], mod_bc(P, r, MOD_SC1), writes=[b_ab])
        k.dma("sp", B[r][:], mod_bc(P, r, MOD_SH1), writes=[b_ab])
    NB = 3
    hb = [k.sb(f"mh{i}", [128, D]) for i in range(NB)]; b_h = k.bufs(NB, "mh")
    tb = [k.sb(f"mt{i}", [128, D]) for i in range(NB)]; b_t = k.bufs(NB, "mt")
    ub = [k.sb(f"mu{i}", [128, D], BF16) for i in range(NB)]; b_u = k.bufs(NB, "mu")
    b_us = k.buf("uscr")
    for tt in range(NT_SEQ):
        i = tt % NB
        r = 1 if tt < 2 else 0
        k.dma("sp", hb[i][:], t["hseq"][tt * 128:(tt + 1) * 128, :], writes=[b_h[i]])
        k.op("pool", lambda e, i=i, r=r: e.tensor_tensor(tb[i][:], hb[i][:], A[r][:], ALU.mult),
             reads=[b_h[i], b_ab], writes=[b_t[i]])
        k.op("dve", lambda e, i=i, r=r: e.tensor_tensor(ub[i][:], tb[i][:], B[r][:], ALU.add),
             reads=[b_t[i], b_ab], writes=[b_u[i]])
        k.dma("act", uscr[tt * 128:(tt + 1) * 128, :], ub[i][:], reads=[b_u[i]], writes=[b_us])
    k.flush()


def emit_ln(k, x, b_x, out, b_out, gB, bB, b_gb, wk):
    st, b_st = wk["st"], wk["b_st"]
    mv, b_mv = wk["mv"], wk["b_mv"]
    rs, b_rs = wk["rs"], wk["b_rs"]
    xn, b_xn = wk["xn"], wk["b_xn"]
    for c in range(2):
        k.op("dve", lambda e, c=c: e.bn_stats(st[:, c, :], x[:, c * 512:(c + 1) * 512]),
             reads=[b_x], writes=[b_st])
    k.op("dve", lambda e: e.bn_aggr(mv[:], st[:]), reads=[b_st], writes=[b_mv])
    k.op("act", lambda e: e.activation(rs[:], mv[:, 1:2], AF.Sqrt, bias=wk["eps"][:], scale=1.0),
         reads=[b_mv, wk["b_eps"]], writes=[b_rs])
    k.op("dve", lambda e: e.reciprocal(rs[:], rs[:]), reads=[b_rs], writes=[b_rs])
    k.op("dve", lambda e: e.tensor_scalar(xn[:], x[:], mv[:, 0:1], rs[:], ALU.subtract, ALU.mult),
         reads=[b_x, b_mv, b_rs], writes=[b_xn])
    k.op("pool", lambda e: e.tensor_tensor(xn[:], xn[:], gB[:], ALU.mult), reads=[b_xn, b_gb], writes=[b_xn])
    k.op("dve", lambda e: e.tensor_tensor(out[:], xn[:], bB[:], ALU.add), reads=[b_xn, b_gb], writes=[b_out])


def ln_work(k, pfx):
    eps = k.sb(pfx + "eps", [128, 1]); b_eps = k.buf()
    k.op("dve", lambda e: e.memset(eps[:], LN_EPS), writes=[b_eps])
    return dict(eps=eps, b_eps=b_eps, st=k.sb(pfx + "st", [128, 2, 6]), b_st=k.buf(), mv=k.sb(pfx + "mv", [128, 2]), b_mv=k.buf(),
                rs=k.sb(pfx + "rs", [128, 1]), b_rs=k.buf(), xn=k.sb(pfx + "xn", [128, D]), b_xn=k.buf())


class PostMixer:
    def __init__(self, k, P, G, b_G):
        self.k, self.P, self.G, self.b_G = k, P, G, b_G
        t = P.t
        self.b_c = k.buf("pmconst")
        self.g1B = [k.sb(f"g1B{r}", [128, D]) for r in range(2)]
        for r in range(2):
            k.dma("sp", self.g1B[r][:], mod_bc(P, r, MOD_G1), writes=[self.b_c])
        self.lgB = k.sb("ln1gB", [128, D]); self.lbB = k.sb("ln1bB", [128, D])
        k.dma("sp", self.lgB[:], bc(t["ln1_g"], [128, D]), writes=[self.b_c])
        k.dma("sp", self.lbB[:], bc(t["ln1_b"], [128, D]), writes=[self.b_c])
        self.sc2c = k.sb("sc2c", [128, 2, 8]); self.sh2c = k.sb("sh2c", [128, 2, 8])
        for r in range(2):
            k.dma("sp", self.sc2c[:, r, :], t["MOD"][r, MOD_SC2 * D:(MOD_SC2 + 1) * D].rearrange("(kc p) -> p kc", p=128),
                  writes=[self.b_c], allow_slow_non_contiguous=True)
            k.dma("sp", self.sh2c[:, r, :], t["MOD"][r, MOD_SH2 * D:(MOD_SH2 + 1) * D].rearrange("(kc p) -> p kc", p=128),
                  writes=[self.b_c], allow_slow_non_contiguous=True)
        self.wr = k.sb("wr32", [128, 8, NE])
        k.dma("sp", self.wr[:], t["w_router"].rearrange("(kc p) n -> p kc n", p=128), writes=[self.b_c])
        self.brB = k.sb("brB", [128, NE])
        k.dma("sp", self.brB[:], bc(t["b_router"], [128, NE]), writes=[self.b_c])
        self.idt = k.sb("pm_ident", [128, 128])
        k.dma("sp", self.idt[:], t["ident"], writes=[self.b_c])
        NB = 2
        self.NB = NB
        self.h = [k.sb(f"pmh{i}", [128, D]) for i in range(NB)]; self.b_h = k.bufs(NB, "pmh")
        self.t1 = [k.sb(f"pmt{i}", [128, D]) for i in range(NB)]; self.b_t1 = k.bufs(NB, "pmt")
        self.h1 = [k.sb(f"pmh1{i}", [128, D]) for i in range(NB)]; self.b_h1 = k.bufs(NB, "pmh1")
        self.lnw = [ln_work(k, f"pml{i}") for i in range(NB)]
        _pT = k.ps("pmpT", [128, 8, 128]); _bpT = k.pbuf("pmpT")
        self.pT = [_pT] * NB; self.b_pT = [_bpT] * NB
        self.u32 = [k.sb(f"pmu32{i}", [128, 8, 128]) for i in range(NB)]; self.b_u32 = k.bufs(NB, "pmu32")
        self.ubf = [k.sb(f"pmubf{i}", [128, 8, 128], BF16) for i in range(NB)]; self.b_ubf = k.bufs(NB, "pmubf")
        _pl = k.ps("pmpl", [128, NE]); _bpl = k.pbuf("pmpl")
        self.pl = [_pl] * NB; self.b_pl = [_bpl] * NB
        self.lg = [k.sb(f"pmlg{i}", [128, NE]) for i in range(NB)]; self.b_lg = k.bufs(NB, "pmlg")
        self.m8 = [k.sb(f"pmm8{i}", [128, 8]) for i in range(NB)]; self.b_m8 = k.bufs(NB, "pmm8")
        self.ex = [k.sb(f"pmex{i}", [128, NE]) for i in range(NB)]; self.b_ex = k.bufs(NB, "pmex")
        self.mk = [k.sb(f"pmmk{i}", [128, NE]) for i in range(NB)]; self.b_mk = k.bufs(NB, "pmmk")
        self.sm = [k.sb(f"pmsm{i}", [128, 2]) for i in range(NB)]; self.b_sm = k.bufs(NB, "pmsm")
        self.b_h1s = k.buf("H1scr")
        self.b_u2s = k.buf("U2Tscr")

    def prefetch(self, tt):
        k, t = self.k, self.P.t
        i = tt % self.NB
        r0, r1 = my_rows(tt)
        k.dma("sp", self.h[i][:], t["hseq"][r0:r1, :], writes=[self.b_h[i]])

    def emit(self, tt, ymix, b_ymix):
        k, t = self.k, self.P.t
        i = tt % self.NB
        r = 1 if tt == 0 else 0
        h, t1, h1 = self.h[i], self.t1[i], self.h1[i]
        k.op("pool", lambda e: e.tensor_tensor(t1[:], ymix, self.g1B[r][:], ALU.mult),
             reads=[b_ymix, self.b_c], writes=[self.b_t1[i]])
        k.op("dve", lambda e: e.scalar_tensor_tensor(t1[:], h[:], DN_ALPHA, t1[:], ALU.mult, ALU.add),
             reads=[self.b_h[i], self.b_t1[i]], writes=[self.b_t1[i]])
        emit_ln(k, t1, self.b_t1[i], h1, self.b_h1[i], self.lgB, self.lbB, self.b_c, self.lnw[i])
        k.dma("act", t["H1"][tt * 128:(tt + 1) * 128, :], h1[:], reads=[self.b_h1[i]], writes=[self.b_h1s])
        pT, u32, ubf = self.pT[i], self.u32[i], self.ubf[i]
        for kc in range(8):
            k.op("pe", lambda e, kc=kc: e.transpose(pT[:, kc, :], h1[:, kc * 128:(kc + 1) * 128], self.idt[:]),
                 reads=[self.b_h1[i], self.b_c], writes=[self.b_pT[i]])
        k.op("dve", lambda e: e.tensor_tensor(u32[:], pT[:], bc(self.sc2c[:, r, :].unsqueeze(2), [128, 8, 128]),
                                              ALU.mult), reads=[self.b_pT[i], self.b_c], writes=[self.b_u32[i]])
        k.op("pool", lambda e: e.tensor_tensor(u32[:], u32[:], bc(self.sh2c[:, r, :].unsqueeze(2), [128, 8, 128]),
                                               ALU.add), reads=[self.b_u32[i], self.b_c], writes=[self.b_u32[i]])
        k.op("act", lambda e: e.copy(ubf[:], u32[:]), reads=[self.b_u32[i]], writes=[self.b_ubf[i]])
        k.dma("act", t["U2T"][:, tt * 128:(tt + 1) * 128].rearrange("(kc p) n -> p kc n", p=128), ubf[:],
              reads=[self.b_ubf[i]], writes=[self.b_u2s])
        pl, lg, m8, ex, mk, sm = self.pl[i], self.lg[i], self.m8[i], self.ex[i], self.mk[i], self.sm[i]
        for kc in range(8):
            k.op("pe", lambda e, kc=kc: e.matmul(pl[:], lhsT=u32[:, kc, :], rhs=self.wr[:, kc, :],
                                                 start=(kc == 0), stop=(kc == 7)),
                 reads=[self.b_u32[i], self.b_c], writes=[self.b_pl[i]])
        k.op("dve", lambda e: e.tensor_tensor(lg[:], pl[:], self.brB[:], ALU.add),
             reads=[self.b_pl[i], self.b_c], writes=[self.b_lg[i]])
        k.op("dve", lambda e: e.max(out=m8[:], in_=lg[:]), reads=[self.b_lg[i]], writes=[self.b_m8[i]])
        k.op("dve", lambda e: e.tensor_scalar(mk[:], lg[:], m8[:, 3:4], None, ALU.is_ge),
             reads=[self.b_lg[i], self.b_m8[i]], writes=[self.b_mk[i]])
        k.op("dve", lambda e: e.tensor_scalar(ex[:], lg[:], m8[:, 0:1], None, ALU.subtract),
             reads=[self.b_lg[i], self.b_m8[i]], writes=[self.b_ex[i]])
        k.op("act", lambda e: e.activation(ex[:], ex[:], AF.Exp), reads=[self.b_ex[i]], writes=[self.b_ex[i]])
        k.op("dve", lambda e: e.tensor_tensor(ex[:], ex[:], mk[:], ALU.mult),
             reads=[self.b_ex[i], self.b_mk[i]], writes=[self.b_ex[i]])
        k.op("dve", lambda e: e.reduce_sum(sm[:, 0:1], ex[:], axis=mybir.AxisListType.X),
             reads=[self.b_ex[i]], writes=[self.b_sm[i]])
        k.op("dve", lambda e: e.reciprocal(sm[:, 1:2], sm[:, 0:1]), reads=[self.b_sm[i]], writes=[self.b_sm[i]])
        k.op("dve", lambda e: e.tensor_scalar(self.G[:, tt, :], ex[:], sm[:, 1:2], None, ALU.mult),
             reads=[self.b_ex[i], self.b_sm[i]], writes=[self.b_G])


def phase_moe(k, P, G, b_G, supertiles):
    t = P.t
    k.phase()
    b_c = k.buf("moeconst")
    idt = k.sb("moe_ident", [128, 128])
    k.dma("sp", idt[:], t["ident"], writes=[b_c])
    bup = k.sb("bup", [128, NE, 16])
    for e4 in range(0, NE, 8):
        k.dma("sp", bup[:, e4:e4 + 8, :], t["b_up"][e4:e4 + 8, :].rearrange("e (fc p) -> p e fc", p=128),
              writes=[b_c], allow_slow_non_contiguous=True)
    bdn = k.sb("bdn", [NE, D])
    k.dma("sp", bdn[:], t["b_down"], writes=[b_c])
    NW = 2
    wu = [k.sb(f"wu{i}", [128, 8, 1024], BF16) for i in range(NW)]; b_wu = k.bufs(NW, "wu")
    wd = [k.sb(f"wd{i}", [128, 4, 1024], BF16) for i in range(NW)]; b_wd = k.bufs(NW, "wd")
    maxtok = max(n for _, n in supertiles) * 128
    maxtl = max(n for _, n in supertiles)
    uT = k.sb("moe_uT", [128, 8, maxtok], BF16); b_uT = k.buf("moe_uT")
    acc = k.sb("moe_acc", [128, maxtl, D]); b_acc = [k.buf(f"acc{i}") for i in range(maxtl)]
    phg = [k.ps(f"phg{i}", [128, 512]) for i in range(2)]; b_phg = k.pbufs(2, "phg")
    phl = [k.ps(f"phl{i}", [128, 512]) for i in range(2)]; b_phl = k.pbufs(2, "phl")
    py = [k.ps(f"py{i}", [128, 1024]) for i in range(2)]; b_py = k.pbufs(2, "py")
    NA = 2
    g1 = [k.sb(f"mg1{i}", [128, 512]) for i in range(NA)]; b_g1 = k.bufs(NA, "mg1")
    sg = [k.sb(f"msg{i}", [128, 512]) for i in range(NA)]; b_sg = k.bufs(NA, "msg")
    l1 = [k.sb(f"ml1{i}", [128, 512]) for i in range(NA)]; b_l1 = k.bufs(NA, "ml1")
    aT = [k.sb(f"maT{i}", [128, 4, 512], BF16) for i in range(2)]; b_aT = k.bufs(2, "maT")
    gt = k.sb("moe_gT", [NE, 128]); b_gt = k.buf("moe_gT")
    b_fs = k.buf("FSscr")

    units = [(e, fh) for e in range(NE) for fh in range(2)]

    def load_unit(ui, slot):
        e, fh = units[ui]
        k.dma("pool", wu[slot][:, :, 0:512],
              t["w_up"][e, :, fh * 512:(fh + 1) * 512].rearrange("(kc p) f -> p kc f", p=128), writes=[b_wu[slot]])
        k.dma("pool", wu[slot][:, :, 512:1024],
              t["w_up"][e, :, 1024 + fh * 512:1024 + (fh + 1) * 512].rearrange("(kc p) f -> p kc f", p=128),
              writes=[b_wu[slot]])
        k.dma("pool", wd[slot][:], t["w_down"][e, fh * 512:(fh + 1) * 512, :].rearrange("(fc p) d -> p fc d", p=128),
              writes=[b_wd[slot]])

    cnt_act = 0
    cnt_chunk = 0
    cnt_y = 0
    for (t0, ntl) in supertiles:
        ntok = ntl * 128
        k.dma("sp", uT[:, :, 0:ntok], t["U2T"][:, t0 * 128:t0 * 128 + ntok].rearrange("(kc p) n -> p kc n", p=128),
              writes=[b_uT])
        for tl in range(ntl):
            pT = py[cnt_y % 2]; bpT = b_py[cnt_y % 2]; cnt_y += 1
            k.op("pe", lambda e, tl=tl, pT=pT: e.transpose(pT[0:NE, 0:128], G[:, t0 + tl, :], idt[:]),
                 reads=[b_G, b_c], writes=[bpT])
            k.op("act", lambda e, pT=pT: e.copy(gt[:], pT[0:NE, 0:128]), reads=[bpT], writes=[b_gt])
            for hf in range(2):
                k.op("pe", lambda e, hf=hf, pT=pT: e.matmul(pT[:, hf * 512:(hf + 1) * 512], lhsT=gt[:],
                                                            rhs=bdn[:, hf * 512:(hf + 1) * 512], start=True, stop=True),
                     reads=[b_gt, b_c, bpT], writes=[bpT])
            k.op("act", lambda e, tl=tl, pT=pT: e.copy(acc[:, tl, :], pT[:]), reads=[bpT], writes=[b_acc[tl]])
        load_unit(0, 0)
        for ui, (ex, fh) in enumerate(units):
            slot = ui % NW
            if ui + 1 < len(units):
                load_unit(ui + 1, (ui + 1) % NW)
            chunks = [(c0, min(512, ntok - c0)) for c0 in range(0, ntok, 512)]
            for (c0, n) in chunks:
                ab = cnt_chunk % 2; cnt_chunk += 1
                for fc in range(4):
                    pb = cnt_act % 2
                    ai = cnt_act % NA
                    cnt_act += 1
                    for kc in range(8):
                        k.op("pe", lambda e, kc=kc, fc=fc, pb=pb, c0=c0, n=n: e.matmul(
                            phg[pb][:, 0:n], lhsT=wu[slot][:, kc, fc * 128:(fc + 1) * 128],
                            rhs=uT[:, kc, c0:c0 + n], start=(kc == 0), stop=(kc == 7)),
                            reads=[b_wu[slot], b_uT], writes=[b_phg[pb]])
                    for kc in range(8):
                        k.op("pe", lambda e, kc=kc, fc=fc, pb=pb, c0=c0, n=n: e.matmul(
                            phl[pb][:, 0:n], lhsT=wu[slot][:, kc, 512 + fc * 128:512 + (fc + 1) * 128],
                            rhs=uT[:, kc, c0:c0 + n], start=(kc == 0), stop=(kc == 7)),
                            reads=[b_wu[slot], b_uT], writes=[b_phl[pb]])
                    fcg = fh * 4 + fc
                    k.op("dve", lambda e, pb=pb, ai=ai, n=n, fcg=fcg, ex=ex: e.tensor_scalar(
                        g1[ai][:, 0:n], phg[pb][:, 0:n], bup[:, ex, fcg:fcg + 1], 7.0, ALU.add, ALU.min),
                        reads=[b_phg[pb], b_c], writes=[b_g1[ai]])
                    k.op("dve", lambda e, pb=pb, ai=ai, n=n, fcg=fcg, ex=ex: e.tensor_scalar(
                        l1[ai][:, 0:n], phl[pb][:, 0:n], bup[:, ex, 8 + fcg:9 + fcg], 7.0, ALU.add, ALU.min),
                        reads=[b_phl[pb], b_c], writes=[b_l1[ai]])
                    k.op("act", lambda e, ai=ai, n=n: e.activation(sg[ai][:, 0:n], g1[ai][:, 0:n], AF.Sigmoid,
                                                                   scale=1.702),
                         reads=[b_g1[ai]], writes=[b_sg[ai]])
                    k.op("pool", lambda e, ai=ai, n=n: e.tensor_scalar(l1[ai][:, 0:n], l1[ai][:, 0:n], -7.0, 1.0,
                                                                       ALU.max, ALU.add),
                         reads=[b_l1[ai]], writes=[b_l1[ai]])
                    k.op("pool", lambda e, ai=ai, n=n: e.tensor_tensor(sg[ai][:, 0:n], sg[ai][:, 0:n], g1[ai][:, 0:n],
                                                                       ALU.mult),
                         reads=[b_sg[ai], b_g1[ai]], writes=[b_sg[ai]])
                    k.op("dve", lambda e, ai=ai, n=n, ab=ab, fc=fc: e.tensor_tensor(
                        aT[ab][:, fc, 0:n], sg[ai][:, 0:n], l1[ai][:, 0:n], ALU.mult),
                        reads=[b_sg[ai], b_l1[ai]], writes=[b_aT[ab]])
                for j in range(n // 128):
                    tl = (c0 // 128) + j
                    yb = cnt_y % 2; cnt_y += 1
                    for hf in range(2):
                        for fc in range(4):
                            k.op("pe", lambda e, hf=hf, fc=fc, yb=yb, j=j, ab=ab: e.matmul(
                                py[yb][:, hf * 512:(hf + 1) * 512], lhsT=aT[ab][:, fc, j * 128:(j + 1) * 128],
                                rhs=wd[slot][:, fc, hf * 512:(hf + 1) * 512], start=(fc == 0), stop=(fc == 3)),
                                reads=[b_aT[ab], b_wd[slot]], writes=[b_py[yb]])
                    k.op("dve", lambda e, yb=yb, tl=tl, ex=ex: e.scalar_tensor_tensor(
                        acc[:, tl, :], py[yb][:], G[:, t0 + tl, ex:ex + 1], acc[:, tl, :], ALU.mult, ALU.add),
                        reads=[b_py[yb], b_G, b_acc[tl]], writes=[b_acc[tl]])
        for tl in range(ntl):
            k.dma("sp", t["FS"][(t0 + tl) * 128:(t0 + tl + 1) * 128, :], acc[:, tl, :], reads=[b_acc[tl]],
                  writes=[b_fs])
    k.flush()


def phase_post_moe(k, P):
    t = P.t
    k.phase()
    b_c = k.buf("t2const")
    g2B = [k.sb(f"g2B{r}", [128, D]) for r in range(2)]
    for r in range(2):
        k.dma("sp", g2B[r][:], mod_bc(P, r, MOD_G2), writes=[b_c])
    lgB = k.sb("ln2gB", [128, D]); lbB = k.sb("ln2bB", [128, D])
    k.dma("sp", lgB[:], bc(t["ln2_g"], [128, D]), writes=[b_c])
    k.dma("sp", lbB[:], bc(t["ln2_b"], [128, D]), writes=[b_c])
    NB = 2
    f = [k.sb(f"t2f{i}", [128, D]) for i in range(NB)]; b_f = k.bufs(NB, "t2f")
    h1 = [k.sb(f"t2h{i}", [128, D]) for i in range(NB)]; b_h1 = k.bufs(NB, "t2h")
    o = [k.sb(f"t2o{i}", [128, D]) for i in range(NB)]; b_o = k.bufs(NB, "t2o")
    lnw = [ln_work(k, f"t2l{i}") for i in range(NB)]
    b_out = k.buf("hout")
    for tt in range(NT_MY):
        i = tt % NB
        r = 1 if tt == 0 else 0
        k.dma("sp", f[i][:], t["FS"][tt * 128:(tt + 1) * 128, :], writes=[b_f[i]])
        k.dma("act", h1[i][:], t["H1"][tt * 128:(tt + 1) * 128, :], writes=[b_h1[i]])
        k.op("pool", lambda e, i=i, r=r: e.tensor_tensor(f[i][:], f[i][:], g2B[r][:], ALU.mult),
             reads=[b_f[i], b_c], writes=[b_f[i]])
        k.op("dve", lambda e, i=i: e.scalar_tensor_tensor(f[i][:], h1[i][:], DN_ALPHA, f[i][:], ALU.mult, ALU.add),
             reads=[b_h1[i], b_f[i]], writes=[b_f[i]])
        emit_ln(k, f[i], b_f[i], o[i], b_o[i], lgB, lbB, b_c, lnw[i])
        k.dma("sp", t["hout"][tt * 128:(tt + 1) * 128, :], o[i][:], reads=[b_o[i]], writes=[b_out])
    k.flush()


SUPERTILES = [(0, 17), (17, 16)]


NCH = NSEQ // 8
NCC = NCTX // 8
NPH = NCH + NCC
NF = NCC + MYLAT // 8
NB_ = NCH
HS_F = 10
HS_B = 11


def declare_s5(P):
    P.din("lam_re", [2, 64, 64]); P.din("lam_im", [2, 64, 64]); P.din("log_step", [2, 64])
    P.din("b_re", [2, 64, 64, 16]); P.din("b_im", [2, 64, 64, 16])
    P.din("c_re", [2, 64, 16, 64]); P.din("c_im", [2, 64, 16, 64])
    P.din("s5_d", [1, D]); P.din("w_glu", [D, 2 * D])
    _pf, P.prefix = P.prefix, ""
    P.din("maskF", [128, 128]); P.din("maskB", [128, 128]); P.din("swapm", [128, 128])
    P.prefix = _pf
    P.dscr("US", [NSEQ, D], BF16)
    P.dscr("YS", [MYTOK, D], BF16)
    P.dscr("S5P", [2, 128, 64 * 128], BF16)
    P.dscr("S5Q", [2, 128, 64 * 128], BF16)
    P.dscr("S5HS", [2, 3, 128, 64 * 11])


def s5_consts(k, P):
    t = P.t
    b_c = k.buf("s5const")
    idt = k.sb("s5_ident", [128, 128]); k.dma("sp", idt[:], t["ident"], writes=[b_c])
    idb = k.sb("s5_identb", [128, 128], BF16); k.dma("pool", idb[:], t["ident"], writes=[b_c])
    swb = k.sb("s5_swapb", [128, 128], BF16); k.dma("pool", swb[:], t["swapm"], writes=[b_c])
    mask = [k.sb("s5_mF", [128, 128]), k.sb("s5_mB", [128, 128])]
    k.dma("sp", mask[0][:], t["maskF"], writes=[b_c]); k.dma("sp", mask[1][:], t["maskB"], writes=[b_c])
    sgn = k.sb("s5_sgn", [128, 1])
    k.op("dve", lambda e: e.memset(sgn[0:64, :], -1.0), writes=[b_c])
    k.op("dve", lambda e: e.memset(sgn[64:128, :], 1.0), writes=[b_c])
    npi = k.sb("s5_npi", [128, 1])
    k.op("dve", lambda e: e.memset(npi[:], -PI), writes=[b_c])
    dcol = k.sb("s5_dcol", [128, 64])
    for tau in range(8):
        k.dma("sp", dcol[tau * 16:(tau + 1) * 16, :], t["s5_d"][0, :].rearrange("(g h) -> h g", h=16),
              writes=[b_c], allow_slow_non_contiguous=True)
    return b_c, idt, idb, swb, mask, sgn, npi, dcol


def phase_s5_setup(k, P, d_out):
    t = P.t
    k.phase()
    d = d_out
    pd = d_out ^ P.sw
    b_c, idt, idb, swb, mask, sgn, npi, dcol = s5_consts(k, P)
    Pbf = {d: k.sb(f"s5_P{d}", [128, 64, 128], BF16)}
    Qbf = {d: k.sb(f"s5_Q{d}", [128, 64, 128], BF16)}
    hsR = {d: k.sb(f"s5_hsR{d}", [128, 64, 11])}
    hsI = {d: k.sb(f"s5_hsI{d}", [128, 64, 11])}
    hsN = {d: k.sb(f"s5_hsN{d}", [128, 64, 11])}
    b_tab = k.buf("s5tab")

    ld = k.sb("s5_ld", [64, 128]); b_ld = k.buf()
    pst = k.ps("s5_pst", [128, 128]); b_pst = k.pbuf()
    cnt = [0]

    def tmp(shape):
        cnt[0] += 1
        return k.sb(f"s5_t{cnt[0]}", shape)

    b_s = k.buf("s5setup")

    def dv(fn):
        k.op("dve", fn, reads=[b_s, b_c], writes=[b_s])

    def cmul(orr, oi, xr, xi, yr, yi, t1, t2):
        dv(lambda e: e.tensor_tensor(t1, xr, yr, ALU.mult))
        dv(lambda e: e.tensor_tensor(t2, xi, yi, ALU.mult))
        dv(lambda e: e.tensor_tensor(t2, t1, t2, ALU.subtract))
        dv(lambda e: e.tensor_tensor(t1, xr, yi, ALU.mult))
        dv(lambda e: e.tensor_tensor(oi, xi, yr, ALU.mult))
        dv(lambda e: e.tensor_tensor(oi, oi, t1, ALU.add))
        dv(lambda e: e.tensor_copy(orr, t2))

    if True:
        lr, li, dt = tmp([128, 64]), tmp([128, 64]), tmp([128, 64])
        for (dst, src) in ((lr, "lam_re"), (li, "lam_im")):
            for hf in range(2):
                k.dma("sp", ld[:, hf * 64:(hf + 1) * 64], t[src][pd], writes=[b_ld])
            k.op("pe", lambda e: e.transpose(pst[:, 0:64], ld[:], idt[0:64, 0:64]), reads=[b_ld, b_c], writes=[b_pst])
            k.op("dve", lambda e, dst=dst: e.tensor_copy(dst[:], pst[:, 0:64]), reads=[b_pst, b_s], writes=[b_s])
        k.dma("sp", dt[:], bc(t["log_step"][pd:pd + 1, :], [128, 64]), writes=[b_s])
        hx, hp = tmp([128, 64]), tmp([128, 64])

        def horner(dst, x, coef):
            dv(lambda e: e.tensor_scalar(dst[:], x[:], float(coef[-1]), float(coef[-2]), ALU.mult, ALU.add))
            for cfv in coef[-3::-1]:
                dv(lambda e: e.tensor_tensor(dst[:], dst[:], x[:], ALU.mult))
                dv(lambda e, cfv=cfv: e.tensor_scalar(dst[:], dst[:], float(cfv), None, ALU.add))

        expc = [1.0 / math.factorial(i) for i in range(11)]
        dv(lambda e: e.tensor_scalar(hx[:], dt[:], 0.125, None, ALU.mult))
        horner(dt, hx, expc)
        for _ in range(3):
            dv(lambda e: e.tensor_tensor(dt[:], dt[:], dt[:], ALU.mult))
        mag, ang, cs, sn = tmp([128, 64]), tmp([128, 64]), tmp([128, 64]), tmp([128, 64])
        dv(lambda e: e.tensor_tensor(hx[:], lr[:], dt[:], ALU.mult))
        horner(mag, hx, expc)
        dv(lambda e: e.tensor_tensor(ang[:], li[:], dt[:], ALU.mult))
        qf, qi = tmp([128, 64]), k.sb("s5_qi", [128, 64], mybir.dt.int32)
        dv(lambda e: e.tensor_scalar(qf[:], ang[:], 1.0 / (2 * PI), None, ALU.mult))
        dv(lambda e: e.tensor_copy(qi[:], qf[:]))
        dv(lambda e: e.tensor_copy(qf[:], qi[:]))
        dv(lambda e: e.scalar_tensor_tensor(ang[:], qf[:], -2 * PI, ang[:], ALU.mult, ALU.add))
        dv(lambda e: e.tensor_scalar(qf[:], ang[:], PI, 2 * PI, ALU.is_gt, ALU.mult))
        dv(lambda e: e.tensor_tensor(ang[:], ang[:], qf[:], ALU.subtract))
        dv(lambda e: e.tensor_scalar(qf[:], ang[:], -PI, 2 * PI, ALU.is_lt, ALU.mult))
        dv(lambda e: e.tensor_tensor(ang[:], ang[:], qf[:], ALU.add))
        dv(lambda e: e.tensor_scalar(ang[:], ang[:], 0.5, None, ALU.mult))
        dv(lambda e: e.tensor_tensor(hx[:], ang[:], ang[:], ALU.mult))
        sinc = [(-1.0) ** i / math.factorial(2 * i + 1) for i in range(7)]
        cosc = [(-1.0) ** i / math.factorial(2 * i) for i in range(7)]
        horner(hp, hx, sinc)
        dv(lambda e: e.tensor_tensor(hp[:], hp[:], ang[:], ALU.mult))
        horner(qf, hx, cosc)
        dv(lambda e: e.scalar_tensor_tensor(sn[:], hp[:], 2.0, qf[:], ALU.mult, ALU.mult))
        dv(lambda e: e.tensor_tensor(cs[:], hp[:], hp[:], ALU.mult))
        dv(lambda e: e.tensor_scalar(cs[:], cs[:], -2.0, 1.0, ALU.mult, ALU.add))
        ar, ai = tmp([128, 64]), tmp([128, 64])
        dv(lambda e: e.tensor_tensor(ar[:], mag[:], cs[:], ALU.mult))
        dv(lambda e: e.tensor_tensor(ai[:], mag[:], sn[:], ALU.mult))
        m2, vr, vi = tmp([128, 64]), tmp([128, 64]), tmp([128, 64])
        dv(lambda e: e.tensor_tensor(m2[:], mag[:], mag[:], ALU.mult))
        dv(lambda e: e.reciprocal(m2[:], m2[:]))
        dv(lambda e: e.tensor_tensor(vr[:], ar[:], m2[:], ALU.mult))
        dv(lambda e: e.scalar_tensor_tensor(vi[:], ai[:], -1.0, m2[:], ALU.mult, ALU.mult))
        den, nr, kr, ki, t1, t2 = (tmp([128, 64]) for _ in range(6))
        dv(lambda e: e.tensor_tensor(den[:], lr[:], lr[:], ALU.mult))
        dv(lambda e: e.tensor_tensor(t1[:], li[:], li[:], ALU.mult))
        dv(lambda e: e.tensor_tensor(den[:], den[:], t1[:], ALU.add))
        dv(lambda e: e.reciprocal(den[:], den[:]))
        dv(lambda e: e.tensor_scalar(nr[:], ar[:], -1.0, None, ALU.add))
        dv(lambda e: e.tensor_tensor(kr[:], nr[:], lr[:], ALU.mult))
        dv(lambda e: e.tensor_tensor(t1[:], ai[:], li[:], ALU.mult))
        dv(lambda e: e.tensor_tensor(kr[:], kr[:], t1[:], ALU.add))
        dv(lambda e: e.tensor_tensor(kr[:], kr[:], den[:], ALU.mult))
        dv(lambda e: e.tensor_tensor(ki[:], ai[:], lr[:], ALU.mult))
        dv(lambda e: e.tensor_tensor(t1[:], nr[:], li[:], ALU.mult))
        dv(lambda e: e.tensor_tensor(ki[:], ki[:], t1[:], ALU.subtract))
        dv(lambda e: e.tensor_tensor(ki[:], ki[:], den[:], ALU.mult))

        def powtab(br_, bi_, desc):
            R, I = tmp([128, 64, 8]), tmp([128, 64, 8])
            s2r, s2i, s4r, s4i = (tmp([128, 64]) for _ in range(4))
            ta, tb = tmp([128, 64, 4]), tmp([128, 64, 4])
            cmul(s2r[:], s2i[:], br_[:], bi_[:], br_[:], bi_[:], ta[:, :, 0], tb[:, :, 0])
            cmul(s4r[:], s4i[:], s2r[:], s2i[:], s2r[:], s2i[:], ta[:, :, 0], tb[:, :, 0])
            i0, i1 = (7, 6) if desc else (0, 1)
            dv(lambda e: e.memset(R[:, :, i0:i0 + 1], 1.0))
            dv(lambda e: e.memset(I[:, :, i0:i0 + 1], 0.0))
            dv(lambda e: e.tensor_copy(R[:, :, i1], br_[:]))
            dv(lambda e: e.tensor_copy(I[:, :, i1], bi_[:]))
            if desc:
                src2, dst2, src4, dst4 = slice(6, 8), slice(4, 6), slice(4, 8), slice(0, 4)
            else:
                src2, dst2, src4, dst4 = slice(0, 2), slice(2, 4), slice(0, 4), slice(4, 8)
            cmul(R[:, :, dst2], I[:, :, dst2], R[:, :, src2], I[:, :, src2],
                 bc(s2r[:].unsqueeze(2), [128, 64, 2]), bc(s2i[:].unsqueeze(2), [128, 64, 2]),
                 ta[:, :, 0:2], tb[:, :, 0:2])
            cmul(R[:, :, dst4], I[:, :, dst4], R[:, :, src4], I[:, :, src4],
                 bc(s4r[:].unsqueeze(2), [128, 64, 4]), bc(s4i[:].unsqueeze(2), [128, 64, 4]),
                 ta[:], tb[:])
            return R, I, s4r, s4i

        desc = (d == 0)
        PR, PI_, a4r, a4i = powtab(ar, ai, desc)
        QR, QI, _, _ = powtab(vr, vi, desc)
        t1w, t2w = tmp([128, 64]), tmp([128, 64])
        cmul(hsR[d][:, :, 0], hsI[d][:, :, 0], a4r[:], a4i[:], a4r[:], a4i[:], t1w[:], t2w[:])
        for lv in range(1, 11):
            cmul(hsR[d][:, :, lv], hsI[d][:, :, lv], hsR[d][:, :, lv - 1], hsI[d][:, :, lv - 1],
                 hsR[d][:, :, lv - 1], hsI[d][:, :, lv - 1], t1w[:], t2w[:])
        dv(lambda e, d=d: e.tensor_scalar(hsI[d][:], hsI[d][:], sgn[:], None, ALU.mult))
        dv(lambda e, d=d: e.tensor_scalar(hsN[d][:], hsI[d][:], -1.0, None, ALU.mult))
        br_, bi_ = tmp([128, 64, 16]), tmp([128, 64, 16])
        for (dst, src) in ((br_, "b_re"), (bi_, "b_im")):
            for hf in range(2):
                k.dma("sp", dst[hf * 64:(hf + 1) * 64], t[src][pd].rearrange("g p h -> p g h"), writes=[b_s])
        bbr, bbi, tq = tmp([128, 64, 16]), tmp([128, 64, 16]), tmp([128, 64, 16])
        krb = bc(kr[:].unsqueeze(2), [128, 64, 16]); kib = bc(ki[:].unsqueeze(2), [128, 64, 16])
        dv(lambda e: e.tensor_tensor(bbr[:], br_[:], krb, ALU.mult))
        dv(lambda e: e.tensor_tensor(tq[:], bi_[:], kib, ALU.mult))
        dv(lambda e: e.tensor_tensor(bbr[:], bbr[:], tq[:], ALU.subtract))
        dv(lambda e: e.tensor_tensor(bbi[:], bi_[:], krb, ALU.mult))
        dv(lambda e: e.tensor_tensor(tq[:], br_[:], kib, ALU.mult))
        dv(lambda e: e.tensor_tensor(bbi[:], bbi[:], tq[:], ALU.add))
        BB1, BB2 = tmp([128, 64, 16]), tmp([128, 64, 16])
        dv(lambda e: e.tensor_copy(BB1[0:64], bbr[0:64]))
        dv(lambda e: e.tensor_copy(BB1[64:128], bbi[64:128]))
        dv(lambda e: e.tensor_copy(BB2[0:64], bbi[0:64]))
        dv(lambda e: e.tensor_copy(BB2[64:128], bbr[64:128]))
        cr, ci = tmp([128, 64, 16]), tmp([128, 64, 16])
        ldc = tmp([128, 128])
        for (dst, src) in ((cr, "c_re"), (ci, "c_im")):
            cv = t[src][pd].rearrange("g h p -> (g h) p")
            for blk in range(8):
                for hf in range(2):
                    k.dma("sp", ldc[:, hf * 64:(hf + 1) * 64], cv[blk * 128:(blk + 1) * 128, :],
                          reads=[b_s], writes=[b_s])
                k.op("pe", lambda e: e.transpose(pst[:], ldc[:], idt[:]), reads=[b_s, b_c], writes=[b_pst])
                k.op("dve", lambda e, dst=dst, blk=blk: e.tensor_copy(
                    dst[:, blk * 8:(blk + 1) * 8, :], pst[:].rearrange("p (g h) -> p g h", h=16)),
                    reads=[b_pst, b_s], writes=[b_s])
        CC1, CC2 = tmp([128, 64, 16]), tmp([128, 64, 16])
        dv(lambda e: e.tensor_copy(CC1[0:64], cr[0:64]))
        dv(lambda e: e.tensor_scalar(CC1[64:128], ci[64:128], -1.0, None, ALU.mult))
        dv(lambda e: e.tensor_scalar(CC2[0:64], ci[0:64], -1.0, None, ALU.mult))
        dv(lambda e: e.tensor_scalar(CC2[64:128], cr[64:128], -1.0, None, ALU.mult))
        PIs = tmp([128, 64, 8])
        dv(lambda e: e.tensor_scalar(PIs[:], PI_[:], sgn[:], None, ALU.mult))
        w1, w2 = tmp([128, 16, 8, 16]), tmp([128, 16, 8, 16])
        for (dstT, XR, XI, M1, M2) in ((Pbf[d], PR, PIs, BB1, BB2), (Qbf[d], QR, QI, CC1, CC2)):
            for g0 in range(0, 64, 16):
                gs = slice(g0, g0 + 16)
                xr = bc(XR[:, gs, :].unsqueeze(3), [128, 16, 8, 16])
                xi = bc(XI[:, gs, :].unsqueeze(3), [128, 16, 8, 16])
                m1 = bc(M1[:, gs, :].unsqueeze(2), [128, 16, 8, 16])
                m2_ = bc(M2[:, gs, :].unsqueeze(2), [128, 16, 8, 16])
                dv(lambda e, xr=xr, m1=m1: e.tensor_tensor(w1[:], xr, m1, ALU.mult))
                dv(lambda e, xi=xi, m2_=m2_: e.tensor_tensor(w2[:], xi, m2_, ALU.mult))
                k.op("dve", lambda e, dstT=dstT, gs=gs: e.tensor_tensor(
                    dstT[:, gs, :].rearrange("p g (t h) -> p g t h", h=16), w1[:], w2[:], ALU.add),
                    reads=[b_s], writes=[b_s, b_tab])

    b_o = k.buf("s5tabscr")
    k.dma("sp", t["S5P"][d], Pbf[d][:].rearrange("p g f -> p (g f)"), reads=[b_s, b_tab], writes=[b_o])
    k.dma("sp", t["S5Q"][d], Qbf[d][:].rearrange("p g f -> p (g f)"), reads=[b_s, b_tab], writes=[b_o])
    for j, tb_ in enumerate((hsR[d], hsI[d], hsN[d])):
        k.dma("sp", t["S5HS"][d, j], tb_[:].rearrange("p g f -> p (g f)"), reads=[b_s, b_tab], writes=[b_o])
    k.flush()


def phase_s5(k, P):
    t = P.t
    k.phase()
    b_c, idt, idb, swb, mask, sgn, npi, dcol = s5_consts(k, P)
    b_tab = k.buf("s5tab")
    Pbf = [k.sb(f"s5_P{d}", [128, 64, 128], BF16) for d in range(2)]
    Qbf = [k.sb(f"s5_Q{d}", [128, 64, 128], BF16) for d in range(2)]
    hsR = [k.sb(f"s5_hsR{d}", [128, 64, 11]) for d in range(2)]
    hsI = [k.sb(f"s5_hsI{d}", [128, 64, 11]) for d in range(2)]
    hsN = [k.sb(f"s5_hsN{d}", [128, 64, 11]) for d in range(2)]
    for d in range(2):
        k.dma("sp", Pbf[d][:].rearrange("p g f -> p (g f)"), t["S5P"][d], writes=[b_tab])
        k.dma("sp", Qbf[d][:].rearrange("p g f -> p (g f)"), t["S5Q"][d], writes=[b_tab])
        for j, tb_ in enumerate((hsR[d], hsI[d], hsN[d])):
            k.dma("sp", tb_[:].rearrange("p g f -> p (g f)"), t["S5HS"][d, j], writes=[b_tab])
    UIN = k.sb("s5_uin", [128, 9, 8, 128], BF16); b_uin = k.buf("s5_uin")
    UG = [k.sb(f"s5_ug{i}", [128, 9, 128], BF16) for i in range(2)]; b_UG = k.bufs(2, "s5_ug")
    UT = [k.sb(f"s5_UT{i}", [128, NPH], BF16) for i in range(2)]; b_UT = k.bufs(2, "s5_UT")
    ptr = [k.ps(f"s5_ptr{i}", [128, 4, 128], BF16) for i in range(2)]; b_ptr = k.pbufs(2, "s5_ptr")
    pgen = k.ps("s5_pgen", [128, 3, 128]); b_pgen = k.pbuf("s5_pgen")
    PT = [k.sb(f"s5_PT{d}", [128, 128], BF16) for d in range(2)]; b_PT = k.bufs(2, "s5_PT")
    PTs = [k.sb(f"s5_PTs{d}", [128, 128], BF16) for d in range(2)]; b_PTs = k.bufs(2, "s5_PTs")
    TM = [k.sb(f"s5_TM{d}", [128, 128], BF16) for d in range(2)]; b_TM = k.bufs(2, "s5_TM")
    tmk = k.sb("s5_tmk", [128, 128]); b_tmk = k.buf()
    px = [k.ps(f"s5_px{i}", [128, 512]) for i in range(2)]; b_px = k.pbufs(2, "s5_px")
    X32 = k.sb("s5_X32", [128, NCH]); b_X32 = k.buf()
    SA = k.sb("s5_SA", [128, NCH]); SB_ = k.sb("s5_SB", [128, NCH]); b_SA, b_SB = k.buf(), k.buf()
    WA = k.sb("s5_WA", [128, NCH]); WB = k.sb("s5_WB", [128, NCH]); b_WA, b_WB = k.buf(), k.buf()
    tS = k.sb("s5_tS", [128, NCH]); tW = k.sb("s5_tW", [128, NCH]); b_tS, b_tW = k.buf(), k.buf()
    tW2 = k.sb("s5_tW2", [128, NCH]); b_tW2 = k.buf()
    SP = [k.sb(f"s5_SP{d}", [128, NCH], BF16) for d in range(2)]; b_SP = k.bufs(2, "s5_SP")
    pyo = k.ps("s5_pyo", [128, 512]); b_pyo = k.pbuf()
    pyc = k.ps("s5_pyc", [128, 16]); b_pyc = k.pbuf()
    ybf = k.sb("s5_ybf", [128, 528], BF16); b_ybf = k.buf()
    pyt = k.ps("s5_pyt", [128, 5, 128], BF16); b_pyt = k.pbuf()
    YO = [k.sb(f"s5_YO{i}", [128, 4, 8, 128], BF16) for i in range(2)]; b_YO = k.bufs(2, "s5_YO")
    YOC = [k.sb(f"s5_YOC{i}", [16, 8, 128], BF16) for i in range(2)]; b_YOC = k.bufs(2, "s5_YOC")
    b_ys = k.buf("YSscr")
    US = t["US"]
    gi = 0
    for gb in range(S5_NGB):
        cs_ = slice(gb * 128, (gb + 1) * 128)
        for blk in range(8):
            r0 = NCTX + blk * 1024
            k.dma("sp", UIN[:, blk], US[r0:r0 + 1024, cs_].rearrange("(c t) ch -> c t ch", t=8), writes=[b_uin])
        k.dma("sp", UIN[0:32, 8], US[0:NCTX, cs_].rearrange("(c t) ch -> c t ch", t=8), writes=[b_uin])
        yo, yoc = YO[gb % 2], YOC[gb % 2]
        for g8 in range(S5_NG8):
            g = gb * 8 + g8
            ut, b_ut = UT[gi % 2], b_UT[gi % 2]
            gi += 1
            hs_ = slice(g8 * 16, (g8 + 1) * 16)
            ug, b_ug = UG[g % 2], b_UG[g % 2]
            k.op("pool", lambda e: e.tensor_copy(ug[:, 0:8, :].rearrange("p a (t h) -> p a t h", h=16),
                                                 UIN[:, 0:8, :, hs_]), reads=[b_uin], writes=[b_ug])
            k.op("pool", lambda e: e.tensor_copy(ug[0:32, 8, :].rearrange("p (t h) -> p t h", h=16),
                                                 UIN[0:32, 8, :, hs_]), reads=[b_uin], writes=[b_ug])
            for q in range(2):
                pp, b_pp = ptr[q], b_ptr[q]
                for j in range(4):
                    blk = q * 4 + j
                    k.op("pe", lambda e, pp=pp, j=j, blk=blk, hs_=hs_: e.transpose(
                        pp[:, j, :], ug[:, blk, :], idb[:]), reads=[b_ug, b_c], writes=[b_pp])
                eng = "act" if q == 0 else "dve"
                if eng == "act":
                    k.op("act", lambda e, pp=pp, q=q, ut=ut: e.copy(
                        ut[:, 32 + q * 512:32 + (q + 1) * 512], pp[:].rearrange("p a b -> p (a b)")),
                        reads=[b_pp], writes=[b_ut])
                else:
                    k.op("dve", lambda e, pp=pp, q=q, ut=ut: e.tensor_copy(
                        ut[:, 32 + q * 512:32 + (q + 1) * 512], pp[:].rearrange("p a b -> p (a b)")),
                        reads=[b_pp], writes=[b_ut])
            pp, b_pp = ptr[0], b_ptr[0]
            k.op("pe", lambda e, pp=pp, hs_=hs_: e.transpose(pp[:, 0, 0:32], ug[0:32, 8, :], idb[0:32, 0:32]),
                 reads=[b_ug, b_c], writes=[b_pp])
            k.op("act", lambda e, pp=pp, ut=ut: e.copy(ut[:, 0:32], pp[:, 0, 0:32]), reads=[b_pp], writes=[b_ut])
            k.op("dve", lambda e, pp=pp, ut=ut: e.tensor_copy(ut[:, NCH:NPH], pp[:, 0, 0:32]), reads=[b_pp],
                 writes=[b_ut])
            if S5_STOP <= 1:
                continue
            for d in range(2):
                c0 = 0 if d == 0 else NCC
                n = NF if d == 0 else NB_
                nlv = HS_F if d == 0 else HS_B
                k.op("pe", lambda e, d=d, g=g: e.matmul(pgen[:, 0, :], lhsT=Pbf[d][:, g, :], rhs=idb[:],
                                                         start=True, stop=True),
                     reads=[b_tab, b_c], writes=[b_pgen])
                k.op("pe", lambda e, d=d, g=g: e.matmul(pgen[:, 1, :], lhsT=Pbf[d][:, g, :], rhs=swb[:],
                                                         start=True, stop=True),
                     reads=[b_tab, b_c], writes=[b_pgen])
                k.op("pe", lambda e, d=d, g=g: e.matmul(pgen[:, 2, :], lhsT=Pbf[d][:, g, :], rhs=Qbf[d][:, g, :],
                                                         start=True, stop=True),
                     reads=[b_tab], writes=[b_pgen])
                if S5_SUB <= 1:
                    continue
                k.op("act", lambda e, d=d: e.copy(PT[d][:], pgen[:, 0, :]), reads=[b_pgen], writes=[b_PT[d]])
                k.op("act", lambda e, d=d: e.copy(PTs[d][:], pgen[:, 1, :]), reads=[b_pgen], writes=[b_PTs[d]])
                if S5_SUB <= 2:
                    continue
                k.op("dve", lambda e, d=d: e.tensor_tensor(tmk[:], pgen[:, 2, :], mask[d][:], ALU.mult),
                     reads=[b_pgen, b_c], writes=[b_tmk])
                if S5_SUB <= 3:
                    continue
                if d == 0:
                    k.op("dve", lambda e, d=d, g=g: e.scalar_tensor_tensor(
                        TM[d][:], idt[:], dcol[:, g:g + 1], tmk[:], ALU.mult, ALU.add),
                        reads=[b_tmk, b_c], writes=[b_TM[d]])
                else:
                    k.op("dve", lambda e, d=d: e.tensor_copy(TM[d][:], tmk[:]), reads=[b_tmk], writes=[b_TM[d]])
                if S5_STOP <= 2:
                    continue
                cols = [(a, min(512, n - a)) for a in range(0, n, 512)]
                for wi, (lhs, b_lhs) in enumerate(((PT[d], b_PT[d]), (PTs[d], b_PTs[d]))):
                    for ci_, (a, w) in enumerate(cols):
                        pb = (wi * len(cols) + ci_) % 2
                        k.op("pe", lambda e, lhs=lhs, a=a, w=w, pb=pb, c0=c0, ut=ut: e.matmul(
                            px[pb][:, 0:w], lhsT=lhs[:], rhs=ut[:, c0 + a:c0 + a + w], start=True, stop=True),
                            reads=[b_lhs, b_ut], writes=[b_px[pb]])
                        if wi == 0:
                            k.op("act", lambda e, a=a, w=w, pb=pb: e.copy(SA[:, a:a + w], px[pb][:, 0:w]),
                                 reads=[b_px[pb]], writes=[b_SA])
                            k.op("dve", lambda e, a=a, w=w, pb=pb: e.tensor_copy(X32[:, a:a + w], px[pb][:, 0:w]),
                                 reads=[b_px[pb]], writes=[b_X32])
                        else:
                            k.op("act", lambda e, a=a, w=w, pb=pb: e.copy(WA[:, a:a + w], px[pb][:, 0:w]),
                                 reads=[b_px[pb]], writes=[b_WA])
                if S5_STOP <= 3:
                    continue
                cur = (SA, b_SA, WA, b_WA)
                nxt = (SB_, b_SB, WB, b_WB)
                for lv in range(nlv):
                    sh = 1 << lv
                    if sh >= n:
                        break
                    So, bSo, Wo, bWo = cur
                    Sn, bSn, Wn, bWn = nxt
                    if d == 0:
                        dst, src, keep = slice(sh, n), slice(0, n - sh), slice(0, sh)
                    else:
                        dst, src, keep = slice(0, n - sh), slice(sh, n), slice(n - sh, n)
                    c1 = hsR[d][:, g, lv:lv + 1]; c2 = hsI[d][:, g, lv:lv + 1]; c2n = hsN[d][:, g, lv:lv + 1]
                    k.op("dve", lambda e, So=So, c1=c1, src=src, dst=dst: e.scalar_tensor_tensor(
                        tS[:, dst], So[:, src], c1, So[:, dst], ALU.mult, ALU.add),
                        reads=[bSo, b_tab], writes=[b_tS])
                    k.op("dve", lambda e, Wo=Wo, Sn=Sn, c2=c2, src=src, dst=dst: e.scalar_tensor_tensor(
                        Sn[:, dst], Wo[:, src], c2, tS[:, dst], ALU.mult, ALU.add),
                        reads=[bWo, b_tS, b_tab], writes=[bSn])
                    k.op("act", lambda e, So=So, Sn=Sn, keep=keep: e.copy(Sn[:, keep], So[:, keep]),
                         reads=[bSo], writes=[bSn])
                    k.op("pool", lambda e, Wo=Wo, c1=c1, src=src, dst=dst: e.tensor_scalar(
                        tW[:, dst], Wo[:, src], c1, None, ALU.mult), reads=[bWo, b_tab], writes=[b_tW])
                    k.op("pool", lambda e, Wo=Wo, dst=dst: e.tensor_tensor(
                        tW[:, dst], tW[:, dst], Wo[:, dst], ALU.add), reads=[bWo, b_tW], writes=[b_tW])
                    k.op("pool", lambda e, So=So, c2n=c2n, src=src, dst=dst: e.tensor_scalar(
                        tW2[:, dst], So[:, src], c2n, None, ALU.mult), reads=[bSo, b_tab], writes=[b_tW2])
                    k.op("pool", lambda e, Wn=Wn, dst=dst: e.tensor_tensor(
                        Wn[:, dst], tW[:, dst], tW2[:, dst], ALU.add), reads=[b_tW, b_tW2], writes=[bWn])
                    k.op("act", lambda e, Wo=Wo, Wn=Wn, keep=keep: e.copy(Wn[:, keep], Wo[:, keep]),
                         reads=[bWo], writes=[bWn])
                    cur, nxt = nxt, cur
                Sf, bSf = cur[0], cur[1]
                k.op("dve", lambda e, Sf=Sf, d=d, n=n: e.tensor_tensor(SP[d][:, 0:n], Sf[:, 0:n], X32[:, 0:n],
                                                                          ALU.subtract),
                     reads=[bSf, b_X32], writes=[b_SP[d]])
            if S5_STOP <= 4:
                continue
            seq = [(TM[0], b_TM[0], ut, b_ut, 32), (Qbf[0][:, g, :], b_tab, SP[0], b_SP[0], 32),
                   (TM[1], b_TM[1], ut, b_ut, 32), (Qbf[1][:, g, :], b_tab, SP[1], b_SP[1], 0)]
            for si, (lhs, b_lhs, rhs, b_rhs, off) in enumerate(seq):
                lh = lhs[:] if si % 2 == 0 else lhs
                k.op("pe", lambda e, lh=lh, rhs=rhs, off=off, si=si: e.matmul(
                    pyo[:], lhsT=lh, rhs=rhs[:, off:off + 512], start=(si == 0), stop=(si == 3)),
                    reads=[b_lhs, b_rhs], writes=[b_pyo])
            seqc = [(TM[0], b_TM[0], ut, b_ut, 0), (Qbf[0][:, g, :], b_tab, SP[0], b_SP[0], 0),
                    (TM[1], b_TM[1], ut, b_ut, NCH), (Qbf[1][:, g, :], b_tab, SP[1], b_SP[1], NCH - NCC)]
            for si, (lhs, b_lhs, rhs, b_rhs, off) in enumerate(seqc):
                lh = lhs[:] if si % 2 == 0 else lhs
                k.op("pe", lambda e, lh=lh, rhs=rhs, off=off, si=si: e.matmul(
                    pyc[:], lhsT=lh, rhs=rhs[:, off:off + 16], start=(si == 0), stop=(si == 3)),
                    reads=[b_lhs, b_rhs], writes=[b_pyc])
            if S5_STOP <= 5:
                continue
            k.op("act", lambda e: e.copy(ybf[:, 0:512], pyo[:]), reads=[b_pyo], writes=[b_ybf])
            k.op("act", lambda e: e.copy(ybf[:, 512:528], pyc[:]), reads=[b_pyc], writes=[b_ybf])
            for j in range(4):
                k.op("pe", lambda e, j=j: e.transpose(pyt[:, j, :], ybf[:, j * 128:(j + 1) * 128], idb[:]),
                     reads=[b_ybf, b_c], writes=[b_pyt])
            k.op("pe", lambda e: e.transpose(pyt[0:16, 4, :], ybf[:, 512:528], idb[:]),
                 reads=[b_ybf, b_c], writes=[b_pyt])
            k.op("dve", lambda e, yo=yo, hs_=hs_: e.tensor_copy(
                yo[:, :, :, hs_], pyt[:, 0:4, :].rearrange("p a (t h) -> p a t h", h=16)),
                reads=[b_pyt], writes=[b_YO[gb % 2]])
            k.op("act", lambda e, yoc=yoc, hs_=hs_: e.copy(
                yoc[:, :, hs_], pyt[0:16, 4, :].rearrange("p (t h) -> p t h", h=16)),
                reads=[b_pyt], writes=[b_YOC[gb % 2]])
        for a_ in range(4):
            k.dma("act", t["YS"][MYCTX + a_ * 1024:MYCTX + (a_ + 1) * 1024, cs_].rearrange("(c t) ch -> c t ch", t=8),
                  yo[:, a_], reads=[b_YO[gb % 2]], writes=[b_ys])
        k.dma("act", t["YS"][0:MYCTX, cs_].rearrange("(c t) ch -> c t ch", t=8), yoc[:],
              reads=[b_YOC[gb % 2]], writes=[b_ys])
    k.flush()


def phase_s5_post(k, P, G, b_G):
    t = P.t
    k.phase()
    pm = PostMixer(k, P, G, b_G)
    b_c = k.buf("s5pconst")
    wg = k.sb("wglu", [128, 8, 2 * D], BF16)
    for kc in range(8):
        k.dma("pool", wg[:, kc, :], t["w_glu"][kc * 128:(kc + 1) * 128, :], writes=[b_c])
    idb = k.sb("s5p_identb", [128, 128], BF16); k.dma("pool", idb[:], t["ident"], writes=[b_c])
    NB = 2
    y = [k.sb(f"gy{i}", [128, D], BF16) for i in range(NB)]; b_y = k.bufs(NB, "gy")
    a = [k.sb(f"ga{i}", [128, D]) for i in range(NB)]; b_a = k.bufs(NB, "ga")
    s = [k.sb(f"gs{i}", [128, D]) for i in range(NB)]; b_s = k.bufs(NB, "gs")
    gl = [k.sb(f"gg{i}", [128, D], BF16) for i in range(NB)]; b_gl = k.bufs(NB, "gg")
    _pT = k.ps("gpT", [128, 8, 128], BF16); _bpT = k.pbuf("gpT")
    pT = [_pT] * NB; b_pT = [_bpT] * NB
    gT = [k.sb(f"ggT{i}", [128, 8, 128], BF16) for i in range(NB)]; b_gT = k.bufs(NB, "ggT")
    pz = k.ps("gpz", [128, 2 * D]); b_pz = k.pbuf("gpz")
    sg = [k.sb(f"gsg{i}", [128, D]) for i in range(NB)]; b_sg = k.bufs(NB, "gsg")
    ym = [k.sb(f"gym{i}", [128, D]) for i in range(NB)]; b_ym = k.bufs(NB, "gym")
    for tt in range(NT_MY):
        i = tt % NB
        pm.prefetch(tt)
        k.dma("sp", y[i][:], t["YS"][tt * 128:(tt + 1) * 128, :], writes=[b_y[i]])
        k.op("pool", lambda e, i=i: e.tensor_tensor(a[i][:], y[i][:], y[i][:], ALU.mult), reads=[b_y[i]],
             writes=[b_a[i]])
        k.op("dve", lambda e, i=i: e.tensor_scalar(a[i][:], a[i][:], 0.044715, 1.0, ALU.mult, ALU.add),
             reads=[b_a[i]], writes=[b_a[i]])
        k.op("pool", lambda e, i=i: e.tensor_tensor(a[i][:], a[i][:], y[i][:], ALU.mult), reads=[b_a[i], b_y[i]],
             writes=[b_a[i]])
        k.op("act", lambda e, i=i: e.activation(s[i][:], a[i][:], AF.Sigmoid, scale=1.5957691216),
             reads=[b_a[i]], writes=[b_s[i]])
        k.op("dve", lambda e, i=i: e.tensor_tensor(gl[i][:], s[i][:], y[i][:], ALU.mult), reads=[b_s[i], b_y[i]],
             writes=[b_gl[i]])
        for kc in range(8):
            k.op("pe", lambda e, i=i, kc=kc: e.transpose(pT[i][:, kc, :], gl[i][:, kc * 128:(kc + 1) * 128], idb[:]),
                 reads=[b_gl[i], b_c], writes=[b_pT[i]])
        k.op("act", lambda e, i=i: e.copy(gT[i][:], pT[i][:]), reads=[b_pT[i]], writes=[b_gT[i]])
        for nb in range(4):
            for kc in range(8):
                k.op("pe", lambda e, i=i, kc=kc, nb=nb: e.matmul(
                    pz[:, nb * 512:(nb + 1) * 512], lhsT=gT[i][:, kc, :], rhs=wg[:, kc, nb * 512:(nb + 1) * 512],
                    start=(kc == 0), stop=(kc == 7)), reads=[b_gT[i], b_c], writes=[b_pz])
        k.op("act", lambda e, i=i: e.activation(sg[i][:], pz[:, D:2 * D], AF.Sigmoid), reads=[b_pz],
             writes=[b_sg[i]])
        k.op("dve", lambda e, i=i: e.tensor_tensor(ym[i][:], pz[:, 0:D], sg[i][:], ALU.mult),
             reads=[b_pz, b_sg[i]], writes=[b_ym[i]])
        pm.emit(tt, ym[i][:], b_ym[i])
    k.flush()


def emit_layer(P, kind):
    with ExitStack() as st:
        k = K(P.nc, st)
        G = k.sb("Gall", [128, NT_MY, NE], glob=True); b_G = k.buf("Gall")
        for v in range(NV):
            P.sw = v % 2
            for nm in ("hseq", "cvec", "hout"):
                P.t[nm] = P.t[f"{nm}_{v}"]
            if kind == "s5":
                phases = [lambda: phase_adaln(k, P), lambda: phase_modulate(k, P, P.t["US"]),
                          lambda: phase_s5_setup(k, P, 0), lambda: phase_s5_setup(k, P, 1), lambda: phase_s5(k, P),
                          lambda: phase_s5_post(k, P, G, b_G), lambda: phase_moe(k, P, G, b_G, SUPERTILES),
                          lambda: phase_post_moe(k, P)]
            else:
                phases = [lambda: phase_adaln(k, P), lambda: phase_modulate(k, P, P.t["US"]),
                          lambda: phase_gla_pass(k, P, 0), lambda: phase_gla_pass(k, P, 1),
                          lambda: phase_gla_post(k, P, G, b_G), lambda: phase_moe(k, P, G, b_G, SUPERTILES),
                          lambda: phase_post_moe(k, P)]
            for ph in phases[:NPHASES]:
                ph()


def build_s5_layer():
    P = Prog("s5")
    declare_io(P)
    declare_common(P)
    declare_s5(P)
    emit_layer(P, "s5")
    return P


NCK = NSEQ // 128
DK = 128
DV = 256
NH = 4


def declare_gla(P):
    P.din("w_in", [D, 3104]); P.din("w_a2", [2, 16, 512]); P.din("b_a2", [2, 512])
    P.din("norm_g", [1, DV]); P.din("w_out", [D, D])
    _pf, P.prefix = P.prefix, ""
    P.din("triF", [128, 128]); P.din("triFs", [128, 128]); P.din("triB", [128, 128]); P.din("triBs", [128, 128])
    P.din("flipm", [128, 128])
    P.prefix = _pf
    P.dscr("US", [NSEQ, D], BF16)
    P.dscr("OS", [2, MYTOK, D])


def chunk_rows(c):
    return c * 128


def phase_gla_pass(k, P, d):
    t = P.t
    k.phase()
    pd = d ^ P.sw
    b_c = k.buf("glaconst"); b_cp = k.buf("glaconstp")
    idb = k.sb("gl_identb", [128, 128], BF16); k.dma("pool", idb[:], t["ident"], writes=[b_cp])
    tri = k.sb("gl_tri", [128, 128]); tris = k.sb("gl_tris", [128, 128])
    k.dma("sp", tri[:], t["triF" if d == 0 else "triB"], writes=[b_c])
    k.dma("sp", tris[:], t["triFs" if d == 0 else "triBs"], writes=[b_c])
    ones = k.sb("gl_ones", [128, 128])
    k.op("dve", lambda e: e.memset(ones[:], 1.0), writes=[b_c])
    win = k.sb("gl_win", [128, 8, 2064], BF16)
    for kc in range(8):
        k.dma("pool", win[:, kc, 0:2048], t["w_in"][kc * 128:(kc + 1) * 128, 0:2048], writes=[b_cp])
        k.dma("pool", win[:, kc, 2048:2064], t["w_in"][kc * 128:(kc + 1) * 128, 3072 + 16 * pd:3088 + 16 * pd],
              writes=[b_cp])
    wa2 = k.sb("gl_wa2", [16, 512]); k.dma("sp", wa2[:], t["w_a2"][pd], writes=[b_c])
    ba2 = k.sb("gl_ba2", [1, 512]); k.dma("sp", ba2[:], t["b_a2"][pd:pd + 1, :], writes=[b_c])
    S32 = k.sb("gl_S32", [128, NH, DV]); b_S32 = k.bufs(NH, "gl_S32")
    Sbf = k.sb("gl_Sbf", [128, NH, DV], BF16); b_Sbf = k.bufs(NH, "gl_Sbf")
    for hd in range(NH):
        k.op("dve", lambda e, hd=hd: e.memset(S32[:, hd, :], 0.0), writes=[b_S32[hd]])
        k.op("pool", lambda e, hd=hd: e.memset(Sbf[:, hd, :], 0.0), writes=[b_Sbf[hd]])
    NB = 2
    u = [k.sb(f"gl_u{i}", [128, D], BF16) for i in range(NB)]; b_u = k.bufs(NB, "gl_u")
    uT = [k.sb(f"gl_uT{i}", [128, 8, 128], BF16) for i in range(NB)]; b_uT = k.bufs(NB, "gl_uT")
    aT = k.sb("gl_aT", [16, 128]); b_aT = k.buf()
    ez = k.sb("gl_ez", [128, 512]); b_ez = k.buf()
    la = k.sb("gl_la", [128, 512]); b_la = k.buf()
    E1 = k.sb("gl_E1", [128, NH, 128]); b_E1 = k.buf()
    E2 = k.sb("gl_E2", [128, NH, 128]); b_E2 = k.buf()
    EK = k.sb("gl_EK", [128, 512]); b_EK = k.buf()
    qd = k.sb("gl_qd", [128, NH, 128], BF16); b_qd = k.buf()
    kd = k.sb("gl_kd", [128, NH, 128], BF16); b_kd = k.buf()
    ke = k.sb("gl_ke", [128, 512], BF16); b_ke = k.buf()
    v = k.sb("gl_v", [128, D], BF16); b_v = k.buf()
    attm = [k.sb(f"gl_attm{i}", [128, 128], BF16) for i in range(2)]; b_attm = k.bufs(2, "gl_attm")
    osb = [k.sb(f"gl_osb{i}", [128, D]) for i in range(2)]; b_osb = k.bufs(2, "gl_osb")
    pA = k.ps("gl_pA", [128, 8, 128], BF16); b_pA = k.pbuf("gl_pA")
    pM = k.ps("gl_pM", [128, 512]); b_pM = k.pbuf("gl_pM")
    pbT = k.ps("gl_pbT", [128, NH, 128]); b_pbT = k.pbuf("gl_pbT")
    pE = k.ps("gl_pE", [128, 512]); b_pE = k.pbuf("gl_pE")
    pq = k.ps("gl_pq", [128, NH, 128]); b_pq = k.pbuf("gl_pq")
    pk = k.ps("gl_pk", [128, NH, 128]); b_pk = k.pbuf("gl_pk")
    pat = pM; b_pat = b_pM
    pV = k.ps("gl_pV", [128, D]); b_pV = k.pbuf("gl_pV")
    b_os = k.buf("OSscr")
    if d == 0:
        order = list(range(0, 2 + 32))
    else:
        order = [1, 0] + list(range(NCK - 1, 1, -1))
    ecol = 127 if d == 0 else 0
    scale = DK ** -0.5
    US = t["US"]

    def load(ci):
        c = order[ci]
        i = ci % NB
        k.dma("sp", u[i][:], US[c * 128:(c + 1) * 128, :], writes=[b_u[i]])

    load(0)
    nmine = 0
    for ci, c in enumerate(order):
        i = ci % NB
        if ci + 1 < len(order):
            load(ci + 1)
        for kc in range(8):
            k.op("pe", lambda e, kc=kc: e.transpose(pA[:, kc, :], u[i][:, kc * 128:(kc + 1) * 128], idb[:]),
                 reads=[b_u[i], b_cp], writes=[b_pA])
        k.op("act", lambda e: e.copy(uT[i][:], pA[:]), reads=[b_pA], writes=[b_uT[i]])
        for kc in range(8):
            k.op("pe", lambda e, kc=kc: e.matmul(pM[0:16, 0:128], lhsT=win[:, kc, 2048:2064], rhs=uT[i][:, kc, :],
                                                 start=(kc == 0), stop=(kc == 7)),
                 reads=[b_cp, b_uT[i]], writes=[b_pM])
        k.op("dve", lambda e: e.tensor_copy(aT[:], pM[0:16, 0:128]), reads=[b_pM], writes=[b_aT])
        k.op("pe", lambda e: e.matmul(pM[:], lhsT=aT[:], rhs=wa2[:], start=True, stop=False),
             reads=[b_aT, b_c], writes=[b_pM])
        k.op("pe", lambda e: e.matmul(pM[:], lhsT=ones[0:1, :], rhs=ba2[:], start=False, stop=True),
             reads=[b_c], writes=[b_pM])
        k.op("act", lambda e: e.activation(ez[:], pM[:], AF.Exp, scale=-1.0), reads=[b_pM], writes=[b_ez])
        k.op("act", lambda e: e.activation(ez[:], ez[:], AF.Ln, bias=ones[:, 0:1], scale=1.0),
             reads=[b_ez, b_c], writes=[b_ez])
        k.op("pool", lambda e: e.tensor_scalar(la[:], ez[:], -1.0 / 16.0, None, ALU.mult), reads=[b_ez],
             writes=[b_la])
        for hd in range(NH):
            k.op("pe", lambda e, hd=hd: e.matmul(pbT[:, hd, :], lhsT=la[:, hd * 128:(hd + 1) * 128], rhs=tri[:],
                                                 start=True, stop=True), reads=[b_la, b_c], writes=[b_pbT])
        k.op("pe", lambda e: e.matmul(pE[:], lhsT=tris[:], rhs=la[:], start=True, stop=True),
             reads=[b_la, b_c], writes=[b_pE])
        k.op("act", lambda e: e.activation(E1[:], pbT[:], AF.Exp), reads=[b_pbT], writes=[b_E1])
        k.op("act", lambda e: e.activation(E2[:], pbT[:], AF.Exp, scale=-1.0), reads=[b_pbT], writes=[b_E2])
        k.op("act", lambda e: e.activation(EK[:], pE[:], AF.Exp), reads=[b_pE], writes=[b_EK])
        for hd in range(NH):
            for kc in range(8):
                k.op("pe", lambda e, hd=hd, kc=kc: e.matmul(pq[:, hd, :], lhsT=win[:, kc, hd * 128:(hd + 1) * 128],
                                                            rhs=uT[i][:, kc, :], start=(kc == 0), stop=(kc == 7)),
                     reads=[b_cp, b_uT[i]], writes=[b_pq])
        k.op("dve", lambda e: e.scalar_tensor_tensor(qd[:], pq[:], scale, E1[:], ALU.mult, ALU.mult),
             reads=[b_pq, b_E1], writes=[b_qd])
        for hd in range(NH):
            for kc in range(8):
                k.op("pe", lambda e, hd=hd, kc=kc: e.matmul(pk[:, hd, :],
                                                            lhsT=win[:, kc, 512 + hd * 128:512 + (hd + 1) * 128],
                                                            rhs=uT[i][:, kc, :], start=(kc == 0), stop=(kc == 7)),
                     reads=[b_cp, b_uT[i]], writes=[b_pk])
        k.op("dve", lambda e: e.tensor_tensor(kd[:], pk[:], E2[:], ALU.mult), reads=[b_pk, b_E2], writes=[b_kd])
        for kc in range(8):
            k.op("pe", lambda e, kc=kc: e.matmul(pE[:], lhsT=uT[i][:, kc, :], rhs=win[:, kc, 512:1024],
                                                 start=(kc == 0), stop=(kc == 7)),
                 reads=[b_cp, b_uT[i]], writes=[b_pE])
        k.op("dve", lambda e: e.tensor_tensor(ke[:], pE[:], EK[:], ALU.mult), reads=[b_pE, b_EK], writes=[b_ke])
        for hf in range(2):
            for kc in range(8):
                k.op("pe", lambda e, kc=kc, hf=hf: e.matmul(pV[:, hf * 512:(hf + 1) * 512], lhsT=uT[i][:, kc, :],
                                                            rhs=win[:, kc, 1024 + hf * 512:1024 + (hf + 1) * 512],
                                                            start=(kc == 0), stop=(kc == 7)),
                     reads=[b_cp, b_uT[i]], writes=[b_pV])
        k.op("act", lambda e: e.copy(v[:], pV[:]), reads=[b_pV], writes=[b_v])
        mine = (c == 0) or (2 <= c < 2 + 32)
        for hd in range(NH):
            am, b_am = attm[hd % 2], b_attm[hd % 2]
            k.op("pe", lambda e, hd=hd: e.matmul(pat[:, 0:128], lhsT=kd[:, hd, :], rhs=qd[:, hd, :],
                                                 start=True, stop=True), reads=[b_kd, b_qd], writes=[b_pat])
            k.op("dve", lambda e, am=am: e.tensor_tensor(am[:], pat[:, 0:128], tri[:], ALU.mult),
                 reads=[b_pat, b_c], writes=[b_am])
            if mine:
                k.op("pe", lambda e, hd=hd, am=am: e.matmul(pV[:, hd * DV:(hd + 1) * DV], lhsT=am[:],
                                                            rhs=v[:, hd * DV:(hd + 1) * DV], start=True, stop=False),
                     reads=[b_am, b_v], writes=[b_pV])
                k.op("pe", lambda e, hd=hd: e.matmul(pV[:, hd * DV:(hd + 1) * DV], lhsT=qd[:, hd, :],
                                                     rhs=Sbf[:, hd, :], start=False, stop=True),
                     reads=[b_qd, b_Sbf[hd]], writes=[b_pV])
            k.op("pe", lambda e, hd=hd: e.matmul(pat[:, 0:DV], lhsT=ke[:, hd * 128:(hd + 1) * 128],
                                                 rhs=v[:, hd * DV:(hd + 1) * DV], start=True, stop=True),
                 reads=[b_ke, b_v], writes=[b_pat])
            k.op("dve", lambda e, hd=hd: e.scalar_tensor_tensor(S32[:, hd, :], S32[:, hd, :],
                                                                E1[:, hd, ecol:ecol + 1], pat[:, 0:DV],
                                                                ALU.mult, ALU.add),
                 reads=[b_S32[hd], b_E1, b_pat], writes=[b_S32[hd]])
            k.op("pool", lambda e, hd=hd: e.tensor_copy(Sbf[:, hd, :], S32[:, hd, :]), reads=[b_S32[hd]],
                 writes=[b_Sbf[hd]])
        if mine:
            ob, b_ob = osb[nmine % 2], b_osb[nmine % 2]
            nmine += 1
            k.op("act", lambda e, ob=ob: e.copy(ob[:], pV[:]), reads=[b_pV], writes=[b_ob])
            row = 0 if c == 0 else MYCTX + (c - 2) * 128
            k.dma("sp", t["OS"][d, row:row + 128, :], ob[:], reads=[b_ob], writes=[b_os])
    k.flush()


def phase_gla_post(k, P, G, b_G):
    t = P.t
    k.phase()
    pm = PostMixer(k, P, G, b_G)
    b_c = k.buf("glpconst"); b_cp = k.buf("glpconstp")
    idb = k.sb("glp_identb", [128, 128], BF16); k.dma("pool", idb[:], t["ident"], writes=[b_cp])
    wg = k.sb("glp_wg", [128, 8, D], BF16)
    wo = k.sb("glp_wo", [128, 8, D], BF16)
    for kc in range(8):
        k.dma("pool", wg[:, kc, :], t["w_in"][kc * 128:(kc + 1) * 128, 2048:3072], writes=[b_cp])
        k.dma("pool", wo[:, kc, :], t["w_out"][kc * 128:(kc + 1) * 128, :], writes=[b_cp])
    ngB = k.sb("glp_ng", [128, DV]); k.dma("sp", ngB[:], bc(t["norm_g"], [128, DV]), writes=[b_c])
    epsc = k.sb("glp_eps", [128, 1]); k.op("dve", lambda e: e.memset(epsc[:], LN_EPS), writes=[b_c])
    NB = 2
    u = [k.sb(f"glp_u{i}", [128, D], BF16) for i in range(NB)]; b_u = k.bufs(NB, "glp_u")
    of = [k.sb(f"glp_of{i}", [128, D]) for i in range(NB)]; b_of = k.bufs(NB, "glp_of")
    ob = [k.sb(f"glp_ob{i}", [128, D]) for i in range(NB)]; b_ob = k.bufs(NB, "glp_ob")
    sq = k.sb("glp_sq", [128, D]); b_sq = k.buf()
    ms = k.sb("glp_ms", [128, NH]); b_ms = k.buf()
    sg = k.sb("glp_sg", [128, D]); b_sg = k.buf()
    zb = k.sb("glp_zb", [128, D], BF16); b_zb = k.buf()
    pT = k.ps("glp_pT", [128, 8, 128], BF16); b_pT = k.pbuf("glp_pT")
    xT = k.sb("glp_xT", [128, 8, 128], BF16); b_xT = k.buf()
    pg = k.ps("glp_pg", [128, D]); b_pg = k.pbuf("glp_pg")
    ym = [k.sb(f"glp_ym{i}", [128, D]) for i in range(NB)]; b_ym = k.bufs(NB, "glp_ym")
    for tt in range(NT_MY):
        i = tt % NB
        pm.prefetch(tt)
        r0, r1 = my_rows(tt)
        k.dma("sp", u[i][:], t["US"][r0:r1, :], writes=[b_u[i]])
        k.dma("act", of[i][:], t["OS"][0, tt * 128:(tt + 1) * 128, :], writes=[b_of[i]])
        k.dma("act", ob[i][:], t["OS"][1, tt * 128:(tt + 1) * 128, :], writes=[b_ob[i]])
        for kc in range(8):
            k.op("pe", lambda e, kc=kc: e.transpose(pT[:, kc, :], u[i][:, kc * 128:(kc + 1) * 128], idb[:]),
                 reads=[b_u[i], b_cp], writes=[b_pT])
        k.op("act", lambda e: e.copy(xT[:], pT[:]), reads=[b_pT], writes=[b_xT])
        for hf in range(2):
            for kc in range(8):
                k.op("pe", lambda e, kc=kc, hf=hf: e.matmul(pg[:, hf * 512:(hf + 1) * 512], lhsT=xT[:, kc, :],
                                                            rhs=wg[:, kc, hf * 512:(hf + 1) * 512],
                                                            start=(kc == 0), stop=(kc == 7)),
                     reads=[b_xT, b_cp], writes=[b_pg])
        k.op("act", lambda e: e.activation(sg[:], pg[:], AF.Silu), reads=[b_pg], writes=[b_sg])
        k.op("pool", lambda e: e.tensor_tensor(of[i][:], of[i][:], ob[i][:], ALU.add), reads=[b_of[i], b_ob[i]],
             writes=[b_of[i]])
        k.op("dve", lambda e: e.tensor_tensor(sq[:], of[i][:], of[i][:], ALU.mult), reads=[b_of[i]], writes=[b_sq])
        k.op("dve", lambda e: e.reduce_sum(ms[:], sq[:].rearrange("p (h e) -> p h e", e=DV),
                                           axis=mybir.AxisListType.X), reads=[b_sq], writes=[b_ms])
        k.op("act", lambda e: e.activation(ms[:], ms[:], AF.Sqrt, bias=epsc[:], scale=1.0 / DV),
             reads=[b_ms, b_c], writes=[b_ms])
        k.op("dve", lambda e: e.reciprocal(ms[:], ms[:]), reads=[b_ms], writes=[b_ms])
        k.op("dve", lambda e: e.tensor_tensor(sq[:].rearrange("p (h e) -> p h e", e=DV),
                                              of[i][:].rearrange("p (h e) -> p h e", e=DV),
                                              bc(ms[:].unsqueeze(2), [128, NH, DV]), ALU.mult),
             reads=[b_of[i], b_ms], writes=[b_sq])
        k.op("pool", lambda e: e.tensor_tensor(sq[:].rearrange("p (h e) -> p h e", e=DV),
                                               sq[:].rearrange("p (h e) -> p h e", e=DV),
                                               bc(ngB[:].unsqueeze(1), [128, NH, DV]), ALU.mult),
             reads=[b_sq, b_c], writes=[b_sq])
        k.op("dve", lambda e: e.tensor_tensor(zb[:], sq[:], sg[:], ALU.mult), reads=[b_sq, b_sg], writes=[b_zb])
        for kc in range(8):
            k.op("pe", lambda e, kc=kc: e.transpose(pT[:, kc, :], zb[:, kc * 128:(kc + 1) * 128], idb[:]),
                 reads=[b_zb, b_cp], writes=[b_pT])
        k.op("act", lambda e: e.copy(xT[:], pT[:]), reads=[b_pT], writes=[b_xT])
        for hf in range(2):
            for kc in range(8):
                k.op("pe", lambda e, kc=kc, hf=hf: e.matmul(pg[:, hf * 512:(hf + 1) * 512], lhsT=xT[:, kc, :],
                                                            rhs=wo[:, kc, hf * 512:(hf + 1) * 512],
                                                            start=(kc == 0), stop=(kc == 7)),
                     reads=[b_xT, b_cp], writes=[b_pg])
        k.op("act", lambda e: e.copy(ym[i][:], pg[:]), reads=[b_pg], writes=[b_ym[i]])
        pm.emit(tt, ym[i][:], b_ym[i])
    k.flush()


def build_gla_layer():
    P = Prog("gla")
    declare_io(P)
    declare_common(P)
    declare_gla(P)
    emit_layer(P, "gla")
    return P


def gla_inputs(inp, j, s):
    c = consts()
    w_in = inp["gla_w_in"][j]
    w_a2, b_a2 = inp["gla_w_a2"][j], inp["gla_b_a2"][j]
    if s == 1:
        w_in = np.concatenate([w_in[:, :3072], w_in[:, 3088:3104], w_in[:, 3072:3088]], axis=1)
        w_a2, b_a2 = w_a2[::-1], b_a2[::-1]
    return {"w_in": w_in, "w_a2": w_a2, "b_a2": b_a2, "norm_g": inp["gla_norm_g"][j][None],
            "w_out": inp["gla_w_out"][j], "triF": c["triF"], "triFs": c["triFs"], "triB": c["triB"],
            "triBs": c["triBs"]}


def phase_handoff(k, P, kind_i, kind_n, ho0, ho1, hs0, hs1):
    t = P.t
    k.phase()
    b_c = k.buf("hoconst")
    flip = k.sb("ho_flip", [128, 128]); k.dma("sp", flip[:], t["flipm"], writes=[b_c])
    NB = 3
    a = [k.sb(f"ho_a{i}", [128, D]) for i in range(NB)]; b_a = k.bufs(NB, "ho_a")
    f = [k.sb(f"ho_f{i}", [128, D]) for i in range(NB)]; b_f = k.bufs(NB, "ho_f")
    pf = [k.ps(f"ho_p{i}", [128, D]) for i in range(2)]; b_pf = k.pbufs(2, "ho_p")
    NAT, CN = t["NAT"], t["CNAT"]
    b_nat = k.buf("NAT")
    cnt = [0]

    def nat_tile(kind, j):
        if kind == "s5":
            return NAT[j * 128:(j + 1) * 128, :]
        return NAT.rearrange("(r w) d -> w r d", w=64)[j]

    def move(dst, src, do_flip, b_dst):
        i = cnt[0] % NB
        cnt[0] += 1
        k.dma("sp", a[i][:], src, reads=[b_nat], writes=[b_a[i]])
        if not do_flip:
            k.dma("act", dst, a[i][:], reads=[b_a[i]], writes=[b_dst])
            return
        pb = cnt[0] % 2
        for hf in range(2):
            k.op("pe", lambda e, hf=hf: e.matmul(pf[pb][:, hf * 512:(hf + 1) * 512], lhsT=flip[:],
                                                 rhs=a[i][:, hf * 512:(hf + 1) * 512], start=True, stop=True),
                 reads=[b_a[i], b_c], writes=[b_pf[pb]])
        k.op("act", lambda e: e.copy(f[i][:], pf[pb][:]), reads=[b_pf[pb]], writes=[b_f[i]])
        k.dma("act", dst, f[i][:], reads=[b_f[i]], writes=[b_dst])

    move(CN[0:128, :], ho0[0:128, :], False, b_nat)
    move(CN[128:256, :], ho1[0:128, :], True, b_nat)
    for j in range(32):
        move(nat_tile(kind_i, j), ho0[MYCTX + j * 128:MYCTX + (j + 1) * 128, :], False, b_nat)
        move(nat_tile(kind_i, 32 + j), ho1[MYCTX + (31 - j) * 128:MYCTX + (32 - j) * 128, :], True, b_nat)
    b_hs = k.buf("HSnext")
    for c in range(2):
        move(hs0[c * 128:(c + 1) * 128, :], CN[c * 128:(c + 1) * 128, :], False, b_hs)
        move(hs1[(1 - c) * 128:(2 - c) * 128, :], CN[c * 128:(c + 1) * 128, :], True, b_hs)
    for j in range(64):
        move(hs0[NCTX + j * 128:NCTX + (j + 1) * 128, :], nat_tile(kind_n, j), False, b_hs)
        move(hs1[NCTX + (63 - j) * 128:NCTX + (64 - j) * 128, :], nat_tile(kind_n, j), True, b_hs)
    k.flush()


def build_fused():
    P = Prog("fused")
    declare_io(P)
    P.dscr("NAT", [NLAT, D]); P.dscr("CNAT", [NCTX, D])
    for v in range(NV):
        for nm in ("HSA", "HSB"):
            P.dscr(f"{nm}_{v}", [NSEQ, D])
        P.dscr(f"HO_{v}", [MYTOK, D])
    kinds = ["s5", "gla", "s5", "gla"]
    for i, kind in enumerate(kinds):
        P.prefix = f"L{i}_"
        declare_common(P)
        (declare_s5 if kind == "s5" else declare_gla)(P)
    layer_t = {}
    with ExitStack() as st:
        k = K(P.nc, st)
        G = k.sb("Gall", [128, NT_MY, NE], glob=True); b_G = k.buf("Gall")
        for i, kind in enumerate(kinds):
            for full, ap in P.ext.items():
                if full.startswith(f"L{i}_"):
                    P.t[full[len(f"L{i}_"):]] = ap
            for v in range(NV):
                P.sw = v % 2
                P.t["cvec"] = P.ext[f"cvec_{v}"]
                if i == 0:
                    P.t["hseq"] = P.ext[f"hseq_{v}"]
                else:
                    P.t["hseq"] = P.t[f"{'HSA' if i % 2 == 1 else 'HSB'}_{v}"]
                P.t["hout"] = P.ext[f"hout_{v}"] if i == 3 else P.t[f"HO_{v}"]
                if kind == "s5":
                    phases = [lambda: phase_adaln(k, P), lambda: phase_modulate(k, P, P.t["US"]),
                              lambda: phase_s5_setup(k, P, 0), lambda: phase_s5_setup(k, P, 1),
                              lambda: phase_s5(k, P), lambda: phase_s5_post(k, P, G, b_G),
                              lambda: phase_moe(k, P, G, b_G, SUPERTILES), lambda: phase_post_moe(k, P)]
                else:
                    phases = [lambda: phase_adaln(k, P), lambda: phase_modulate(k, P, P.t["US"]),
                              lambda: phase_gla_pass(k, P, 0), lambda: phase_gla_pass(k, P, 1),
                              lambda: phase_gla_post(k, P, G, b_G),
                              lambda: phase_moe(k, P, G, b_G, SUPERTILES), lambda: phase_post_moe(k, P)]
                for ph in phases:
                    ph()
            if i < 3:
                nxt = "HSA" if (i + 1) % 2 == 1 else "HSB"
                for bb in range(NV // 2):
                    phase_handoff(k, P, kind, kinds[i + 1], P.t[f"HO_{2 * bb}"], P.t[f"HO_{2 * bb + 1}"],
                                  P.t[f"{nxt}_{2 * bb}"], P.t[f"{nxt}_{2 * bb + 1}"])
    return P


_FUSED = []


def run_fused(inp):
    if not _FUSED:
        _FUSED.append(build_fused())
    P = _FUSED[0]
    h, hc = inp["x"], inp["ctx"]
    allv = [(b, s) for b in range(4) for s in range(2)]
    groups = [allv[p * NV:(p + 1) * NV] for p in range(NPHYS)]
    maps = []
    for grp in groups:
        m = dict(consts())
        for v, (b, s) in enumerate(grp):
            lat, ctx = h[b], hc[b]
            if s == 1:
                lat, ctx = lat[::-1], ctx[::-1]
            m[f"hseq_{v}"] = np.concatenate([ctx, lat], axis=0)
            m[f"cvec_{v}"] = np.stack([inp["c"][b], inp["c_ctx"]])
        for i in range(4):
            lw = common_inputs(inp, i, 0)
            lw.update(s5_inputs(inp, i // 2, 0) if i % 2 == 0 else gla_inputs(inp, i // 2, 0))
            for kk, vv in lw.items():
                if kk not in m:
                    m[f"L{i}_{kk}"] = vv
        ins = [n for n in P.ext if not n.startswith("hout_")]
        maps.append({n: np.ascontiguousarray(m[n], dtype=np.float32) for n in ins})
    res = run_bass_kernel_spmd(P.nc, maps, core_ids=list(range(NPHYS)))
    hn = np.empty_like(h)
    for p, grp in enumerate(groups):
        for v, (b, s) in enumerate(grp):
            o = res.results[p][f"hout_{v}"][MYCTX:]
            if s == 0:
                lat0 = o
            else:
                hn[b] = seq_unorder("gla", np.concatenate([lat0, o[::-1]], axis=0))
    return hn


DEBUG = False
_CONST = {}


def consts():
    if not _CONST:
        idx = np.arange(128)
        tau = idx // 16
        _CONST["ident"] = np.eye(128, dtype=np.float32)
        _CONST["maskF"] = (tau[None, :] >= tau[:, None]).astype(np.float32)
        _CONST["maskB"] = (tau[:, None] >= tau[None, :]).astype(np.float32)
        sw = np.zeros((128, 128), np.float32)
        sw[idx, (idx + 64) % 128] = 1.0
        _CONST["swapm"] = sw
        _CONST["flipm"] = np.ascontiguousarray(np.eye(128, dtype=np.float32)[::-1])
        jj, ii = idx[:, None], idx[None, :]
        _CONST["triF"] = (jj <= ii).astype(np.float32)
        _CONST["triFs"] = (jj > ii).astype(np.float32)
        _CONST["triB"] = (jj >= ii).astype(np.float32)
        _CONST["triBs"] = (jj < ii).astype(np.float32)
    return _CONST


def seq_order(kind, hb):
    if kind == "s5":
        return hb
    return hb.reshape(128, 64, D).transpose(1, 0, 2).reshape(NLAT, D)


def seq_unorder(kind, lat):
    if kind == "s5":
        return lat
    return lat.reshape(64, 128, D).transpose(1, 0, 2).reshape(NLAT, D)


def common_inputs(inp, i, b):
    c = consts()
    m = {
        "cvec": np.ascontiguousarray(np.stack([inp["c"][b], inp["c_ctx"]])),
        "w_ada": inp["w_ada"][i], "b_ada": inp["b_ada"][i][None],
        "ln1_g": inp["ln1_g"][i][None], "ln1_b": inp["ln1_b"][i][None],
        "ln2_g": inp["ln2_g"][i][None], "ln2_b": inp["ln2_b"][i][None],
        "w_router": inp["moe_w_router"][i], "b_router": inp["moe_b_router"][i][None],
        "w_up": inp["moe_w_up"][i], "b_up": inp["moe_b_up"][i],
        "w_down": inp["moe_w_down"][i], "b_down": inp["moe_b_down"][i],
        "ident": c["ident"],
    }
    return m


def s5_inputs(inp, j, s):
    c = consts()
    sl = slice(None) if s == 0 else slice(None, None, -1)
    f = lambda a: np.ascontiguousarray(a[j][sl])
    return {
        "lam_re": f(inp["s5_lam_re"]), "lam_im": f(inp["s5_lam_im"]), "log_step": f(inp["s5_log_step"]),
        "b_re": f(inp["s5_b_re"]), "b_im": f(inp["s5_b_im"]), "c_re": f(inp["s5_c_re"]), "c_im": f(inp["s5_c_im"]),
        "s5_d": inp["s5_d"][j][None], "w_glu": inp["s5_w_glu"][j],
        "maskF": c["maskF"], "maskB": c["maskB"], "swapm": c["swapm"],
    }


_PROGS = {}


def get_prog(kind):
    if kind not in _PROGS:
        _PROGS[kind] = build_s5_layer() if kind == "s5" else build_gla_layer()
    return _PROGS[kind]


def run_layer(inp, i, h, hc, cores=None):
    kind = "s5" if i % 2 == 0 else "gla"
    P = get_prog(kind)
    allv = [(b, s) for b in range(4) for s in range(2)]
    groups = [allv[p * NV:(p + 1) * NV] for p in range(NPHYS)]
    maps = []
    for grp in groups:
        m = common_inputs(inp, i, 0)
        m.pop("cvec")
        for v, (b, s) in enumerate(grp):
            lat = seq_order(kind, h[b])
            ctx = hc[b]
            if s == 1:
                lat, ctx = lat[::-1], ctx[::-1]
            m[f"hseq_{v}"] = np.concatenate([ctx, lat], axis=0)
            m[f"cvec_{v}"] = np.stack([inp["c"][b], inp["c_ctx"]])
        m.update(s5_inputs(inp, i // 2, 0) if kind == "s5" else gla_inputs(inp, i // 2, 0))
        maps.append({kk: np.ascontiguousarray(vv, dtype=np.float32) for kk, vv in m.items() if kk in P.t})
    res = run_bass_kernel_spmd(P.nc, maps, core_ids=list(range(NPHYS)))
    hn = np.empty_like(h)
    hcn = np.empty_like(hc)
    lat_new = {}
    for p, grp in enumerate(groups):
        for v, (b, s) in enumerate(grp):
            o = res.results[p][f"hout_{v}"]
            if s == 0:
                hcn[b, 0:128] = o[0:128]
                lat_new[(b, 0)] = o[128:]
            else:
                hcn[b, 128:256] = o[0:128][::-1]
                lat_new[(b, 1)] = o[128:][::-1]
    for b in range(4):
        hn[b] = seq_unorder(kind, np.concatenate([lat_new[(b, 0)], lat_new[(b, 1)]], axis=0))
    return hn, hcn, res


def kernel(**inp):
    inp = {kk: np.asarray(v, dtype=np.float32) for kk, v in inp.items()}
    return run_fused(inp)
```

```python
from contextlib import ExitStack
import math
import numpy as np
import concourse.bass as bass
import concourse.mybir as mybir
from concourse.bass_utils import run_bass_kernel_spmd

F32 = mybir.dt.float32
BF16 = mybir.dt.bfloat16
ALU = mybir.AluOpType
AF = mybir.ActivationFunctionType

D = 1024
NCTX = 256
NLAT = 8192
NSEQ = NCTX + NLAT
MYCTX = 128
MYLAT = 4096
MYTOK = MYCTX + MYLAT
NT_MY = MYTOK // 128
NT_SEQ = NSEQ // 128
NE = 32
DN_ALPHA = 8 ** 0.25
LN_EPS = 1e-5
PI = math.pi

ENGS = ("pe", "act", "dve", "pool", "sp")
DEBUG_SCR = False
NV = 2
NPHYS = 4
NPHASES = 100
S5_NGB = 8
S5_NG8 = 8
S5_STOP = 99
S5_SUB = 99


class Buf:
    __slots__ = ("name", "lw", "rd", "dsem", "psem", "excl")

    def __init__(self, name, excl=False):
        self.name = name
        self.excl = excl
        self.lw = None
        self.rd = []
        self.dsem = None
        self.psem = None


class _Rec:
    def __getattr__(self, name):
        def call(*a, **kw):
            self.rec = (name, a, kw)
            return self
        return call


class K:
    def __init__(self, nc, stack):
        self.nc = nc
        self.gstack = stack
        self.sems = {}
        self.cnt = {}
        for e in ENGS:
            self._mksem("E_" + e)
        self.free_d = []
        self.free_p = []
        self.nd = 0
        self.nbuf = 0
        self._reset()

    def _reset(self):
        self.prog = {e: [] for e in ENGS}
        self.seen = {e: dict(self.cnt) for e in ENGS}
        self.dbufs = []
        self.pbufs_ = []

    def _mksem(self, key):
        self.sems[key] = self.gstack.enter_context(self.nc.semaphore(key))
        self.cnt[key] = 0
        return key

    def buf(self, name=None, excl=False):
        self.nbuf += 1
        return Buf(name or f"b{self.nbuf}", excl)

    def bufs(self, n, name="b", excl=False):
        return [self.buf(f"{name}{i}", excl) for i in range(n)]

    def pbuf(self, name=None):
        return self.buf(name, True)

    def pbufs(self, n, name="p"):
        return self.bufs(n, name, True)

    def phase(self):
        self.pstack = ExitStack()
        return self.pstack

    def sb(self, name, shape, dt=F32, glob=False):
        st = self.gstack if glob else self.pstack
        self.nbuf += 1
        return st.enter_context(self.nc.sbuf_tensor(f"{name}_{self.nbuf}", list(shape), dt))

    def ps(self, name, shape, dt=F32):
        self.nbuf += 1
        esz = 4 if dt == F32 else 2
        n = 1
        for d_ in shape[1:]:
            n *= d_
        per_bank = 2048 // esz
        npad = ((n + per_bank - 1) // per_bank) * per_bank
        tt = self.pstack.enter_context(self.nc.psum_tensor(f"{name}_{self.nbuf}", [128, npad], dt))
        v = tt[0:shape[0], 0:n]
        if len(shape) == 3:
            v = v.rearrange("p (a b) -> p a b", b=shape[2])
        elif len(shape) == 4:
            v = v.rearrange("p (a b c) -> p a b c", b=shape[2], c=shape[3])
        return v

    def _deps(self, eng, reads, writes):
        waits = {}

        def need(ev):
            if ev is not None and waits.get(ev[0], 0) < ev[1]:
                waits[ev[0]] = ev[1]
        for b in reads:
            need(b.lw)
        for b in writes:
            need(b.lw)
            for r in b.rd:
                need(r)
        out = {}
        seen = self.seen[eng]
        for kk, v in waits.items():
            if eng == "pe" and kk == "E_pe":
                continue
            if seen.get(kk, 0) >= v:
                continue
            seen[kk] = v
            out[kk] = v
        return out

    def _commit(self, ev, reads, writes):
        for b in reads:
            b.rd.append(ev)
            if len(b.rd) > 24:
                m = {}
                for kk, v in b.rd:
                    if m.get(kk, 0) < v:
                        m[kk] = v
                b.rd = list(m.items())
        for b in writes:
            b.lw = ev
            b.rd = []

    def op(self, eng, fn, reads=(), writes=()):
        ex = [b for b in reads if b.excl]
        if ex:
            writes = list(writes) + ex
        waits = self._deps(eng, reads, writes)
        key = "E_" + eng
        self.cnt[key] += 1
        ev = (key, self.cnt[key])
        sems = self.sems
        rec = _Rec()
        fn(rec)
        name, a, kw = rec.rec

        def run(e, waits=waits, name=name, a=a, kw=kw, sem=sems[key]):
            for kk, v in waits.items():
                e.wait_ge(sems[kk], v)
            getattr(e, name)(*a, **kw).then_inc(sem, 1)
        self.prog[eng].append(run)
        self._commit(ev, reads, writes)

    def dma(self, eng, out, in_, reads=(), writes=(), **kw):
        waits = self._deps(eng, reads, writes)
        b = writes[0]
        if eng == "pool":
            if getattr(b, "psem", None) is None:
                if self.free_p:
                    b.psem = self.free_p.pop()
                else:
                    self.nd += 1
                    b.psem = self._mksem(f"DP_{self.nd}")
                self.pbufs_.append(b)
            key = b.psem
        else:
            if b.dsem is None:
                if self.free_d:
                    b.dsem = self.free_d.pop()
                else:
                    self.nd += 1
                    b.dsem = self._mksem(f"D_{self.nd}")
                self.dbufs.append(b)
            key = b.dsem
        self.cnt[key] += 16
        ev = (key, self.cnt[key])
        sems = self.sems

        def run(e, waits=waits, sem=sems[key]):
            for kk, v in waits.items():
                e.wait_ge(sems[kk], v)
            e.dma_start(out=out, in_=in_, **kw).then_inc(sem, 16)
        self.prog[eng].append(run)
        self._commit(ev, reads, writes)

    def flush(self):
        final = {kk: v for kk, v in self.cnt.items() if v > 0}
        sems = self.sems

        def bar(e, final=final):
            for kk, v in final.items():
                e.wait_ge(sems[kk], v)
        prog = self.prog
        for eng in ENGS:
            prog[eng].append(bar)
        nc = self.nc
        with nc.Block() as block:
            @block.sync
            def _(e):
                for f in prog["sp"]:
                    f(e)

            @block.tensor
            def _(e):
                for f in prog["pe"]:
                    f(e)

            @block.vector
            def _(e):
                for f in prog["dve"]:
                    f(e)

            @block.scalar
            def _(e):
                for f in prog["act"]:
                    f(e)

            @block.gpsimd
            def _(e):
                for f in prog["pool"]:
                    f(e)
        for b in self.dbufs:
            self.free_d.append(b.dsem)
            b.dsem = None
        for b in self.pbufs_:
            self.free_p.append(b.psem)
            b.psem = None
        self._reset()
        self.pstack.close()


def bc(ap, shape):
    return ap.broadcast_to(list(shape))


class Prog:
    def __init__(self, kind):
        self.kind = kind
        self.nc = bass.Bass("TRN2", target_bir_lowering=False)
        self.t = {}
        self.ext = {}
        self.scr = set()
        self.prefix = ""
        self.sw = 0

    def din(self, name, shape, dt=F32):
        full = self.prefix + name
        if full not in self.ext:
            self.ext[full] = self.nc.dram_tensor(full, list(shape), dt, kind="ExternalInput").ap()
        self.t[name] = self.ext[full]
        return self.t[name]

    def dout(self, name, shape, dt=F32):
        self.t[name] = self.nc.dram_tensor(name, list(shape), dt, kind="ExternalOutput").ap()
        self.ext[name] = self.t[name]
        return self.t[name]

    def dscr(self, name, shape, dt=F32):
        if name in self.t and name in self.scr:
            return self.t[name]
        self.scr.add(name)
        self.t[name] = self.nc.dram_tensor(name, list(shape), dt, kind=("ExternalOutput" if DEBUG_SCR else "Internal")).ap()
        return self.t[name]


def declare_io(P):
    for v in range(NV):
        P.din(f"hseq_{v}", [NSEQ, D])
        P.din(f"cvec_{v}", [2, D])
        P.dout(f"hout_{v}", [MYTOK, D])


def declare_common(P):
    P.din("w_ada", [D, 6 * D])
    P.din("b_ada", [1, 6 * D])
    for n in ("ln1_g", "ln1_b", "ln2_g", "ln2_b"):
        P.din(n, [1, D])
    P.din("w_router", [D, NE])
    P.din("b_router", [1, NE])
    if NPHASES >= 7:
        P.din("w_up", [NE, D, 2 * D])
        P.din("b_up", [NE, 2 * D])
        P.din("w_down", [NE, D, D])
        P.din("b_down", [NE, D])
    _pf, P.prefix = P.prefix, ""
    P.din("ident", [128, 128])
    P.prefix = _pf
    P.dscr("MOD", [2, 6 * D])
    P.dscr("H1", [MYTOK, D])
    P.dscr("U2T", [D, MYTOK], BF16)
    P.dscr("FS", [MYTOK, D])


def my_rows(t):
    if t == 0:
        return 0, 128
    return NCTX + (t - 1) * 128, NCTX + t * 128


def phase_adaln(k, P):
    t = P.t
    k.phase()
    cT = k.sb("cT", [128, 8, 2]); b_cT = k.buf()
    for r in range(2):
        k.dma("sp", cT[:, :, r], t["cvec"][r].rearrange("(kc p) -> p kc", p=128), writes=[b_cT],
              allow_slow_non_contiguous=True)
    sT = k.sb("sT", [128, 8, 2]); b_sT = k.buf()
    k.op("act", lambda e: e.activation(sT[:], cT[:], AF.Silu), reads=[b_cT], writes=[b_sT])
    wbuf = [k.sb(f"wada{i}", [128, 8, 512]) for i in range(2)]
    b_w = k.bufs(2, "wada")
    bb = [k.sb(f"bada{i}", [2, 512]) for i in range(2)]
    b_bb = k.bufs(2, "bada")
    pm = [k.ps(f"pmod{i}", [2, 512]) for i in range(2)]
    b_pm = k.pbufs(2, "pmod")
    res = [k.sb(f"rmod{i}", [2, 512]) for i in range(2)]
    b_res = k.bufs(2, "rmod")
    b_mod = k.buf("MODscr")
    for j in range(12):
        i = j % 2
        k.dma("sp", wbuf[i][:], t["w_ada"][:, j * 512:(j + 1) * 512].rearrange("(kc p) n -> p kc n", p=128),
              writes=[b_w[i]])
        k.dma("act", bb[i][:], bc(t["b_ada"][0:1, j * 512:(j + 1) * 512], [2, 512]), writes=[b_bb[i]])
        for kc in range(8):
            k.op("pe", lambda e, i=i, kc=kc: e.matmul(pm[i][:], lhsT=sT[:, kc, :], rhs=wbuf[i][:, kc, :],
                                                      start=(kc == 0), stop=(kc == 7)),
                 reads=[b_sT, b_w[i]], writes=[b_pm[i]])
        add1 = 1.0 if j in (2, 3, 8, 9) else 0.0
        k.op("dve", lambda e, i=i, add1=add1: e.scalar_tensor_tensor(
            res[i][:], pm[i][:], add1, bb[i][:], ALU.add, ALU.add),
            reads=[b_pm[i], b_bb[i]], writes=[b_res[i]])
        k.dma("sp", t["MOD"][:, j * 512:(j + 1) * 512], res[i][:], reads=[b_res[i]], writes=[b_mod])
    k.flush()


MOD_SH1, MOD_SC1, MOD_G1, MOD_SH2, MOD_SC2, MOD_G2 = range(6)


def mod_bc(P, r, which, n=128):
    return bc(P.t["MOD"][r:r + 1, which * D:(which + 1) * D], [n, D])


def phase_modulate(k, P, uscr):
    t = P.t
    k.phase()
    A = [k.sb(f"mA{r}", [128, D]) for r in range(2)]
    B = [k.sb(f"mB{r}", [128, D]) for r in range(2)]
    b_ab = k.buf()
    for r in range(2):
        k.dma("sp", A[r][:], mod_bc(P, r, MOD_SC1), writes=[b_ab])
        k.dma("sp", B[r][:], mod_bc(P, r, MOD_SH1), writes=[b_ab])
    NB = 3
    hb = [k.sb(f"mh{i}", [128, D]) for i in range(NB)]; b_h = k.bufs(NB, "mh")
    tb = [k.sb(f"mt{i}", [128, D]) for i in range(NB)]; b_t = k.bufs(NB, "mt")
    ub = [k.sb(f"mu{i}", [128, D], BF16) for i in range(NB)]; b_u = k.bufs(NB, "mu")
    b_us = k.buf("uscr")
    for tt in range(NT_SEQ):
        i = tt % NB
        r = 1 if tt < 2 else 0
        k.dma("sp", hb[i][:], t["hseq"][tt * 128:(tt + 1) * 128, :], writes=[b_h[i]])
        k.op("pool", lambda e, i=i, r=r: e.tensor_tensor(tb[i][:], hb[i][:], A[r][:], ALU.mult),
             reads=[b_h[i], b_ab], writes=[b_t[i]])
        k.op("dve", lambda e, i=i, r=r: e.tensor_tensor(ub[i][:], tb[i][:], B[r][:], ALU.add),
             reads=[b_t[i], b_ab], writes=[b_u[i]])
        k.dma("act", uscr[tt * 128:(tt + 1) * 128, :], ub[i][:], reads=[b_u[i]], writes=[b_us])
    k.flush()


def emit_ln(k, x, b_x, out, b_out, gB, bB, b_gb, wk):
    st, b_st = wk["st"], wk["b_st"]
    mv, b_mv = wk["mv"], wk["b_mv"]
    rs, b_rs = wk["rs"], wk["b_rs"]
    xn, b_xn = wk["xn"], wk["b_xn"]
    for c in range(2):
        k.op("dve", lambda e, c=c: e.bn_stats(st[:, c, :], x[:, c * 512:(c + 1) * 512]),
             reads=[b_x], writes=[b_st])
    k.op("dve", lambda e: e.bn_aggr(mv[:], st[:]), reads=[b_st], writes=[b_mv])
    k.op("act", lambda e: e.activation(rs[:], mv[:, 1:2], AF.Sqrt, bias=wk["eps"][:], scale=1.0),
         reads=[b_mv, wk["b_eps"]], writes=[b_rs])
    k.op("dve", lambda e: e.reciprocal(rs[:], rs[:]), reads=[b_rs], writes=[b_rs])
    k.op("dve", lambda e: e.tensor_scalar(xn[:], x[:], mv[:, 0:1], rs[:], ALU.subtract, ALU.mult),
         reads=[b_x, b_mv, b_rs], writes=[b_xn])
    k.op("pool", lambda e: e.tensor_tensor(xn[:], xn[:], gB[:], ALU.mult), reads=[b_xn, b_gb], writes=[b_xn])
    k.op("dve", lambda e: e.tensor_tensor(out[:], xn[:], bB[:], ALU.add), reads=[b_xn, b_gb], writes=[b_out])


def ln_work(k, pfx):
    eps = k.sb(pfx + "eps", [128, 1]); b_eps = k.buf()
    k.op("dve", lambda e: e.memset(eps[:], LN_EPS), writes=[b_eps])
    return dict(eps=eps, b_eps=b_eps, st=k.sb(pfx + "st", [128, 2, 6]), b_st=k.buf(), mv=k.sb(pfx + "mv", [128, 2]), b_mv=k.buf(),
                rs=k.sb(pfx + "rs", [128, 1]), b_rs=k.buf(), xn=k.sb(pfx + "xn", [128, D]), b_xn=k.buf())


class PostMixer:
    def __init__(self, k, P, G, b_G):
        self.k, self.P, self.G, self.b_G = k, P, G, b_G
        t = P.t
        self.b_c = k.buf("pmconst")
        self.g1B = [k.sb(f"g1B{r}", [128, D]) for r in range(2)]
        for r in range(2):
            k.dma("sp", self.g1B[r][:], mod_bc(P, r, MOD_G1), writes=[self.b_c])
        self.lgB = k.sb("ln1gB", [128, D]); self.lbB = k.sb("ln1bB", [128, D])
        k.dma("sp", self.lgB[:], bc(t["ln1_g"], [128, D]), writes=[self.b_c])
        k.dma("sp", self.lbB[:], bc(t["ln1_b"], [128, D]), writes=[self.b_c])
        self.sc2c = k.sb("sc2c", [128, 2, 8]); self.sh2c = k.sb("sh2c", [128, 2, 8])
        for r in range(2):
            k.dma("sp", self.sc2c[:, r, :], t["MOD"][r, MOD_SC2 * D:(MOD_SC2 + 1) * D].rearrange("(kc p) -> p kc", p=128),
                  writes=[self.b_c], allow_slow_non_contiguous=True)
            k.dma("sp", self.sh2c[:, r, :], t["MOD"][r, MOD_SH2 * D:(MOD_SH2 + 1) * D].rearrange("(kc p) -> p kc", p=128),
                  writes=[self.b_c], allow_slow_non_contiguous=True)
        self.wr = k.sb("wr32", [128, 8, NE])
        k.dma("sp", self.wr[:], t["w_router"].rearrange("(kc p) n -> p kc n", p=128), writes=[self.b_c])
        self.brB = k.sb("brB", [128, NE])
        k.dma("sp", self.brB[:], bc(t["b_router"], [128, NE]), writes=[self.b_c])
        self.idt = k.sb("pm_ident", [128, 128])
        k.dma("sp", self.idt[:], t["ident"], writes=[self.b_c])
        NB = 2
        self.NB = NB
        self.h = [k.sb(f"pmh{i}", [128, D]) for i in range(NB)]; self.b_h = k.bufs(NB, "pmh")
        self.t1 = [k.sb(f"pmt{i}", [128, D]) for i in range(NB)]; self.b_t1 = k.bufs(NB, "pmt")
        self.h1 = [k.sb(f"pmh1{i}", [128, D]) for i in range(NB)]; self.b_h1 = k.bufs(NB, "pmh1")
        self.lnw = [ln_work(k, f"pml{i}") for i in range(NB)]
        _pT = k.ps("pmpT", [128, 8, 128]); _bpT = k.pbuf("pmpT")
        self.pT = [_pT] * NB; self.b_pT = [_bpT] * NB
        self.u32 = [k.sb(f"pmu32{i}", [128, 8, 128]) for i in range(NB)]; self.b_u32 = k.bufs(NB, "pmu32")
        self.ubf = [k.sb(f"pmubf{i}", [128, 8, 128], BF16) for i in range(NB)]; self.b_ubf = k.bufs(NB, "pmubf")
        _pl = k.ps("pmpl", [128, NE]); _bpl = k.pbuf("pmpl")
        self.pl = [_pl] * NB; self.b_pl = [_bpl] * NB
        self.lg = [k.sb(f"pmlg{i}", [128, NE]) for i in range(NB)]; self.b_lg = k.bufs(NB, "pmlg")
        self.m8 = [k.sb(f"pmm8{i}", [128, 8]) for i in range(NB)]; self.b_m8 = k.bufs(NB, "pmm8")
        self.ex = [k.sb(f"pmex{i}", [128, NE]) for i in range(NB)]; self.b_ex = k.bufs(NB, "pmex")
        self.mk = [k.sb(f"pmmk{i}", [128, NE]) for i in range(NB)]; self.b_mk = k.bufs(NB, "pmmk")
        self.sm = [k.sb(f"pmsm{i}", [128, 2]) for i in range(NB)]; self.b_sm = k.bufs(NB, "pmsm")
        self.b_h1s = k.buf("H1scr")
        self.b_u2s = k.buf("U2Tscr")

    def prefetch(self, tt):
        k, t = self.k, self.P.t
        i = tt % self.NB
        r0, r1 = my_rows(tt)
        k.dma("sp", self.h[i][:], t["hseq"][r0:r1, :], writes=[self.b_h[i]])

    def emit(self, tt, ymix, b_ymix):
        k, t = self.k, self.P.t
        i = tt % self.NB
        r = 1 if tt == 0 else 0
        h, t1, h1 = self.h[i], self.t1[i], self.h1[i]
        k.op("pool", lambda e: e.tensor_tensor(t1[:], ymix, self.g1B[r][:], ALU.mult),
             reads=[b_ymix, self.b_c], writes=[self.b_t1[i]])
        k.op("dve", lambda e: e.scalar_tensor_tensor(t1[:], h[:], DN_ALPHA, t1[:], ALU.mult, ALU.add),
             reads=[self.b_h[i], self.b_t1[i]], writes=[self.b_t1[i]])
        emit_ln(k, t1, self.b_t1[i], h1, self.b_h1[i], self.lgB, self.lbB, self.b_c, self.lnw[i])
        k.dma("act", t["H1"][tt * 128:(tt + 1) * 128, :], h1[:], reads=[self.b_h1[i]], writes=[self.b_h1s])
        pT, u32, ubf = self.pT[i], self.u32[i], self.ubf[i]
        for kc in range(8):
            k.op("pe", lambda e, kc=kc: e.transpose(pT[:, kc, :], h1[:, kc * 128:(kc + 1) * 128], self.idt[:]),
                 reads=[self.b_h1[i], self.b_c], writes=[self.b_pT[i]])
        k.op("dve", lambda e: e.tensor_tensor(u32[:], pT[:], bc(self.sc2c[:, r, :].unsqueeze(2), [128, 8, 128]),
                                              ALU.mult), reads=[self.b_pT[i], self.b_c], writes=[self.b_u32[i]])
        k.op("pool", lambda e: e.tensor_tensor(u32[:], u32[:], bc(self.sh2c[:, r, :].unsqueeze(2), [128, 8, 128]),
                                               ALU.add), reads=[self.b_u32[i], self.b_c], writes=[self.b_u32[i]])
        k.op("act", lambda e: e.copy(ubf[:], u32[:]), reads=[self.b_u32[i]], writes=[self.b_ubf[i]])
        k.dma("act", t["U2T"][:, tt * 128:(tt + 1) * 128].rearrange("(kc p) n -> p kc n", p=128), ubf[:],
              reads=[self.b_ubf[i]], writes=[self.b_u2s])
        pl, lg, m8, ex, mk, sm = self.pl[i], self.lg[i], self.m8[i], self.ex[i], self.mk[i], self.sm[i]
        for kc in range(8):
            k.op("pe", lambda e, kc=kc: e.matmul(pl[:], lhsT=u32[:, kc, :], rhs=self.wr[:, kc, :],
                                                 start=(kc == 0), stop=(kc == 7)),
                 reads=[self.b_u32[i], self.b_c], writes=[self.b_pl[i]])
        k.op("dve", lambda e: e.tensor_tensor(lg[:], pl[:], self.brB[:], ALU.add),
             reads=[self.b_pl[i], self.b_c], writes=[self.b_lg[i]])
        k.op("dve", lambda e: e.max(out=m8[:], in_=lg[:]), reads=[self.b_lg[i]], writes=[self.b_m8[i]])
        k.op("dve", lambda e: e.tensor_scalar(mk[:], lg[:], m8[:, 3:4], None, ALU.is_ge),
             reads=[self.b_lg[i], self.b_m8[i]], writes=[self.b_mk[i]])
        k.op("dve", lambda e: e.tensor_scalar(ex[:], lg[:], m8[:, 0:1], None, ALU.subtract),
             reads=[self.b_lg[i], self.b_m8[i]], writes=[self.b_ex[i]])
        k.op("act", lambda e: e.activation(ex[:], ex[:], AF.Exp), reads=[self.b_ex[i]], writes=[self.b_ex[i]])
        k.op("dve", lambda e: e.tensor_tensor(ex[:], ex[:], mk[:], ALU.mult),
             reads=[self.b_ex[i], self.b_mk[i]], writes=[self.b_ex[i]])
        k.op("dve", lambda e: e.reduce_sum(sm[:, 0:1], ex[:], axis=mybir.AxisListType.X),
             reads=[self.b_ex[i]], writes=[self.b_sm[i]])
        k.op("dve", lambda e: e.reciprocal(sm[:, 1:2], sm[:, 0:1]), reads=[self.b_sm[i]], writes=[self.b_sm[i]])
        k.op("dve", lambda e: e.tensor_scalar(self.G[:, tt, :], ex[:], sm[:, 1:2], None, ALU.mult),
             reads=[self.b_ex[i], self.b_sm[i]], writes=[self.b_G])


def phase_moe(k, P, G, b_G, supertiles):
    t = P.t
    k.phase()
    b_c = k.buf("moeconst")
    idt = k.sb("moe_ident", [128, 128])
    k.dma("sp", idt[:], t["ident"], writes=[b_c])
    bup = k.sb("bup", [128, NE, 16])
    for e4 in range(0, NE, 8):
        k.dma("sp", bup[:, e4:e4 + 8, :], t["b_up"][e4:e4 + 8, :].rearrange("e (fc p) -> p e fc", p=128),
              writes=[b_c], allow_slow_non_contiguous=True)
    k.op("dve", lambda e: e.tensor_scalar(bup[:, :, 8:16], bup[:, :, 8:16], 1.0, None, ALU.add),
         reads=[b_c], writes=[b_c])
    bdn = k.sb("bdn", [NE, D])
    k.dma("sp", bdn[:], t["b_down"], writes=[b_c])
    NW = 2
    wu = [k.sb(f"wu{i}", [128, 8, 1024], BF16) for i in range(NW)]; b_wu = k.bufs(NW, "wu")
    wd = [k.sb(f"wd{i}", [128, 4, 1024], BF16) for i in range(NW)]; b_wd = k.bufs(NW, "wd")
    maxtok = max(n for _, n in supertiles) * 128
    maxtl = max(n for _, n in supertiles)
    uT = k.sb("moe_uT", [128, 8, maxtok], BF16); b_uT = k.buf("moe_uT")
    acc = k.sb("moe_acc", [128, maxtl, D]); b_acc = [k.buf(f"acc{i}") for i in range(maxtl)]
    phg = [k.ps(f"phg{i}", [128, 512]) for i in range(2)]; b_phg = k.pbufs(2, "phg")
    phl = [k.ps(f"phl{i}", [128, 512]) for i in range(2)]; b_phl = k.pbufs(2, "phl")
    py = [k.ps(f"py{i}", [128, 1024]) for i in range(2)]; b_py = k.pbufs(2, "py")
    NA = 2
    g1 = [k.sb(f"mg1{i}", [128, 512]) for i in range(NA)]; b_g1 = k.bufs(NA, "mg1")
    sg = [k.sb(f"msg{i}", [128, 512]) for i in range(NA)]; b_sg = k.bufs(NA, "msg")
    l1 = [k.sb(f"ml1{i}", [128, 512]) for i in range(NA)]; b_l1 = k.bufs(NA, "ml1")
    aT = [k.sb(f"maT{i}", [128, 4, 512], BF16) for i in range(2)]; b_aT = k.bufs(2, "maT")
    gt = k.sb("moe_gT", [NE, 128]); b_gt = k.buf("moe_gT")
    b_fs = k.buf("FSscr")

    units = [(e, fh) for e in range(NE) for fh in range(2)]

    def load_unit(ui, slot):
        e, fh = units[ui]
        k.dma("pool", wu[slot][:, :, 0:512],
              t["w_up"][e, :, fh * 512:(fh + 1) * 512].rearrange("(kc p) f -> p kc f", p=128), writes=[b_wu[slot]])
        k.dma("pool", wu[slot][:, :, 512:1024],
              t["w_up"][e, :, 1024 + fh * 512:1024 + (fh + 1) * 512].rearrange("(kc p) f -> p kc f", p=128),
              writes=[b_wu[slot]])
        k.dma("pool", wd[slot][:], t["w_down"][e, fh * 512:(fh + 1) * 512, :].rearrange("(fc p) d -> p fc d", p=128),
              writes=[b_wd[slot]])

    cnt_act = 0
    cnt_chunk = 0
    cnt_y = 0
    for (t0, ntl) in supertiles:
        ntok = ntl * 128
        k.dma("sp", uT[:, :, 0:ntok], t["U2T"][:, t0 * 128:t0 * 128 + ntok].rearrange("(kc p) n -> p kc n", p=128),
              writes=[b_uT])
        for tl in range(ntl):
            pT = py[cnt_y % 2]; bpT = b_py[cnt_y % 2]; cnt_y += 1
            k.op("pe", lambda e, tl=tl, pT=pT: e.transpose(pT[0:NE, 0:128], G[:, t0 + tl, :], idt[:]),
                 reads=[b_G, b_c], writes=[bpT])
            k.op("act", lambda e, pT=pT: e.copy(gt[:], pT[0:NE, 0:128]), reads=[bpT], writes=[b_gt])
            for hf in range(2):
                k.op("pe", lambda e, hf=hf, pT=pT: e.matmul(pT[:, hf * 512:(hf + 1) * 512], lhsT=gt[:],
                                                            rhs=bdn[:, hf * 512:(hf + 1) * 512], start=True, stop=True),
                     reads=[b_gt, b_c, bpT], writes=[bpT])
            k.op("act", lambda e, tl=tl, pT=pT: e.copy(acc[:, tl, :], pT[:]), reads=[bpT], writes=[b_acc[tl]])
        load_unit(0, 0)
        for ui, (ex, fh) in enumerate(units):
            slot = ui % NW
            if ui + 1 < len(units):
                load_unit(ui + 1, (ui + 1) % NW)
            chunks = [(c0, min(512, ntok - c0)) for c0 in range(0, ntok, 512)]
            for (c0, n) in chunks:
                ab = cnt_chunk % 2; cnt_chunk += 1
                for fc in range(4):
                    pb = cnt_act % 2
                    ai = cnt_act % NA
                    cnt_act += 1
                    for kc in range(8):
                        k.op("pe", lambda e, kc=kc, fc=fc, pb=pb, c0=c0, n=n: e.matmul(
                            phg[pb][:, 0:n], lhsT=wu[slot][:, kc, fc * 128:(fc + 1) * 128],
                            rhs=uT[:, kc, c0:c0 + n], start=(kc == 0), stop=(kc == 7)),
                            reads=[b_wu[slot], b_uT], writes=[b_phg[pb]])
                    for kc in range(8):
                        k.op("pe", lambda e, kc=kc, fc=fc, pb=pb, c0=c0, n=n: e.matmul(
                            phl[pb][:, 0:n], lhsT=wu[slot][:, kc, 512 + fc * 128:512 + (fc + 1) * 128],
                            rhs=uT[:, kc, c0:c0 + n], start=(kc == 0), stop=(kc == 7)),
                            reads=[b_wu[slot], b_uT], writes=[b_phl[pb]])
                    fcg = fh * 4 + fc
                    k.op("dve", lambda e, pb=pb, ai=ai, n=n, fcg=fcg, ex=ex: e.tensor_scalar(
                        g1[ai][:, 0:n], phg[pb][:, 0:n], bup[:, ex, fcg:fcg + 1], 7.0, ALU.add, ALU.min),
                        reads=[b_phg[pb], b_c], writes=[b_g1[ai]])
                    k.op("dve", lambda e, pb=pb, ai=ai, n=n, fcg=fcg, ex=ex: e.tensor_scalar(
                        l1[ai][:, 0:n], phl[pb][:, 0:n], bup[:, ex, 8 + fcg:9 + fcg], 8.0, ALU.add, ALU.min),
                        reads=[b_phl[pb], b_c], writes=[b_l1[ai]])
                    k.op("act", lambda e, ai=ai, n=n: e.activation(sg[ai][:, 0:n], g1[ai][:, 0:n], AF.Sigmoid,
                                                                   scale=1.702),
                         reads=[b_g1[ai]], writes=[b_sg[ai]])
                    k.op("dve", lambda e, ai=ai, n=n: e.tensor_tensor(sg[ai][:, 0:n], sg[ai][:, 0:n], g1[ai][:, 0:n],
                                                                      ALU.mult),
                         reads=[b_sg[ai], b_g1[ai]], writes=[b_sg[ai]])
                    k.op("dve", lambda e, ai=ai, n=n, ab=ab, fc=fc: e.scalar_tensor_tensor(
                        aT[ab][:, fc, 0:n], l1[ai][:, 0:n], -6.0, sg[ai][:, 0:n], ALU.max, ALU.mult),
                        reads=[b_sg[ai], b_l1[ai]], writes=[b_aT[ab]])
                for j in range(n // 128):
                    tl = (c0 // 128) + j
                    yb = cnt_y % 2; cnt_y += 1
                    for hf in range(2):
                        for fc in range(4):
                            k.op("pe", lambda e, hf=hf, fc=fc, yb=yb, j=j, ab=ab: e.matmul(
                                py[yb][:, hf * 512:(hf + 1) * 512], lhsT=aT[ab][:, fc, j * 128:(j + 1) * 128],
                                rhs=wd[slot][:, fc, hf * 512:(hf + 1) * 512], start=(fc == 0), stop=(fc == 3)),
                                reads=[b_aT[ab], b_wd[slot]], writes=[b_py[yb]])
                    k.op("dve", lambda e, yb=yb, tl=tl, ex=ex: e.scalar_tensor_tensor(
                        acc[:, tl, :], py[yb][:], G[:, t0 + tl, ex:ex + 1], acc[:, tl, :], ALU.mult, ALU.add),
                        reads=[b_py[yb], b_G, b_acc[tl]], writes=[b_acc[tl]])
        for tl in range(ntl):
            k.dma("sp", t["FS"][(t0 + tl) * 128:(t0 + tl + 1) * 128, :], acc[:, tl, :], reads=[b_acc[tl]],
                  writes=[b_fs])
    k.flush()


def phase_post_moe(k, P):
    t = P.t
    k.phase()
    b_c = k.buf("t2const")
    g2B = [k.sb(f"g2B{r}", [128, D]) for r in range(2)]
    for r in range(2):
        k.dma("sp", g2B[r][:], mod_bc(P, r, MOD_G2), writes=[b_c])
    lgB = k.sb("ln2gB", [128, D]); lbB = k.sb("ln2bB", [128, D])
    k.dma("sp", lgB[:], bc(t["ln2_g"], [128, D]), writes=[b_c])
    k.dma("sp", lbB[:], bc(t["ln2_b"], [128, D]), writes=[b_c])
    NB = 2
    f = [k.sb(f"t2f{i}", [128, D]) for i in range(NB)]; b_f = k.bufs(NB, "t2f")
    h1 = [k.sb(f"t2h{i}", [128, D]) for i in range(NB)]; b_h1 = k.bufs(NB, "t2h")
    o = [k.sb(f"t2o{i}", [128, D]) for i in range(NB)]; b_o = k.bufs(NB, "t2o")
    lnw = [ln_work(k, f"t2l{i}") for i in range(NB)]
    b_out = k.buf("hout")
    for tt in range(NT_MY):
        i = tt % NB
        r = 1 if tt == 0 else 0
        k.dma("sp", f[i][:], t["FS"][tt * 128:(tt + 1) * 128, :], writes=[b_f[i]])
        k.dma("act", h1[i][:], t["H1"][tt * 128:(tt + 1) * 128, :], writes=[b_h1[i]])
        k.op("pool", lambda e, i=i, r=r: e.tensor_tensor(f[i][:], f[i][:], g2B[r][:], ALU.mult),
             reads=[b_f[i], b_c], writes=[b_f[i]])
        k.op("dve", lambda e, i=i: e.scalar_tensor_tensor(f[i][:], h1[i][:], DN_ALPHA, f[i][:], ALU.mult, ALU.add),
             reads=[b_h1[i], b_f[i]], writes=[b_f[i]])
        emit_ln(k, f[i], b_f[i], o[i], b_o[i], lgB, lbB, b_c, lnw[i])
        k.dma("sp", t["hout"][tt * 128:(tt + 1) * 128, :], o[i][:], reads=[b_o[i]], writes=[b_out])
    k.flush()


SUPERTILES = [(0, 17), (17, 16)]


NCH = NSEQ // 8
NCC = NCTX // 8
NPH = NCH + NCC
NF = NCC + MYLAT // 8
NB_ = NCH
HS_F = 10
HS_B = 11


def declare_s5(P):
    P.din("lam_re", [2, 64, 64]); P.din("lam_im", [2, 64, 64]); P.din("log_step", [2, 64])
    P.din("b_re", [2, 64, 64, 16]); P.din("b_im", [2, 64, 64, 16])
    P.din("c_re", [2, 64, 16, 64]); P.din("c_im", [2, 64, 16, 64])
    P.din("s5_d", [1, D]); P.din("w_glu", [D, 2 * D])
    _pf, P.prefix = P.prefix, ""
    P.din("maskF", [128, 128]); P.din("maskB", [128, 128]); P.din("swapm", [128, 128])
    P.prefix = _pf
    P.dscr("US", [NSEQ, D], BF16)
    P.dscr("YS", [MYTOK, D], BF16)
    P.dscr("S5P", [2, 128, 64 * 128], BF16)
    P.dscr("S5Q", [2, 128, 64 * 128], BF16)
    P.dscr("S5HS", [2, 3, 128, 64 * 11])


def s5_consts(k, P):
    t = P.t
    b_c = k.buf("s5const")
    idt = k.sb("s5_ident", [128, 128]); k.dma("sp", idt[:], t["ident"], writes=[b_c])
    idb = k.sb("s5_identb", [128, 128], BF16); k.dma("pool", idb[:], t["ident"], writes=[b_c])
    swb = k.sb("s5_swapb", [128, 128], BF16); k.dma("pool", swb[:], t["swapm"], writes=[b_c])
    mask = [k.sb("s5_mF", [128, 128]), k.sb("s5_mB", [128, 128])]
    k.dma("sp", mask[0][:], t["maskF"], writes=[b_c]); k.dma("sp", mask[1][:], t["maskB"], writes=[b_c])
    sgn = k.sb("s5_sgn", [128, 1])
    k.op("dve", lambda e: e.memset(sgn[0:64, :], -1.0), writes=[b_c])
    k.op("dve", lambda e: e.memset(sgn[64:128, :], 1.0), writes=[b_c])
    npi = k.sb("s5_npi", [128, 1])
    k.op("dve", lambda e: e.memset(npi[:], -PI), writes=[b_c])
    dcol = k.sb("s5_dcol", [128, 64])
    for tau in range(8):
        k.dma("sp", dcol[tau * 16:(tau + 1) * 16, :], t["s5_d"][0, :].rearrange("(g h) -> h g", h=16),
              writes=[b_c], allow_slow_non_contiguous=True)
    return b_c, idt, idb, swb, mask, sgn, npi, dcol


def phase_s5_setup(k, P, d_out):
    t = P.t
    k.phase()
    d = d_out
    pd = d_out ^ P.sw
    b_c, idt, idb, swb, mask, sgn, npi, dcol = s5_consts(k, P)
    Pbf = {d: k.sb(f"s5_P{d}", [128, 64, 128], BF16)}
    Qbf = {d: k.sb(f"s5_Q{d}", [128, 64, 128], BF16)}
    hsR = {d: k.sb(f"s5_hsR{d}", [128, 64, 11])}
    hsI = {d: k.sb(f"s5_hsI{d}", [128, 64, 11])}
    hsN = {d: k.sb(f"s5_hsN{d}", [128, 64, 11])}
    b_tab = k.buf("s5tab")

    ld = k.sb("s5_ld", [64, 128]); b_ld = k.buf()
    pst = k.ps("s5_pst", [128, 128]); b_pst = k.pbuf()
    cnt = [0]

    def tmp(shape):
        cnt[0] += 1
        return k.sb(f"s5_t{cnt[0]}", shape)

    b_s = k.buf("s5setup")

    def dv(fn):
        k.op("dve", fn, reads=[b_s, b_c], writes=[b_s])

    def cmul(orr, oi, xr, xi, yr, yi, t1, t2):
        dv(lambda e: e.tensor_tensor(t1, xr, yr, ALU.mult))
        dv(lambda e: e.tensor_tensor(t2, xi, yi, ALU.mult))
        dv(lambda e: e.tensor_tensor(t2, t1, t2, ALU.subtract))
        dv(lambda e: e.tensor_tensor(t1, xr, yi, ALU.mult))
        dv(lambda e: e.tensor_tensor(oi, xi, yr, ALU.mult))
        dv(lambda e: e.tensor_tensor(oi, oi, t1, ALU.add))
        dv(lambda e: e.tensor_copy(orr, t2))

    if True:
        lr, li, dt = tmp([128, 64]), tmp([128, 64]), tmp([128, 64])
        for (dst, src) in ((lr, "lam_re"), (li, "lam_im")):
            for hf in range(2):
                k.dma("sp", ld[:, hf * 64:(hf + 1) * 64], t[src][pd], writes=[b_ld])
            k.op("pe", lambda e: e.transpose(pst[:, 0:64], ld[:], idt[0:64, 0:64]), reads=[b_ld, b_c], writes=[b_pst])
            k.op("dve", lambda e, dst=dst: e.tensor_copy(dst[:], pst[:, 0:64]), reads=[b_pst, b_s], writes=[b_s])
        k.dma("sp", dt[:], bc(t["log_step"][pd:pd + 1, :], [128, 64]), writes=[b_s])
        hx, hp = tmp([128, 64]), tmp([128, 64])

        def horner(dst, x, coef):
            dv(lambda e: e.tensor_scalar(dst[:], x[:], float(coef[-1]), float(coef[-2]), ALU.mult, ALU.add))
            for cfv in coef[-3::-1]:
                dv(lambda e: e.tensor_tensor(dst[:], dst[:], x[:], ALU.mult))
                dv(lambda e, cfv=cfv: e.tensor_scalar(dst[:], dst[:], float(cfv), None, ALU.add))

        expc = [1.0 / math.factorial(i) for i in range(11)]
        dv(lambda e: e.tensor_scalar(hx[:], dt[:], 0.125, None, ALU.mult))
        horner(dt, hx, expc)
        for _ in range(3):
            dv(lambda e: e.tensor_tensor(dt[:], dt[:], dt[:], ALU.mult))
        mag, ang, cs, sn = tmp([128, 64]), tmp([128, 64]), tmp([128, 64]), tmp([128, 64])
        dv(lambda e: e.tensor_tensor(hx[:], lr[:], dt[:], ALU.mult))
        horner(mag, hx, expc)
        dv(lambda e: e.tensor_tensor(ang[:], li[:], dt[:], ALU.mult))
        qf, qi = tmp([128, 64]), k.sb("s5_qi", [128, 64], mybir.dt.int32)
        dv(lambda e: e.tensor_scalar(qf[:], ang[:], 1.0 / (2 * PI), None, ALU.mult))
        dv(lambda e: e.tensor_copy(qi[:], qf[:]))
        dv(lambda e: e.tensor_copy(qf[:], qi[:]))
        dv(lambda e: e.scalar_tensor_tensor(ang[:], qf[:], -2 * PI, ang[:], ALU.mult, ALU.add))
        dv(lambda e: e.tensor_scalar(qf[:], ang[:], PI, 2 * PI, ALU.is_gt, ALU.mult))
        dv(lambda e: e.tensor_tensor(ang[:], ang[:], qf[:], ALU.subtract))
        dv(lambda e: e.tensor_scalar(qf[:], ang[:], -PI, 2 * PI, ALU.is_lt, ALU.mult))
        dv(lambda e: e.tensor_tensor(ang[:], ang[:], qf[:], ALU.add))
        dv(lambda e: e.tensor_scalar(ang[:], ang[:], 0.5, None, ALU.mult))
        dv(lambda e: e.tensor_tensor(hx[:], ang[:], ang[:], ALU.mult))
        sinc = [(-1.0) ** i / math.factorial(2 * i + 1) for i in range(7)]
        cosc = [(-1.0) ** i / math.factorial(2 * i) for i in range(7)]
        horner(hp, hx, sinc)
        dv(lambda e: e.tensor_tensor(hp[:], hp[:], ang[:], ALU.mult))
        horner(qf, hx, cosc)
        dv(lambda e: e.scalar_tensor_tensor(sn[:], hp[:], 2.0, qf[:], ALU.mult, ALU.mult))
        dv(lambda e: e.tensor_tensor(cs[:], hp[:], hp[:], ALU.mult))
        dv(lambda e: e.tensor_scalar(cs[:], cs[:], -2.0, 1.0, ALU.mult, ALU.add))
        ar, ai = tmp([128, 64]), tmp([128, 64])
        dv(lambda e: e.tensor_tensor(ar[:], mag[:], cs[:], ALU.mult))
        dv(lambda e: e.tensor_tensor(ai[:], mag[:], sn[:], ALU.mult))
        m2, vr, vi = tmp([128, 64]), tmp([128, 64]), tmp([128, 64])
        dv(lambda e: e.tensor_tensor(m2[:], mag[:], mag[:], ALU.mult))
        dv(lambda e: e.reciprocal(m2[:], m2[:]))
        dv(lambda e: e.tensor_tensor(vr[:], ar[:], m2[:], ALU.mult))
        dv(lambda e: e.scalar_tensor_tensor(vi[:], ai[:], -1.0, m2[:], ALU.mult, ALU.mult))
        den, nr, kr, ki, t1, t2 = (tmp([128, 64]) for _ in range(6))
        dv(lambda e: e.tensor_tensor(den[:], lr[:], lr[:], ALU.mult))
        dv(lambda e: e.tensor_tensor(t1[:], li[:], li[:], ALU.mult))
        dv(lambda e: e.tensor_tensor(den[:], den[:], t1[:], ALU.add))
        dv(lambda e: e.reciprocal(den[:], den[:]))
        dv(lambda e: e.tensor_scalar(nr[:], ar[:], -1.0, None, ALU.add))
        dv(lambda e: e.tensor_tensor(kr[:], nr[:], lr[:], ALU.mult))
        dv(lambda e: e.tensor_tensor(t1[:], ai[:], li[:], ALU.mult))
        dv(lambda e: e.tensor_tensor(kr[:], kr[:], t1[:], ALU.add))
        dv(lambda e: e.tensor_tensor(kr[:], kr[:], den[:], ALU.mult))
        dv(lambda e: e.tensor_tensor(ki[:], ai[:], lr[:], ALU.mult))
        dv(lambda e: e.tensor_tensor(t1[:], nr[:], li[:], ALU.mult))
        dv(lambda e: e.tensor_tensor(ki[:], ki[:], t1[:], ALU.subtract))
        dv(lambda e: e.tensor_tensor(ki[:], ki[:], den[:], ALU.mult))

        def powtab(br_, bi_, desc):
            R, I = tmp([128, 64, 8]), tmp([128, 64, 8])
            s2r, s2i, s4r, s4i = (tmp([128, 64]) for _ in range(4))
            ta, tb = tmp([128, 64, 4]), tmp([128, 64, 4])
            cmul(s2r[:], s2i[:], br_[:], bi_[:], br_[:], bi_[:], ta[:, :, 0], tb[:, :, 0])
            cmul(s4r[:], s4i[:], s2r[:], s2i[:], s2r[:], s2i[:], ta[:, :, 0], tb[:, :, 0])
            i0, i1 = (7, 6) if desc else (0, 1)
            dv(lambda e: e.memset(R[:, :, i0:i0 + 1], 1.0))
            dv(lambda e: e.memset(I[:, :, i0:i0 + 1], 0.0))
            dv(lambda e: e.tensor_copy(R[:, :, i1], br_[:]))
            dv(lambda e: e.tensor_copy(I[:, :, i1], bi_[:]))
            if desc:
                src2, dst2, src4, dst4 = slice(6, 8), slice(4, 6), slice(4, 8), slice(0, 4)
            else:
                src2, dst2, src4, dst4 = slice(0, 2), slice(2, 4), slice(0, 4), slice(4, 8)
            cmul(R[:, :, dst2], I[:, :, dst2], R[:, :, src2], I[:, :, src2],
                 bc(s2r[:].unsqueeze(2), [128, 64, 2]), bc(s2i[:].unsqueeze(2), [128, 64, 2]),
                 ta[:, :, 0:2], tb[:, :, 0:2])
            cmul(R[:, :, dst4], I[:, :, dst4], R[:, :, src4], I[:, :, src4],
                 bc(s4r[:].unsqueeze(2), [128, 64, 4]), bc(s4i[:].unsqueeze(2), [128, 64, 4]),
                 ta[:], tb[:])
            return R, I, s4r, s4i

        desc = (d == 0)
        PR, PI_, a4r, a4i = powtab(ar, ai, desc)
        QR, QI, _, _ = powtab(vr, vi, desc)
        t1w, t2w = tmp([128, 64]), tmp([128, 64])
        cmul(hsR[d][:, :, 0], hsI[d][:, :, 0], a4r[:], a4i[:], a4r[:], a4i[:], t1w[:], t2w[:])
        for lv in range(1, 11):
            cmul(hsR[d][:, :, lv], hsI[d][:, :, lv], hsR[d][:, :, lv - 1], hsI[d][:, :, lv - 1],
                 hsR[d][:, :, lv - 1], hsI[d][:, :, lv - 1], t1w[:], t2w[:])
        dv(lambda e, d=d: e.tensor_scalar(hsI[d][:], hsI[d][:], sgn[:], None, ALU.mult))
        dv(lambda e, d=d: e.tensor_scalar(hsN[d][:], hsI[d][:], -1.0, None, ALU.mult))
        br_, bi_ = tmp([128, 64, 16]), tmp([128, 64, 16])
        for (dst, src) in ((br_, "b_re"), (bi_, "b_im")):
            for hf in range(2):
                k.dma("sp", dst[hf * 64:(hf + 1) * 64], t[src][pd].rearrange("g p h -> p g h"), writes=[b_s])
        bbr, bbi, tq = tmp([128, 64, 16]), tmp([128, 64, 16]), tmp([128, 64, 16])
        krb = bc(kr[:].unsqueeze(2), [128, 64, 16]); kib = bc(ki[:].unsqueeze(2), [128, 64, 16])
        dv(lambda e: e.tensor_tensor(bbr[:], br_[:], krb, ALU.mult))
        dv(lambda e: e.tensor_tensor(tq[:], bi_[:], kib, ALU.mult))
        dv(lambda e: e.tensor_tensor(bbr[:], bbr[:], tq[:], ALU.subtract))
        dv(lambda e: e.tensor_tensor(bbi[:], bi_[:], krb, ALU.mult))
        dv(lambda e: e.tensor_tensor(tq[:], br_[:], kib, ALU.mult))
        dv(lambda e: e.tensor_tensor(bbi[:], bbi[:], tq[:], ALU.add))
        BB1, BB2 = tmp([128, 64, 16]), tmp([128, 64, 16])
        dv(lambda e: e.tensor_copy(BB1[0:64], bbr[0:64]))
        dv(lambda e: e.tensor_copy(BB1[64:128], bbi[64:128]))
        dv(lambda e: e.tensor_copy(BB2[0:64], bbi[0:64]))
        dv(lambda e: e.tensor_copy(BB2[64:128], bbr[64:128]))
        cr, ci = tmp([128, 64, 16]), tmp([128, 64, 16])
        ldc = tmp([128, 128])
        for (dst, src) in ((cr, "c_re"), (ci, "c_im")):
            cv = t[src][pd].rearrange("g h p -> (g h) p")
            for blk in range(8):
                for hf in range(2):
                    k.dma("sp", ldc[:, hf * 64:(hf + 1) * 64], cv[blk * 128:(blk + 1) * 128, :],
                          reads=[b_s], writes=[b_s])
                k.op("pe", lambda e: e.transpose(pst[:], ldc[:], idt[:]), reads=[b_s, b_c], writes=[b_pst])
                k.op("dve", lambda e, dst=dst, blk=blk: e.tensor_copy(
                    dst[:, blk * 8:(blk + 1) * 8, :], pst[:].rearrange("p (g h) -> p g h", h=16)),
                    reads=[b_pst, b_s], writes=[b_s])
        CC1, CC2 = tmp([128, 64, 16]), tmp([128, 64, 16])
        dv(lambda e: e.tensor_copy(CC1[0:64], cr[0:64]))
        dv(lambda e: e.tensor_scalar(CC1[64:128], ci[64:128], -1.0, None, ALU.mult))
        dv(lambda e: e.tensor_scalar(CC2[0:64], ci[0:64], -1.0, None, ALU.mult))
        dv(lambda e: e.tensor_scalar(CC2[64:128], cr[64:128], -1.0, None, ALU.mult))
        PIs = tmp([128, 64, 8])
        dv(lambda e: e.tensor_scalar(PIs[:], PI_[:], sgn[:], None, ALU.mult))
        w1, w2 = tmp([128, 16, 8, 16]), tmp([128, 16, 8, 16])
        for (dstT, XR, XI, M1, M2) in ((Pbf[d], PR, PIs, BB1, BB2), (Qbf[d], QR, QI, CC1, CC2)):
            for g0 in range(0, 64, 16):
                gs = slice(g0, g0 + 16)
                xr = bc(XR[:, gs, :].unsqueeze(3), [128, 16, 8, 16])
                xi = bc(XI[:, gs, :].unsqueeze(3), [128, 16, 8, 16])
                m1 = bc(M1[:, gs, :].unsqueeze(2), [128, 16, 8, 16])
                m2_ = bc(M2[:, gs, :].unsqueeze(2), [128, 16, 8, 16])
                dv(lambda e, xr=xr, m1=m1: e.tensor_tensor(w1[:], xr, m1, ALU.mult))
                dv(lambda e, xi=xi, m2_=m2_: e.tensor_tensor(w2[:], xi, m2_, ALU.mult))
                k.op("dve", lambda e, dstT=dstT, gs=gs: e.tensor_tensor(
                    dstT[:, gs, :].rearrange("p g (t h) -> p g t h", h=16), w1[:], w2[:], ALU.add),
                    reads=[b_s], writes=[b_s, b_tab])

    b_o = k.buf("s5tabscr")
    k.dma("sp", t["S5P"][d], Pbf[d][:].rearrange("p g f -> p (g f)"), reads=[b_s, b_tab], writes=[b_o])
    k.dma("sp", t["S5Q"][d], Qbf[d][:].rearrange("p g f -> p (g f)"), reads=[b_s, b_tab], writes=[b_o])
    for j, tb_ in enumerate((hsR[d], hsI[d], hsN[d])):
        k.dma("sp", t["S5HS"][d, j], tb_[:].rearrange("p g f -> p (g f)"), reads=[b_s, b_tab], writes=[b_o])
    k.flush()


def phase_s5(k, P):
    t = P.t
    k.phase()
    b_c, idt, idb, swb, mask, sgn, npi, dcol = s5_consts(k, P)
    b_tab = k.buf("s5tab")
    Pbf = [k.sb(f"s5_P{d}", [128, 64, 128], BF16) for d in range(2)]
    Qbf = [k.sb(f"s5_Q{d}", [128, 64, 128], BF16) for d in range(2)]
    hsR = [k.sb(f"s5_hsR{d}", [128, 64, 11]) for d in range(2)]
    hsI = [k.sb(f"s5_hsI{d}", [128, 64, 11]) for d in range(2)]
    hsN = [k.sb(f"s5_hsN{d}", [128, 64, 11]) for d in range(2)]
    for d in range(2):
        k.dma("sp", Pbf[d][:].rearrange("p g f -> p (g f)"), t["S5P"][d], writes=[b_tab])
        k.dma("sp", Qbf[d][:].rearrange("p g f -> p (g f)"), t["S5Q"][d], writes=[b_tab])
        for j, tb_ in enumerate((hsR[d], hsI[d], hsN[d])):
            k.dma("sp", tb_[:].rearrange("p g f -> p (g f)"), t["S5HS"][d, j], writes=[b_tab])
    UIN = k.sb("s5_uin", [128, 9, 8, 128], BF16); b_uin = k.buf("s5_uin")
    UG = [k.sb(f"s5_ug{i}", [128, 9, 128], BF16) for i in range(2)]; b_UG = k.bufs(2, "s5_ug")
    UT = [k.sb(f"s5_UT{i}", [128, NPH], BF16) for i in range(2)]; b_UT = k.bufs(2, "s5_UT")
    ptr = [k.ps(f"s5_ptr{i}", [128, 4, 128], BF16) for i in range(2)]; b_ptr = k.pbufs(2, "s5_ptr")
    pgen = k.ps("s5_pgen", [128, 3, 128]); b_pgen = k.pbuf("s5_pgen")
    PT = [k.sb(f"s5_PT{d}", [128, 128], BF16) for d in range(2)]; b_PT = k.bufs(2, "s5_PT")
    PTs = [k.sb(f"s5_PTs{d}", [128, 128], BF16) for d in range(2)]; b_PTs = k.bufs(2, "s5_PTs")
    TM = [k.sb(f"s5_TM{d}", [128, 128], BF16) for d in range(2)]; b_TM = k.bufs(2, "s5_TM")
    tmk = k.sb("s5_tmk", [128, 128]); b_tmk = k.buf()
    px = [k.ps(f"s5_px{i}", [128, 512]) for i in range(2)]; b_px = k.pbufs(2, "s5_px")
    X32 = k.sb("s5_X32", [128, NCH]); b_X32 = k.buf()
    SA = k.sb("s5_SA", [128, NCH]); SB_ = k.sb("s5_SB", [128, NCH]); b_SA, b_SB = k.buf(), k.buf()
    WA = k.sb("s5_WA", [128, NCH]); WB = k.sb("s5_WB", [128, NCH]); b_WA, b_WB = k.buf(), k.buf()
    tS = k.sb("s5_tS", [128, NCH]); tW = k.sb("s5_tW", [128, NCH]); b_tS, b_tW = k.buf(), k.buf()
    tW2 = k.sb("s5_tW2", [128, NCH]); b_tW2 = k.buf()
    SP = [k.sb(f"s5_SP{d}", [128, NCH], BF16) for d in range(2)]; b_SP = k.bufs(2, "s5_SP")
    pyo = k.ps("s5_pyo", [128, 512]); b_pyo = k.pbuf()
    pyc = k.ps("s5_pyc", [128, 16]); b_pyc = k.pbuf()
    ybf = k.sb("s5_ybf", [128, 528], BF16); b_ybf = k.buf()
    pyt = k.ps("s5_pyt", [128, 5, 128], BF16); b_pyt = k.pbuf()
    YO = [k.sb(f"s5_YO{i}", [128, 4, 8, 128], BF16) for i in range(2)]; b_YO = k.bufs(2, "s5_YO")
    YOC = [k.sb(f"s5_YOC{i}", [16, 8, 128], BF16) for i in range(2)]; b_YOC = k.bufs(2, "s5_YOC")
    b_ys = k.buf("YSscr")
    US = t["US"]
    gi = 0
    for gb in range(S5_NGB):
        cs_ = slice(gb * 128, (gb + 1) * 128)
        for blk in range(8):
            r0 = NCTX + blk * 1024
            k.dma("sp", UIN[:, blk], US[r0:r0 + 1024, cs_].rearrange("(c t) ch -> c t ch", t=8), writes=[b_uin])
        k.dma("sp", UIN[0:32, 8], US[0:NCTX, cs_].rearrange("(c t) ch -> c t ch", t=8), writes=[b_uin])
        yo, yoc = YO[gb % 2], YOC[gb % 2]
        for g8 in range(S5_NG8):
            g = gb * 8 + g8
            ut, b_ut = UT[gi % 2], b_UT[gi % 2]
            gi += 1
            hs_ = slice(g8 * 16, (g8 + 1) * 16)
            ug, b_ug = UG[g % 2], b_UG[g % 2]
            k.op("pool", lambda e: e.tensor_copy(ug[:, 0:8, :].rearrange("p a (t h) -> p a t h", h=16),
                                                 UIN[:, 0:8, :, hs_]), reads=[b_uin], writes=[b_ug])
            k.op("pool", lambda e: e.tensor_copy(ug[0:32, 8, :].rearrange("p (t h) -> p t h", h=16),
                                                 UIN[0:32, 8, :, hs_]), reads=[b_uin], writes=[b_ug])
            for q in range(2):
                pp, b_pp = ptr[q], b_ptr[q]
                for j in range(4):
                    blk = q * 4 + j
                    k.op("pe", lambda e, pp=pp, j=j, blk=blk, hs_=hs_: e.transpose(
                        pp[:, j, :], ug[:, blk, :], idb[:]), reads=[b_ug, b_c], writes=[b_pp])
                eng = "act" if q == 0 else "dve"
                if eng == "act":
                    k.op("act", lambda e, pp=pp, q=q, ut=ut: e.copy(
                        ut[:, 32 + q * 512:32 + (q + 1) * 512], pp[:].rearrange("p a b -> p (a b)")),
                        reads=[b_pp], writes=[b_ut])
                else:
                    k.op("dve", lambda e, pp=pp, q=q, ut=ut: e.tensor_copy(
                        ut[:, 32 + q * 512:32 + (q + 1) * 512], pp[:].rearrange("p a b -> p (a b)")),
                        reads=[b_pp], writes=[b_ut])
            pp, b_pp = ptr[0], b_ptr[0]
            k.op("pe", lambda e, pp=pp, hs_=hs_: e.transpose(pp[:, 0, 0:32], ug[0:32, 8, :], idb[0:32, 0:32]),
                 reads=[b_ug, b_c], writes=[b_pp])
            k.op("act", lambda e, pp=pp, ut=ut: e.copy(ut[:, 0:32], pp[:, 0, 0:32]), reads=[b_pp], writes=[b_ut])
            k.op("dve", lambda e, pp=pp, ut=ut: e.tensor_copy(ut[:, NCH:NPH], pp[:, 0, 0:32]), reads=[b_pp],
                 writes=[b_ut])
            if S5_STOP <= 1:
                continue
            for d in range(2):
                c0 = 0 if d == 0 else NCC
                n = NF if d == 0 else NB_
                nlv = HS_F if d == 0 else HS_B
                k.op("pe", lambda e, d=d, g=g: e.matmul(pgen[:, 0, :], lhsT=Pbf[d][:, g, :], rhs=idb[:],
                                                         start=True, stop=True),
                     reads=[b_tab, b_c], writes=[b_pgen])
                k.op("pe", lambda e, d=d, g=g: e.matmul(pgen[:, 1, :], lhsT=Pbf[d][:, g, :], rhs=swb[:],
                                                         start=True, stop=True),
                     reads=[b_tab, b_c], writes=[b_pgen])
                k.op("pe", lambda e, d=d, g=g: e.matmul(pgen[:, 2, :], lhsT=Pbf[d][:, g, :], rhs=Qbf[d][:, g, :],
                                                         start=True, stop=True),
                     reads=[b_tab], writes=[b_pgen])
                if S5_SUB <= 1:
                    continue
                k.op("act", lambda e, d=d: e.copy(PT[d][:], pgen[:, 0, :]), reads=[b_pgen], writes=[b_PT[d]])
                k.op("act", lambda e, d=d: e.copy(PTs[d][:], pgen[:, 1, :]), reads=[b_pgen], writes=[b_PTs[d]])
                if S5_SUB <= 2:
                    continue
                k.op("dve", lambda e, d=d: e.tensor_tensor(tmk[:], pgen[:, 2, :], mask[d][:], ALU.mult),
                     reads=[b_pgen, b_c], writes=[b_tmk])
                if S5_SUB <= 3:
                    continue
                if d == 0:
                    k.op("dve", lambda e, d=d, g=g: e.scalar_tensor_tensor(
                        TM[d][:], idt[:], dcol[:, g:g + 1], tmk[:], ALU.mult, ALU.add),
                        reads=[b_tmk, b_c], writes=[b_TM[d]])
                else:
                    k.op("dve", lambda e, d=d: e.tensor_copy(TM[d][:], tmk[:]), reads=[b_tmk], writes=[b_TM[d]])
                if S5_STOP <= 2:
                    continue
                cols = [(a, min(512, n - a)) for a in range(0, n, 512)]
                for wi, (lhs, b_lhs) in enumerate(((PT[d], b_PT[d]), (PTs[d], b_PTs[d]))):
                    for ci_, (a, w) in enumerate(cols):
                        pb = (wi * len(cols) + ci_) % 2
                        k.op("pe", lambda e, lhs=lhs, a=a, w=w, pb=pb, c0=c0, ut=ut: e.matmul(
                            px[pb][:, 0:w], lhsT=lhs[:], rhs=ut[:, c0 + a:c0 + a + w], start=True, stop=True),
                            reads=[b_lhs, b_ut], writes=[b_px[pb]])
                        if wi == 0:
                            k.op("act", lambda e, a=a, w=w, pb=pb: e.copy(SA[:, a:a + w], px[pb][:, 0:w]),
                                 reads=[b_px[pb]], writes=[b_SA])
                            k.op("dve", lambda e, a=a, w=w, pb=pb: e.tensor_copy(X32[:, a:a + w], px[pb][:, 0:w]),
                                 reads=[b_px[pb]], writes=[b_X32])
                        else:
                            k.op("act", lambda e, a=a, w=w, pb=pb: e.copy(WA[:, a:a + w], px[pb][:, 0:w]),
                                 reads=[b_px[pb]], writes=[b_WA])
                if S5_STOP <= 3:
                    continue
                cur = (SA, b_SA, WA, b_WA)
                nxt = (SB_, b_SB, WB, b_WB)
                for lv in range(nlv):
                    sh = 1 << lv
                    if sh >= n:
                        break
                    So, bSo, Wo, bWo = cur
                    Sn, bSn, Wn, bWn = nxt
                    if d == 0:
                        dst, src, keep = slice(sh, n), slice(0, n - sh), slice(0, sh)
                    else:
                        dst, src, keep = slice(0, n - sh), slice(sh, n), slice(n - sh, n)
                    c1 = hsR[d][:, g, lv:lv + 1]; c2 = hsI[d][:, g, lv:lv + 1]; c2n = hsN[d][:, g, lv:lv + 1]
                    k.op("dve", lambda e, So=So, c1=c1, src=src, dst=dst: e.scalar_tensor_tensor(
                        tS[:, dst], So[:, src], c1, So[:, dst], ALU.mult, ALU.add),
                        reads=[bSo, b_tab], writes=[b_tS])
                    k.op("dve", lambda e, Wo=Wo, Sn=Sn, c2=c2, src=src, dst=dst: e.scalar_tensor_tensor(
                        Sn[:, dst], Wo[:, src], c2, tS[:, dst], ALU.mult, ALU.add),
                        reads=[bWo, b_tS, b_tab], writes=[bSn])
                    k.op("act", lambda e, So=So, Sn=Sn, keep=keep: e.copy(Sn[:, keep], So[:, keep]),
                         reads=[bSo], writes=[bSn])
                    k.op("pool", lambda e, Wo=Wo, c1=c1, src=src, dst=dst: e.tensor_scalar(
                        tW[:, dst], Wo[:, src], c1, None, ALU.mult), reads=[bWo, b_tab], writes=[b_tW])
                    k.op("pool", lambda e, Wo=Wo, dst=dst: e.tensor_tensor(
                        tW[:, dst], tW[:, dst], Wo[:, dst], ALU.add), reads=[bWo, b_tW], writes=[b_tW])
                    k.op("pool", lambda e, So=So, c2n=c2n, src=src, dst=dst: e.tensor_scalar(
                        tW2[:, dst], So[:, src], c2n, None, ALU.mult), reads=[bSo, b_tab], writes=[b_tW2])
                    k.op("pool", lambda e, Wn=Wn, dst=dst: e.tensor_tensor(
                        Wn[:, dst], tW[:, dst], tW2[:, dst], ALU.add), reads=[b_tW, b_tW2], writes=[bWn])
                    k.op("act", lambda e, Wo=Wo, Wn=Wn, keep=keep: e.copy(Wn[:, keep], Wo[:, keep]),
                         reads=[bWo], writes=[bWn])
                    cur, nxt = nxt, cur
                Sf, bSf = cur[0], cur[1]
                k.op("dve", lambda e, Sf=Sf, d=d, n=n: e.tensor_tensor(SP[d][:, 0:n], Sf[:, 0:n], X32[:, 0:n],
                                                                          ALU.subtract),
                     reads=[bSf, b_X32], writes=[b_SP[d]])
            if S5_STOP <= 4:
                continue
            seq = [(TM[0], b_TM[0], ut, b_ut, 32), (Qbf[0][:, g, :], b_tab, SP[0], b_SP[0], 32),
                   (TM[1], b_TM[1], ut, b_ut, 32), (Qbf[1][:, g, :], b_tab, SP[1], b_SP[1], 0)]
            for si, (lhs, b_lhs, rhs, b_rhs, off) in enumerate(seq):
                lh = lhs[:] if si % 2 == 0 else lhs
                k.op("pe", lambda e, lh=lh, rhs=rhs, off=off, si=si: e.matmul(
                    pyo[:], lhsT=lh, rhs=rhs[:, off:off + 512], start=(si == 0), stop=(si == 3)),
                    reads=[b_lhs, b_rhs], writes=[b_pyo])
            seqc = [(TM[0], b_TM[0], ut, b_ut, 0), (Qbf[0][:, g, :], b_tab, SP[0], b_SP[0], 0),
                    (TM[1], b_TM[1], ut, b_ut, NCH), (Qbf[1][:, g, :], b_tab, SP[1], b_SP[1], NCH - NCC)]
            for si, (lhs, b_lhs, rhs, b_rhs, off) in enumerate(seqc):
                lh = lhs[:] if si % 2 == 0 else lhs
                k.op("pe", lambda e, lh=lh, rhs=rhs, off=off, si=si: e.matmul(
                    pyc[:], lhsT=lh, rhs=rhs[:, off:off + 16], start=(si == 0), stop=(si == 3)),
                    reads=[b_lhs, b_rhs], writes=[b_pyc])
            if S5_STOP <= 5:
                continue
            k.op("act", lambda e: e.copy(ybf[:, 0:512], pyo[:]), reads=[b_pyo], writes=[b_ybf])
            k.op("act", lambda e: e.copy(ybf[:, 512:528], pyc[:]), reads=[b_pyc], writes=[b_ybf])
            for j in range(4):
                k.op("pe", lambda e, j=j: e.transpose(pyt[:, j, :], ybf[:, j * 128:(j + 1) * 128], idb[:]),
                     reads=[b_ybf, b_c], writes=[b_pyt])
            k.op("pe", lambda e: e.transpose(pyt[0:16, 4, :], ybf[:, 512:528], idb[:]),
                 reads=[b_ybf, b_c], writes=[b_pyt])
            k.op("dve", lambda e, yo=yo, hs_=hs_: e.tensor_copy(
                yo[:, :, :, hs_], pyt[:, 0:4, :].rearrange("p a (t h) -> p a t h", h=16)),
                reads=[b_pyt], writes=[b_YO[gb % 2]])
            k.op("act", lambda e, yoc=yoc, hs_=hs_: e.copy(
                yoc[:, :, hs_], pyt[0:16, 4, :].rearrange("p (t h) -> p t h", h=16)),
                reads=[b_pyt], writes=[b_YOC[gb % 2]])
        for a_ in range(4):
            k.dma("act", t["YS"][MYCTX + a_ * 1024:MYCTX + (a_ + 1) * 1024, cs_].rearrange("(c t) ch -> c t ch", t=8),
                  yo[:, a_], reads=[b_YO[gb % 2]], writes=[b_ys])
        k.dma("act", t["YS"][0:MYCTX, cs_].rearrange("(c t) ch -> c t ch", t=8), yoc[:],
              reads=[b_YOC[gb % 2]], writes=[b_ys])
    k.flush()


def phase_s5_post(k, P, G, b_G):
    t = P.t
    k.phase()
    pm = PostMixer(k, P, G, b_G)
    b_c = k.buf("s5pconst")
    wg = k.sb("wglu", [128, 8, 2 * D], BF16)
    for kc in range(8):
        k.dma("pool", wg[:, kc, :], t["w_glu"][kc * 128:(kc + 1) * 128, :], writes=[b_c])
    idb = k.sb("s5p_identb", [128, 128], BF16); k.dma("pool", idb[:], t["ident"], writes=[b_c])
    NB = 2
    y = [k.sb(f"gy{i}", [128, D], BF16) for i in range(NB)]; b_y = k.bufs(NB, "gy")
    a = [k.sb(f"ga{i}", [128, D]) for i in range(NB)]; b_a = k.bufs(NB, "ga")
    s = [k.sb(f"gs{i}", [128, D]) for i in range(NB)]; b_s = k.bufs(NB, "gs")
    gl = [k.sb(f"gg{i}", [128, D], BF16) for i in range(NB)]; b_gl = k.bufs(NB, "gg")
    _pT = k.ps("gpT", [128, 8, 128], BF16); _bpT = k.pbuf("gpT")
    pT = [_pT] * NB; b_pT = [_bpT] * NB
    gT = [k.sb(f"ggT{i}", [128, 8, 128], BF16) for i in range(NB)]; b_gT = k.bufs(NB, "ggT")
    pz = k.ps("gpz", [128, 2 * D]); b_pz = k.pbuf("gpz")
    sg = [k.sb(f"gsg{i}", [128, D]) for i in range(NB)]; b_sg = k.bufs(NB, "gsg")
    ym = [k.sb(f"gym{i}", [128, D]) for i in range(NB)]; b_ym = k.bufs(NB, "gym")
    for tt in range(NT_MY):
        i = tt % NB
        pm.prefetch(tt)
        k.dma("sp", y[i][:], t["YS"][tt * 128:(tt + 1) * 128, :], writes=[b_y[i]])
        k.op("pool", lambda e, i=i: e.tensor_tensor(a[i][:], y[i][:], y[i][:], ALU.mult), reads=[b_y[i]],
             writes=[b_a[i]])
        k.op("dve", lambda e, i=i: e.tensor_scalar(a[i][:], a[i][:], 0.044715, 1.0, ALU.mult, ALU.add),
             reads=[b_a[i]], writes=[b_a[i]])
        k.op("pool", lambda e, i=i: e.tensor_tensor(a[i][:], a[i][:], y[i][:], ALU.mult), reads=[b_a[i], b_y[i]],
             writes=[b_a[i]])
        k.op("act", lambda e, i=i: e.activation(s[i][:], a[i][:], AF.Sigmoid, scale=1.5957691216),
             reads=[b_a[i]], writes=[b_s[i]])
        k.op("dve", lambda e, i=i: e.tensor_tensor(gl[i][:], s[i][:], y[i][:], ALU.mult), reads=[b_s[i], b_y[i]],
             writes=[b_gl[i]])
        for kc in range(8):
            k.op("pe", lambda e, i=i, kc=kc: e.transpose(pT[i][:, kc, :], gl[i][:, kc * 128:(kc + 1) * 128], idb[:]),
                 reads=[b_gl[i], b_c], writes=[b_pT[i]])
        k.op("act", lambda e, i=i: e.copy(gT[i][:], pT[i][:]), reads=[b_pT[i]], writes=[b_gT[i]])
        for nb in range(4):
            for kc in range(8):
                k.op("pe", lambda e, i=i, kc=kc, nb=nb: e.matmul(
                    pz[:, nb * 512:(nb + 1) * 512], lhsT=gT[i][:, kc, :], rhs=wg[:, kc, nb * 512:(nb + 1) * 512],
                    start=(kc == 0), stop=(kc == 7)), reads=[b_gT[i], b_c], writes=[b_pz])
        k.op("act", lambda e, i=i: e.activation(sg[i][:], pz[:, D:2 * D], AF.Sigmoid), reads=[b_pz],
             writes=[b_sg[i]])
        k.op("dve", lambda e, i=i: e.tensor_tensor(ym[i][:], pz[:, 0:D], sg[i][:], ALU.mult),
             reads=[b_pz, b_sg[i]], writes=[b_ym[i]])
        pm.emit(tt, ym[i][:], b_ym[i])
    k.flush()


def emit_layer(P, kind):
    with ExitStack() as st:
        k = K(P.nc, st)
        G = k.sb("Gall", [128, NT_MY, NE], glob=True); b_G = k.buf("Gall")
        for v in range(NV):
            P.sw = v % 2
            for nm in ("hseq", "cvec", "hout"):
                P.t[nm] = P.t[f"{nm}_{v}"]
            if kind == "s5":
                phases = [lambda: phase_adaln(k, P), lambda: phase_modulate(k, P, P.t["US"]),
                          lambda: phase_s5_setup(k, P, 0), lambda: phase_s5_setup(k, P, 1), lambda: phase_s5(k, P),
                          lambda: phase_s5_post(k, P, G, b_G), lambda: phase_moe(k, P, G, b_G, SUPERTILES),
                          lambda: phase_post_moe(k, P)]
            else:
                phases = [lambda: phase_adaln(k, P), lambda: phase_modulate(k, P, P.t["US"]),
                          lambda: phase_gla_pass(k, P, 0), lambda: phase_gla_pass(k, P, 1),
                          lambda: phase_gla_post(k, P, G, b_G), lambda: phase_moe(k, P, G, b_G, SUPERTILES),
                          lambda: phase_post_moe(k, P)]
            for ph in phases[:NPHASES]:
                ph()


def build_s5_layer():
    P = Prog("s5")
    declare_io(P)
    declare_common(P)
    declare_s5(P)
    emit_layer(P, "s5")
    return P


NCK = NSEQ // 128
DK = 128
DV = 256
NH = 4


def declare_gla(P):
    P.din("w_in", [D, 3104]); P.din("w_a2", [2, 16, 512]); P.din("b_a2", [2, 512])
    P.din("norm_g", [1, DV]); P.din("w_out", [D, D])
    _pf, P.prefix = P.prefix, ""
    P.din("triF", [128, 128]); P.din("triFs", [128, 128]); P.din("triB", [128, 128]); P.din("triBs", [128, 128])
    P.din("flipm", [128, 128])
    P.prefix = _pf
    P.dscr("US", [NSEQ, D], BF16)
    P.dscr("OS", [2, MYTOK, D])


def chunk_rows(c):
    return c * 128


def phase_gla_pass(k, P, d):
    t = P.t
    k.phase()
    pd = d ^ P.sw
    b_c = k.buf("glaconst"); b_cp = k.buf("glaconstp")
    idb = k.sb("gl_identb", [128, 128], BF16); k.dma("pool", idb[:], t["ident"], writes=[b_cp])
    tri = k.sb("gl_tri", [128, 128]); tris = k.sb("gl_tris", [128, 128])
    k.dma("sp", tri[:], t["triF" if d == 0 else "triB"], writes=[b_c])
    k.dma("sp", tris[:], t["triFs" if d == 0 else "triBs"], writes=[b_c])
    ones = k.sb("gl_ones", [128, 128])
    k.op("dve", lambda e: e.memset(ones[:], 1.0), writes=[b_c])
    win = k.sb("gl_win", [128, 8, 2064], BF16)
    for kc in range(8):
        k.dma("pool", win[:, kc, 0:2048], t["w_in"][kc * 128:(kc + 1) * 128, 0:2048], writes=[b_cp])
        k.dma("pool", win[:, kc, 2048:2064], t["w_in"][kc * 128:(kc + 1) * 128, 3072 + 16 * pd:3088 + 16 * pd],
              writes=[b_cp])
    wa2 = k.sb("gl_wa2", [16, 512]); k.dma("sp", wa2[:], t["w_a2"][pd], writes=[b_c])
    ba2 = k.sb("gl_ba2", [1, 512]); k.dma("sp", ba2[:], t["b_a2"][pd:pd + 1, :], writes=[b_c])
    S32 = k.sb("gl_S32", [128, NH, DV]); b_S32 = k.bufs(NH, "gl_S32")
    Sbf = k.sb("gl_Sbf", [128, NH, DV], BF16); b_Sbf = k.bufs(NH, "gl_Sbf")
    for hd in range(NH):
        k.op("dve", lambda e, hd=hd: e.memset(S32[:, hd, :], 0.0), writes=[b_S32[hd]])
        k.op("pool", lambda e, hd=hd: e.memset(Sbf[:, hd, :], 0.0), writes=[b_Sbf[hd]])
    NB = 2
    u = [k.sb(f"gl_u{i}", [128, D], BF16) for i in range(NB)]; b_u = k.bufs(NB, "gl_u")
    uT = [k.sb(f"gl_uT{i}", [128, 8, 128], BF16) for i in range(NB)]; b_uT = k.bufs(NB, "gl_uT")
    aT = k.sb("gl_aT", [16, 128]); b_aT = k.buf()
    ez = k.sb("gl_ez", [128, 512]); b_ez = k.buf()
    la = k.sb("gl_la", [128, 512]); b_la = k.buf()
    E1 = k.sb("gl_E1", [128, NH, 128]); b_E1 = k.buf()
    E2 = k.sb("gl_E2", [128, NH, 128]); b_E2 = k.buf()
    EK = k.sb("gl_EK", [128, 512]); b_EK = k.buf()
    qd = k.sb("gl_qd", [128, NH, 128], BF16); b_qd = k.buf()
    kd = k.sb("gl_kd", [128, NH, 128], BF16); b_kd = k.buf()
    ke = k.sb("gl_ke", [128, 512], BF16); b_ke = k.buf()
    v = k.sb("gl_v", [128, D], BF16); b_v = k.buf()
    attm = [k.sb(f"gl_attm{i}", [128, 128], BF16) for i in range(2)]; b_attm = k.bufs(2, "gl_attm")
    osb = [k.sb(f"gl_osb{i}", [128, D]) for i in range(2)]; b_osb = k.bufs(2, "gl_osb")
    pA = k.ps("gl_pA", [128, 8, 128], BF16); b_pA = k.pbuf("gl_pA")
    pM = k.ps("gl_pM", [128, 512]); b_pM = k.pbuf("gl_pM")
    pbT = k.ps("gl_pbT", [128, NH, 128]); b_pbT = k.pbuf("gl_pbT")
    pE = k.ps("gl_pE", [128, 512]); b_pE = k.pbuf("gl_pE")
    pq = k.ps("gl_pq", [128, NH, 128]); b_pq = k.pbuf("gl_pq")
    pk = k.ps("gl_pk", [128, NH, 128]); b_pk = k.pbuf("gl_pk")
    pat = pM; b_pat = b_pM
    pV = k.ps("gl_pV", [128, D]); b_pV = k.pbuf("gl_pV")
    b_os = k.buf("OSscr")
    if d == 0:
        order = list(range(0, 2 + 32))
    else:
        order = [1, 0] + list(range(NCK - 1, 1, -1))
    ecol = 127 if d == 0 else 0
    scale = DK ** -0.5
    US = t["US"]

    def load(ci):
        c = order[ci]
        i = ci % NB
        k.dma("sp", u[i][:], US[c * 128:(c + 1) * 128, :], writes=[b_u[i]])

    load(0)
    nmine = 0
    for ci, c in enumerate(order):
        i = ci % NB
        if ci + 1 < len(order):
            load(ci + 1)
        for kc in range(8):
            k.op("pe", lambda e, kc=kc: e.transpose(pA[:, kc, :], u[i][:, kc * 128:(kc + 1) * 128], idb[:]),
                 reads=[b_u[i], b_cp], writes=[b_pA])
        k.op("act", lambda e: e.copy(uT[i][:], pA[:]), reads=[b_pA], writes=[b_uT[i]])
        for kc in range(8):
            k.op("pe", lambda e, kc=kc: e.matmul(pM[0:16, 0:128], lhsT=win[:, kc, 2048:2064], rhs=uT[i][:, kc, :],
                                                 start=(kc == 0), stop=(kc == 7)),
                 reads=[b_cp, b_uT[i]], writes=[b_pM])
        k.op("dve", lambda e: e.tensor_copy(aT[:], pM[0:16, 0:128]), reads=[b_pM], writes=[b_aT])
        k.op("pe", lambda e: e.matmul(pM[:], lhsT=aT[:], rhs=wa2[:], start=True, stop=False),
             reads=[b_aT, b_c], writes=[b_pM])
        k.op("pe", lambda e: e.matmul(pM[:], lhsT=ones[0:1, :], rhs=ba2[:], start=False, stop=True),
             reads=[b_c], writes=[b_pM])
        k.op("act", lambda e: e.activation(ez[:], pM[:], AF.Exp, scale=-1.0), reads=[b_pM], writes=[b_ez])
        k.op("act", lambda e: e.activation(ez[:], ez[:], AF.Ln, bias=ones[:, 0:1], scale=1.0),
             reads=[b_ez, b_c], writes=[b_ez])
        k.op("pool", lambda e: e.tensor_scalar(la[:], ez[:], -1.0 / 16.0, None, ALU.mult), reads=[b_ez],
             writes=[b_la])
        for hd in range(NH):
            k.op("pe", lambda e, hd=hd: e.matmul(pbT[:, hd, :], lhsT=la[:, hd * 128:(hd + 1) * 128], rhs=tri[:],
                                                 start=True, stop=True), reads=[b_la, b_c], writes=[b_pbT])
        k.op("pe", lambda e: e.matmul(pE[:], lhsT=tris[:], rhs=la[:], start=True, stop=True),
             reads=[b_la, b_c], writes=[b_pE])
        k.op("act", lambda e: e.activation(E1[:], pbT[:], AF.Exp), reads=[b_pbT], writes=[b_E1])
        k.op("act", lambda e: e.activation(E2[:], pbT[:], AF.Exp, scale=-1.0), reads=[b_pbT], writes=[b_E2])
        k.op("act", lambda e: e.activation(EK[:], pE[:], AF.Exp), reads=[b_pE], writes=[b_EK])
        for hd in range(NH):
            for kc in range(8):
                k.op("pe", lambda e, hd=hd, kc=kc: e.matmul(pq[:, hd, :], lhsT=win[:, kc, hd * 128:(hd + 1) * 128],
                                                            rhs=uT[i][:, kc, :], start=(kc == 0), stop=(kc == 7)),
                     reads=[b_cp, b_uT[i]], writes=[b_pq])
        k.op("dve", lambda e: e.scalar_tensor_tensor(qd[:], pq[:], scale, E1[:], ALU.mult, ALU.mult),
             reads=[b_pq, b_E1], writes=[b_qd])
        for hd in range(NH):
            for kc in range(8):
                k.op("pe", lambda e, hd=hd, kc=kc: e.matmul(pk[:, hd, :],
                                                            lhsT=win[:, kc, 512 + hd * 128:512 + (hd + 1) * 128],
                                                            rhs=uT[i][:, kc, :], start=(kc == 0), stop=(kc == 7)),
                     reads=[b_cp, b_uT[i]], writes=[b_pk])
        k.op("dve", lambda e: e.tensor_tensor(kd[:], pk[:], E2[:], ALU.mult), reads=[b_pk, b_E2], writes=[b_kd])
        for kc in range(8):
            k.op("pe", lambda e, kc=kc: e.matmul(pE[:], lhsT=uT[i][:, kc, :], rhs=win[:, kc, 512:1024],
                                                 start=(kc == 0), stop=(kc == 7)),
                 reads=[b_cp, b_uT[i]], writes=[b_pE])
        k.op("dve", lambda e: e.tensor_tensor(ke[:], pE[:], EK[:], ALU.mult), reads=[b_pE, b_EK], writes=[b_ke])
        for hf in range(2):
            for kc in range(8):
                k.op("pe", lambda e, kc=kc, hf=hf: e.matmul(pV[:, hf * 512:(hf + 1) * 512], lhsT=uT[i][:, kc, :],
                                                            rhs=win[:, kc, 1024 + hf * 512:1024 + (hf + 1) * 512],
                                                            start=(kc == 0), stop=(kc == 7)),
                     reads=[b_cp, b_uT[i]], writes=[b_pV])
        k.op("act", lambda e: e.copy(v[:], pV[:]), reads=[b_pV], writes=[b_v])
        mine = (c == 0) or (2 <= c < 2 + 32)
        for hd in range(NH):
            am, b_am = attm[hd % 2], b_attm[hd % 2]
            k.op("pe", lambda e, hd=hd: e.matmul(pat[:, 0:128], lhsT=kd[:, hd, :], rhs=qd[:, hd, :],
                                                 start=True, stop=True), reads=[b_kd, b_qd], writes=[b_pat])
            k.op("dve", lambda e, am=am: e.tensor_tensor(am[:], pat[:, 0:128], tri[:], ALU.mult),
                 reads=[b_pat, b_c], writes=[b_am])
            if mine:
                k.op("pe", lambda e, hd=hd, am=am: e.matmul(pV[:, hd * DV:(hd + 1) * DV], lhsT=am[:],
                                                            rhs=v[:, hd * DV:(hd + 1) * DV], start=True, stop=False),
                     reads=[b_am, b_v], writes=[b_pV])
                k.op("pe", lambda e, hd=hd: e.matmul(pV[:, hd * DV:(hd + 1) * DV], lhsT=qd[:, hd, :],
                                                     rhs=Sbf[:, hd, :], start=False, stop=True),
                     reads=[b_qd, b_Sbf[hd]], writes=[b_pV])
            k.op("pe", lambda e, hd=hd: e.matmul(pat[:, 0:DV], lhsT=ke[:, hd * 128:(hd + 1) * 128],
                                                 rhs=v[:, hd * DV:(hd + 1) * DV], start=True, stop=True),
                 reads=[b_ke, b_v], writes=[b_pat])
            k.op("dve", lambda e, hd=hd: e.scalar_tensor_tensor(S32[:, hd, :], S32[:, hd, :],
                                                                E1[:, hd, ecol:ecol + 1], pat[:, 0:DV],
                                                                ALU.mult, ALU.add),
                 reads=[b_S32[hd], b_E1, b_pat], writes=[b_S32[hd]])
            k.op("pool", lambda e, hd=hd: e.tensor_copy(Sbf[:, hd, :], S32[:, hd, :]), reads=[b_S32[hd]],
                 writes=[b_Sbf[hd]])
        if mine:
            ob, b_ob = osb[nmine % 2], b_osb[nmine % 2]
            nmine += 1
            k.op("act", lambda e, ob=ob: e.copy(ob[:], pV[:]), reads=[b_pV], writes=[b_ob])
            row = 0 if c == 0 else MYCTX + (c - 2) * 128
            k.dma("sp", t["OS"][d, row:row + 128, :], ob[:], reads=[b_ob], writes=[b_os])
    k.flush()


def phase_gla_post(k, P, G, b_G):
    t = P.t
    k.phase()
    pm = PostMixer(k, P, G, b_G)
    b_c = k.buf("glpconst"); b_cp = k.buf("glpconstp")
    idb = k.sb("glp_identb", [128, 128], BF16); k.dma("pool", idb[:], t["ident"], writes=[b_cp])
    wg = k.sb("glp_wg", [128, 8, D], BF16)
    wo = k.sb("glp_wo", [128, 8, D], BF16)
    for kc in range(8):
        k.dma("pool", wg[:, kc, :], t["w_in"][kc * 128:(kc + 1) * 128, 2048:3072], writes=[b_cp])
        k.dma("pool", wo[:, kc, :], t["w_out"][kc * 128:(kc + 1) * 128, :], writes=[b_cp])
    ngB = k.sb("glp_ng", [128, DV]); k.dma("sp", ngB[:], bc(t["norm_g"], [128, DV]), writes=[b_c])
    epsc = k.sb("glp_eps", [128, 1]); k.op("dve", lambda e: e.memset(epsc[:], LN_EPS), writes=[b_c])
    NB = 2
    u = [k.sb(f"glp_u{i}", [128, D], BF16) for i in range(NB)]; b_u = k.bufs(NB, "glp_u")
    of = [k.sb(f"glp_of{i}", [128, D]) for i in range(NB)]; b_of = k.bufs(NB, "glp_of")
    ob = [k.sb(f"glp_ob{i}", [128, D]) for i in range(NB)]; b_ob = k.bufs(NB, "glp_ob")
    sq = k.sb("glp_sq", [128, D]); b_sq = k.buf()
    ms = k.sb("glp_ms", [128, NH]); b_ms = k.buf()
    sg = k.sb("glp_sg", [128, D]); b_sg = k.buf()
    zb = k.sb("glp_zb", [128, D], BF16); b_zb = k.buf()
    pT = k.ps("glp_pT", [128, 8, 128], BF16); b_pT = k.pbuf("glp_pT")
    xT = k.sb("glp_xT", [128, 8, 128], BF16); b_xT = k.buf()
    pg = k.ps("glp_pg", [128, D]); b_pg = k.pbuf("glp_pg")
    ym = [k.sb(f"glp_ym{i}", [128, D]) for i in range(NB)]; b_ym = k.bufs(NB, "glp_ym")
    for tt in range(NT_MY):
        i = tt % NB
        pm.prefetch(tt)
        r0, r1 = my_rows(tt)
        k.dma("sp", u[i][:], t["US"][r0:r1, :], writes=[b_u[i]])
        k.dma("act", of[i][:], t["OS"][0, tt * 128:(tt + 1) * 128, :], writes=[b_of[i]])
        k.dma("act", ob[i][:], t["OS"][1, tt * 128:(tt + 1) * 128, :], writes=[b_ob[i]])
        for kc in range(8):
            k.op("pe", lambda e, kc=kc: e.transpose(pT[:, kc, :], u[i][:, kc * 128:(kc + 1) * 128], idb[:]),
                 reads=[b_u[i], b_cp], writes=[b_pT])
        k.op("act", lambda e: e.copy(xT[:], pT[:]), reads=[b_pT], writes=[b_xT])
        for hf in range(2):
            for kc in range(8):
                k.op("pe", lambda e, kc=kc, hf=hf: e.matmul(pg[:, hf * 512:(hf + 1) * 512], lhsT=xT[:, kc, :],
                                                            rhs=wg[:, kc, hf * 512:(hf + 1) * 512],
                                                            start=(kc == 0), stop=(kc == 7)),
                     reads=[b_xT, b_cp], writes=[b_pg])
        k.op("act", lambda e: e.activation(sg[:], pg[:], AF.Silu), reads=[b_pg], writes=[b_sg])
        k.op("pool", lambda e: e.tensor_tensor(of[i][:], of[i][:], ob[i][:], ALU.add), reads=[b_of[i], b_ob[i]],
             writes=[b_of[i]])
        k.op("dve", lambda e: e.tensor_tensor(sq[:], of[i][:], of[i][:], ALU.mult), reads=[b_of[i]], writes=[b_sq])
        k.op("dve", lambda e: e.reduce_sum(ms[:], sq[:].rearrange("p (h e) -> p h e", e=DV),
                                           axis=mybir.AxisListType.X), reads=[b_sq], writes=[b_ms])
        k.op("act", lambda e: e.activation(ms[:], ms[:], AF.Sqrt, bias=epsc[:], scale=1.0 / DV),
             reads=[b_ms, b_c], writes=[b_ms])
        k.op("dve", lambda e: e.reciprocal(ms[:], ms[:]), reads=[b_ms], writes=[b_ms])
        k.op("dve", lambda e: e.tensor_tensor(sq[:].rearrange("p (h e) -> p h e", e=DV),
                                              of[i][:].rearrange("p (h e) -> p h e", e=DV),
                                              bc(ms[:].unsqueeze(2), [128, NH, DV]), ALU.mult),
             reads=[b_of[i], b_ms], writes=[b_sq])
        k.op("pool", lambda e: e.tensor_tensor(sq[:].rearrange("p (h e) -> p h e", e=DV),
                                               sq[:].rearrange("p (h e) -> p h e", e=DV),
                                               bc(ngB[:].unsqueeze(1), [128, NH, DV]), ALU.mult),
             reads=[b_sq, b_c], writes=[b_sq])
        k.op("dve", lambda e: e.tensor_tensor(zb[:], sq[:], sg[:], ALU.mult), reads=[b_sq, b_sg], writes=[b_zb])
        for kc in range(8):
            k.op("pe", lambda e, kc=kc: e.transpose(pT[:, kc, :], zb[:, kc * 128:(kc + 1) * 128], idb[:]),
                 reads=[b_zb, b_cp], writes=[b_pT])
        k.op("act", lambda e: e.copy(xT[:], pT[:]), reads=[b_pT], writes=[b_xT])
        for hf in range(2):
            for kc in range(8):
                k.op("pe", lambda e, kc=kc, hf=hf: e.matmul(pg[:, hf * 512:(hf + 1) * 512], lhsT=xT[:, kc, :],
                                                            rhs=wo[:, kc, hf * 512:(hf + 1) * 512],
                                                            start=(kc == 0), stop=(kc == 7)),
                     reads=[b_xT, b_cp], writes=[b_pg])
        k.op("act", lambda e: e.copy(ym[i][:], pg[:]), reads=[b_pg], writes=[b_ym[i]])
        pm.emit(tt, ym[i][:], b_ym[i])
    k.flush()


def build_gla_layer():
    P = Prog("gla")
    declare_io(P)
    declare_common(P)
    declare_gla(P)
    emit_layer(P, "gla")
    return P


def gla_inputs(inp, j, s):
    c = consts()
    w_in = inp["gla_w_in"][j]
    w_a2, b_a2 = inp["gla_w_a2"][j], inp["gla_b_a2"][j]
    if s == 1:
        w_in = np.concatenate([w_in[:, :3072], w_in[:, 3088:3104], w_in[:, 3072:3088]], axis=1)
        w_a2, b_a2 = w_a2[::-1], b_a2[::-1]
    return {"w_in": w_in, "w_a2": w_a2, "b_a2": b_a2, "norm_g": inp["gla_norm_g"][j][None],
            "w_out": inp["gla_w_out"][j], "triF": c["triF"], "triFs": c["triFs"], "triB": c["triB"],
            "triBs": c["triBs"]}


def phase_handoff(k, P, kind_i, kind_n, ho0, ho1, hs0, hs1):
    t = P.t
    k.phase()
    b_c = k.buf("hoconst")
    flip = k.sb("ho_flip", [128, 128]); k.dma("sp", flip[:], t["flipm"], writes=[b_c])
    NB = 3
    a = [k.sb(f"ho_a{i}", [128, D]) for i in range(NB)]; b_a = k.bufs(NB, "ho_a")
    f = [k.sb(f"ho_f{i}", [128, D]) for i in range(NB)]; b_f = k.bufs(NB, "ho_f")
    pf = [k.ps(f"ho_p{i}", [128, D]) for i in range(2)]; b_pf = k.pbufs(2, "ho_p")
    NAT, CN = t["NAT"], t["CNAT"]
    b_nat = k.buf("NAT")
    cnt = [0]

    def nat_tile(kind, j):
        if kind == "s5":
            return NAT[j * 128:(j + 1) * 128, :]
        return NAT.rearrange("(r w) d -> w r d", w=64)[j]

    def move(dst, src, do_flip, b_dst):
        i = cnt[0] % NB
        cnt[0] += 1
        k.dma("sp", a[i][:], src, reads=[b_nat], writes=[b_a[i]])
        if not do_flip:
            k.dma("act", dst, a[i][:], reads=[b_a[i]], writes=[b_dst])
            return
        pb = cnt[0] % 2
        for hf in range(2):
            k.op("pe", lambda e, hf=hf: e.matmul(pf[pb][:, hf * 512:(hf + 1) * 512], lhsT=flip[:],
                                                 rhs=a[i][:, hf * 512:(hf + 1) * 512], start=True, stop=True),
                 reads=[b_a[i], b_c], writes=[b_pf[pb]])
        k.op("act", lambda e: e.copy(f[i][:], pf[pb][:]), reads=[b_pf[pb]], writes=[b_f[i]])
        k.dma("act", dst, f[i][:], reads=[b_f[i]], writes=[b_dst])

    move(CN[0:128, :], ho0[0:128, :], False, b_nat)
    move(CN[128:256, :], ho1[0:128, :], True, b_nat)
    for j in range(32):
        move(nat_tile(kind_i, j), ho0[MYCTX + j * 128:MYCTX + (j + 1) * 128, :], False, b_nat)
        move(nat_tile(kind_i, 32 + j), ho1[MYCTX + (31 - j) * 128:MYCTX + (32 - j) * 128, :], True, b_nat)
    b_hs = k.buf("HSnext")
    for c in range(2):
        move(hs0[c * 128:(c + 1) * 128, :], CN[c * 128:(c + 1) * 128, :], False, b_hs)
        move(hs1[(1 - c) * 128:(2 - c) * 128, :], CN[c * 128:(c + 1) * 128, :], True, b_hs)
    for j in range(64):
        move(hs0[NCTX + j * 128:NCTX + (j + 1) * 128, :], nat_tile(kind_n, j), False, b_hs)
        move(hs1[NCTX + (63 - j) * 128:NCTX + (64 - j) * 128, :], nat_tile(kind_n, j), True, b_hs)
    k.flush()


def build_fused():
    P = Prog("fused")
    declare_io(P)
    P.dscr("NAT", [NLAT, D]); P.dscr("CNAT", [NCTX, D])
    for v in range(NV):
        for nm in ("HSA", "HSB"):
            P.dscr(f"{nm}_{v}", [NSEQ, D])
        P.dscr(f"HO_{v}", [MYTOK, D])
    kinds = ["s5", "gla", "s5", "gla"]
    for i, kind in enumerate(kinds):
        P.prefix = f"L{i}_"
        declare_common(P)
        (declare_s5 if kind == "s5" else declare_gla)(P)
    layer_t = {}
    with ExitStack() as st:
        k = K(P.nc, st)
        G = k.sb("Gall", [128, NT_MY, NE], glob=True); b_G = k.buf("Gall")
        for i, kind in enumerate(kinds):
            for full, ap in P.ext.items():
                if full.startswith(f"L{i}_"):
                    P.t[full[len(f"L{i}_"):]] = ap
            for v in range(NV):
                P.sw = v % 2
                P.t["cvec"] = P.ext[f"cvec_{v}"]
                if i == 0:
                    P.t["hseq"] = P.ext[f"hseq_{v}"]
                else:
                    P.t["hseq"] = P.t[f"{'HSA' if i % 2 == 1 else 'HSB'}_{v}"]
                P.t["hout"] = P.ext[f"hout_{v}"] if i == 3 else P.t[f"HO_{v}"]
                if kind == "s5":
                    phases = [lambda: phase_adaln(k, P), lambda: phase_modulate(k, P, P.t["US"]),
                              lambda: phase_s5_setup(k, P, 0), lambda: phase_s5_setup(k, P, 1),
                              lambda: phase_s5(k, P), lambda: phase_s5_post(k, P, G, b_G),
                              lambda: phase_moe(k, P, G, b_G, SUPERTILES), lambda: phase_post_moe(k, P)]
                else:
                    phases = [lambda: phase_adaln(k, P), lambda: phase_modulate(k, P, P.t["US"]),
                              lambda: phase_gla_pass(k, P, 0), lambda: phase_gla_pass(k, P, 1),
                              lambda: phase_gla_post(k, P, G, b_G),
                              lambda: phase_moe(k, P, G, b_G, SUPERTILES), lambda: phase_post_moe(k, P)]
                for ph in phases:
                    ph()
            if i < 3:
                nxt = "HSA" if (i + 1) % 2 == 1 else "HSB"
                for bb in range(NV // 2):
                    phase_handoff(k, P, kind, kinds[i + 1], P.t[f"HO_{2 * bb}"], P.t[f"HO_{2 * bb + 1}"],
                                  P.t[f"{nxt}_{2 * bb}"], P.t[f"{nxt}_{2 * bb + 1}"])
    return P


_FUSED = []


def run_fused(inp):
    if not _FUSED:
        _FUSED.append(build_fused())
    P = _FUSED[0]
    h, hc = inp["x"], inp["ctx"]
    allv = [(b, s) for b in range(4) for s in range(2)]
    groups = [allv[p * NV:(p + 1) * NV] for p in range(NPHYS)]
    maps = []
    for grp in groups:
        m = dict(consts())
        for v, (b, s) in enumerate(grp):
            lat, ctx = h[b], hc[b]
            if s == 1:
                lat, ctx = lat[::-1], ctx[::-1]
            m[f"hseq_{v}"] = np.concatenate([ctx, lat], axis=0)
            m[f"cvec_{v}"] = np.stack([inp["c"][b], inp["c_ctx"]])
        for i in range(4):
            lw = common_inputs(inp, i, 0)
            lw.update(s5_inputs(inp, i // 2, 0) if i % 2 == 0 else gla_inputs(inp, i // 2, 0))
            for kk, vv in lw.items():
                if kk not in m:
                    m[f"L{i}_{kk}"] = vv
        ins = [n for n in P.ext if not n.startswith("hout_")]
        maps.append({n: np.ascontiguousarray(m[n], dtype=np.float32) for n in ins})
    res = run_bass_kernel_spmd(P.nc, maps, core_ids=list(range(NPHYS)))
    hn = np.empty_like(h)
    for p, grp in enumerate(groups):
        for v, (b, s) in enumerate(grp):
            o = res.results[p][f"hout_{v}"][MYCTX:]
            if s == 0:
                lat0 = o
            else:
                hn[b] = seq_unorder("gla", np.concatenate([lat0, o[::-1]], axis=0))
    return hn


DEBUG = False
_CONST = {}


def consts():
    if not _CONST:
        idx = np.arange(128)
        tau = idx // 16
        _CONST["ident"] = np.eye(128, dtype=np.float32)
        _CONST["maskF"] = (tau[None, :] >= tau[:, None]).astype(np.float32)
        _CONST["maskB"] = (tau[:, None] >= tau[None, :]).astype(np.float32)
        sw = np.zeros((128, 128), np.float32)
        sw[idx, (idx + 64) % 128] = 1.0
        _CONST["swapm"] = sw
        _CONST["flipm"] = np.ascontiguousarray(np.eye(128, dtype=np.float32)[::-1])
        jj, ii = idx[:, None], idx[None, :]
        _CONST["triF"] = (jj <= ii).astype(np.float32)
        _CONST["triFs"] = (jj > ii).astype(np.float32)
        _CONST["triB"] = (jj >= ii).astype(np.float32)
        _CONST["triBs"] = (jj < ii).astype(np.float32)
    return _CONST


def seq_order(kind, hb):
    if kind == "s5":
        return hb
    return hb.reshape(128, 64, D).transpose(1, 0, 2).reshape(NLAT, D)


def seq_unorder(kind, lat):
    if kind == "s5":
        return lat
    return lat.reshape(64, 128, D).transpose(1, 0, 2).reshape(NLAT, D)


def common_inputs(inp, i, b):
    c = consts()
    m = {
        "cvec": np.ascontiguousarray(np.stack([inp["c"][b], inp["c_ctx"]])),
        "w_ada": inp["w_ada"][i], "b_ada": inp["b_ada"][i][None],
        "ln1_g": inp["ln1_g"][i][None], "ln1_b": inp["ln1_b"][i][None],
        "ln2_g": inp["ln2_g"][i][None], "ln2_b": inp["ln2_b"][i][None],
        "w_router": inp["moe_w_router"][i], "b_router": inp["moe_b_router"][i][None],
        "w_up": inp["moe_w_up"][i], "b_up": inp["moe_b_up"][i],
        "w_down": inp["moe_w_down"][i], "b_down": inp["moe_b_down"][i],
        "ident": c["ident"],
    }
    return m


def s5_inputs(inp, j, s):
    c = consts()
    sl = slice(None) if s == 0 else slice(None, None, -1)
    f = lambda a: np.ascontiguousarray(a[j][sl])
    return {
        "lam_re": f(inp["s5_lam_re"]), "lam_im": f(inp["s5_lam_im"]), "log_step": f(inp["s5_log_step"]),
        "b_re": f(inp["s5_b_re"]), "b_im": f(inp["s5_b_im"]), "c_re": f(inp["s5_c_re"]), "c_im": f(inp["s5_c_im"]),
        "s5_d": inp["s5_d"][j][None], "w_glu": inp["s5_w_glu"][j],
        "maskF": c["maskF"], "maskB": c["maskB"], "swapm": c["swapm"],
    }


_PROGS = {}


def get_prog(kind):
    if kind not in _PROGS:
        _PROGS[kind] = build_s5_layer() if kind == "s5" else build_gla_layer()
    return _PROGS[kind]


def run_layer(inp, i, h, hc, cores=None):
    kind = "s5" if i % 2 == 0 else "gla"
    P = get_prog(kind)
    allv = [(b, s) for b in range(4) for s in range(2)]
    groups = [allv[p * NV:(p + 1) * NV] for p in range(NPHYS)]
    maps = []
    for grp in groups:
        m = common_inputs(inp, i, 0)
        m.pop("cvec")
        for v, (b, s) in enumerate(grp):
            lat = seq_order(kind, h[b])
            ctx = hc[b]
            if s == 1:
                lat, ctx = lat[::-1], ctx[::-1]
            m[f"hseq_{v}"] = np.concatenate([ctx, lat], axis=0)
            m[f"cvec_{v}"] = np.stack([inp["c"][b], inp["c_ctx"]])
        m.update(s5_inputs(inp, i // 2, 0) if kind == "s5" else gla_inputs(inp, i // 2, 0))
        maps.append({kk: np.ascontiguousarray(vv, dtype=np.float32) for kk, vv in m.items() if kk in P.t})
    res = run_bass_kernel_spmd(P.nc, maps, core_ids=list(range(NPHYS)))
    hn = np.empty_like(h)
    hcn = np.empty_like(hc)
    lat_new = {}
    for p, grp in enumerate(groups):
        for v, (b, s) in enumerate(grp):
            o = res.results[p][f"hout_{v}"]
            if s == 0:
                hcn[b, 0:128] = o[0:128]
                lat_new[(b, 0)] = o[128:]
            else:
                hcn[b, 128:256] = o[0:128][::-1]
                lat_new[(b, 1)] = o[128:][::-1]
    for b in range(4):
        hn[b] = seq_unorder(kind, np.concatenate([lat_new[(b, 0)], lat_new[(b, 1)]], axis=0))
    return hn, hcn, res


def kernel(**inp):
    inp = {kk: np.asarray(v, dtype=np.float32) for kk, v in inp.items()}
    return run_fused(inp)
```
